# Optimizing a Trainium2 kernel written in Bass

```python
import math
import jax, jax.numpy as jnp
from jax import lax
import numpy as np

D_MODEL = 1024
BATCH = 4
SEQ = 8192
DEPTH = 4

EPS = 1e-6
MLA_HEADS = 4
MLA_NOPE = 128
MLA_ROPE = 64
MLA_V = 128
MLA_Q_RANK = 384
MLA_KV_RANK = 256
MLA_ROPE_BASE = 10000.0
Q_BLOCK = 128
GLA_HEADS = 4
GLA_DK = 32
GLA_DV = 64
GLA_GATE_RANK = 16
GLA_GATE_NORM = 16.0
GLA_CHUNK = 64
RET_HEADS = 4
RET_DK = 32
RET_DV = 64
RET_CHUNK = 64
RET_ROPE_BASE = 10000.0
D_MIX = MLA_HEADS * MLA_V + GLA_HEADS * GLA_DV + RET_HEADS * RET_DV
IN_SIZES = (MLA_Q_RANK, MLA_KV_RANK, MLA_ROPE,
            GLA_HEADS * GLA_DK, GLA_HEADS * GLA_DK, GLA_HEADS * GLA_DV, GLA_GATE_RANK, GLA_HEADS * GLA_DV,
            RET_HEADS * RET_DK, RET_HEADS * RET_DK, RET_HEADS * RET_DV, RET_HEADS * RET_DV)
D_IN = sum(IN_SIZES)
D_FF = 2816
CONV_W = 3

kernel_name = "hybrid_mla_gla_retnet_convffn"


def rmsnorm(x, gain):
    xf = x.astype(jnp.float32)
    y = xf * lax.rsqrt(jnp.mean(xf * xf, axis=-1, keepdims=True) + EPS)
    return (y * gain.astype(jnp.float32)).astype(x.dtype)


def head_rmsnorm(x, gain):
    H, d = x.shape[-2], x.shape[-1]
    return rmsnorm(x, gain.reshape(H, d))


def rope_tables(S, dim, base):
    inv = base ** (-(jnp.arange(0, dim, 2, dtype=jnp.float32) / dim))
    ang = jnp.arange(S, dtype=jnp.float32)[:, None] * inv[None, :]
    return jnp.cos(ang), jnp.sin(ang)


def apply_rope(x, cos, sin):
    half = x.shape[-1] // 2
    c = cos[None, :, None, :].astype(x.dtype)
    s = sin[None, :, None, :].astype(x.dtype)
    x1, x2 = x[..., :half], x[..., half:]
    return jnp.concatenate([x1 * c - x2 * s, x2 * c + x1 * s], axis=-1)


def mla_attention(q_nope, q_rope, k_nope, k_rope, v):
    B, S, H, dn = q_nope.shape
    dr = q_rope.shape[-1]
    nb = S // Q_BLOCK
    scale = (MLA_NOPE + MLA_ROPE) ** -0.5
    qn = q_nope.reshape(B, nb, Q_BLOCK, H, dn).transpose(1, 0, 2, 3, 4)
    qr = q_rope.reshape(B, nb, Q_BLOCK, H, dr).transpose(1, 0, 2, 3, 4)
    key_pos = jnp.arange(S)

    def one_block(args):
        qn_b, qr_b, start = args
        s = (jnp.einsum('bqhd,bkhd->bhqk', qn_b, k_nope)
             + jnp.einsum('bqhd,bkd->bhqk', qr_b, k_rope)).astype(jnp.float32) * scale
        q_pos = start + jnp.arange(Q_BLOCK)
        mask = key_pos[None, :] <= q_pos[:, None]
        s = jnp.where(mask[None, None], s, jnp.finfo(jnp.float32).min)
        p = jax.nn.softmax(s, axis=-1).astype(v.dtype)
        return jnp.einsum('bhqk,bkhd->bqhd', p, v)

    out = lax.map(one_block, (qn, qr, jnp.arange(nb) * Q_BLOCK))
    return out.transpose(1, 0, 2, 3, 4).reshape(B, S, H, v.shape[-1])


def mla_branch(c_q, c_kv, k_rope, q_norm, w_uq, kv_norm, w_ukv, cos, sin):
    B, S, _ = c_q.shape
    q = (rmsnorm(c_q, q_norm) @ w_uq).reshape(B, S, MLA_HEADS, MLA_NOPE + MLA_ROPE)
    q_nope, q_rope = q[..., :MLA_NOPE], apply_rope(q[..., MLA_NOPE:], cos, sin)
    kv = (rmsnorm(c_kv, kv_norm) @ w_ukv).reshape(B, S, MLA_HEADS, MLA_NOPE + MLA_V)
    k_nope, v = kv[..., :MLA_NOPE], kv[..., MLA_NOPE:]
    k_r = apply_rope(k_rope[:, :, None, :], cos, sin)[:, :, 0, :]
    return mla_attention(q_nope, q_rope, k_nope, k_r, v)


def gla_chunked(q, k, v, log_a):
    B, S, H, dk = q.shape
    dv = v.shape[-1]
    C = GLA_CHUNK
    n = S // C
    to_chunks = lambda t: t.astype(jnp.float32).reshape(B, n, C, H, t.shape[-1]).transpose(1, 0, 3, 2, 4)
    qc, kc, vc, gc = to_chunks(q), to_chunks(k), to_chunks(v), to_chunks(log_a)
    tril = jnp.tril(jnp.ones((C, C), dtype=bool))

    def step(state, inp):
        qb, kb, vb, gb = inp
        b = jnp.cumsum(gb, axis=-2)
        inter = jnp.einsum('bhik,bhkv->bhiv', qb * jnp.exp(b), state)
        diff = b[..., :, None, :] - b[..., None, :, :]
        dec = jnp.where(tril[:, :, None], jnp.exp(jnp.where(tril[:, :, None], diff, 0.0)), 0.0)
        A = jnp.einsum('bhik,bhjk,bhijk->bhij', qb, kb, dec)
        intra = jnp.einsum('bhij,bhjv->bhiv', A, vb)
        b_end = b[..., -1:, :]
        new_state = state * jnp.exp(b_end)[..., 0, :, None] + jnp.einsum(
            'bhjk,bhjv->bhkv', kb * jnp.exp(b_end - b), vb)
        return new_state, inter + intra

    state0 = jnp.zeros((B, H, dk, dv), jnp.float32)
    _, out = lax.scan(step, state0, (qc, kc, vc, gc))
    return out.transpose(1, 0, 3, 2, 4).reshape(B, S, H, dv).astype(v.dtype)


def gla_branch(q, k, v, gate_lr, g, w_gate, b_gate, out_norm):
    B, S, _ = q.shape
    qh = q.reshape(B, S, GLA_HEADS, GLA_DK) * (GLA_DK ** -0.5)
    kh = k.reshape(B, S, GLA_HEADS, GLA_DK)
    vh = v.reshape(B, S, GLA_HEADS, GLA_DV)
    gk = (gate_lr @ w_gate + b_gate).astype(jnp.float32)
    log_a = (jax.nn.log_sigmoid(gk) / GLA_GATE_NORM).reshape(B, S, GLA_HEADS, GLA_DK)
    o = gla_chunked(qh, kh, vh, log_a)
    o = head_rmsnorm(o, out_norm).reshape(B, S, GLA_HEADS * GLA_DV)
    return o * jax.nn.silu(g)


def retention_chunked(q, k, v):
    B, S, H, dk = q.shape
    dv = v.shape[-1]
    C = RET_CHUNK
    n = S // C
    to_chunks = lambda t: t.astype(jnp.float32).reshape(B, n, C, H, t.shape[-1]).transpose(0, 3, 1, 2, 4)
    qc, kc, vc = to_chunks(q), to_chunks(k), to_chunks(v)
    log_g = jnp.log(1.0 - 2.0 ** (-5.0 - jnp.arange(H, dtype=jnp.float32)))
    idx = jnp.arange(C, dtype=jnp.float32)
    diff = idx[:, None] - idx[None, :]
    D = jnp.where(diff >= 0, jnp.exp(log_g[:, None, None] * jnp.maximum(diff, 0.0)), 0.0)
    scores = jnp.einsum('bhnid,bhnjd->bhnij', qc, kc) * D[None, :, None]
    intra = jnp.einsum('bhnij,bhnjv->bhniv', scores, vc)
    k_dec = kc * jnp.exp(log_g[:, None] * (C - 1 - idx)[None, :])[None, :, None, :, None]
    U = jnp.einsum('bhnjd,bhnjv->nbhdv', k_dec, vc)
    chunk_decay = jnp.exp(log_g * C)[None, :, None, None]

    def step(R, u):
        return R * chunk_decay + u, R

    _, R_prev = lax.scan(step, jnp.zeros((B, H, dk, dv), jnp.float32), U)
    q_dec = qc * jnp.exp(log_g[:, None] * (idx + 1.0)[None, :])[None, :, None, :, None]
    inter = jnp.einsum('bhnid,nbhdv->bhniv', q_dec, R_prev)
    out = (intra + inter).transpose(0, 2, 3, 1, 4).reshape(B, S, H, dv)
    return out.astype(v.dtype)


def retention_branch(q, k, v, g, out_norm, cos, sin):
    B, S, _ = q.shape
    qh = apply_rope(q.reshape(B, S, RET_HEADS, RET_DK), cos, sin)
    kh = apply_rope(k.reshape(B, S, RET_HEADS, RET_DK), cos, sin) * (RET_DK ** -0.5)
    vh = v.reshape(B, S, RET_HEADS, RET_DV)
    o = retention_chunked(qh, kh, vh)
    o = head_rmsnorm(o, out_norm).reshape(B, S, RET_HEADS * RET_DV)
    return o * jax.nn.silu(g)


def causal_dwconv(a, w, b):
    S = a.shape[1]
    p = jnp.pad(a, ((0, 0), (CONV_W - 1, 0), (0, 0)))
    out = b
    for kk in range(CONV_W):
        out = out + p[:, kk:kk + S] * w[kk]
    return out


def setup_inputs(seed: int = 0) -> dict:
    key = jax.random.key(seed)
    ks = jax.random.split(key, 19)
    f32 = jnp.float32
    nrm = lambda k, shape, scale: jax.random.normal(k, shape, f32) * scale
    gain = lambda k, shape: 1.0 + 0.02 * jax.random.normal(k, shape, f32)
    return {
        "x": nrm(ks[0], (BATCH, SEQ, D_MODEL), 1.0),
        "attn_norm": gain(ks[1], (DEPTH, D_MODEL)),
        "w_in": nrm(ks[2], (DEPTH, D_MODEL, D_IN), D_MODEL ** -0.5),
        "mla_q_norm": gain(ks[3], (DEPTH, MLA_Q_RANK)),
        "mla_w_uq": nrm(ks[4], (DEPTH, MLA_Q_RANK, MLA_HEADS * (MLA_NOPE + MLA_ROPE)), MLA_Q_RANK ** -0.5),
        "mla_kv_norm": gain(ks[5], (DEPTH, MLA_KV_RANK)),
        "mla_w_ukv": nrm(ks[6], (DEPTH, MLA_KV_RANK, MLA_HEADS * (MLA_NOPE + MLA_V)), MLA_KV_RANK ** -0.5),
        "mla_out_norm": gain(ks[7], (DEPTH, MLA_HEADS * MLA_V)),
        "gla_w_gate": nrm(ks[8], (DEPTH, GLA_GATE_RANK, GLA_HEADS * GLA_DK), GLA_GATE_RANK ** -0.5),
        "gla_b_gate": nrm(ks[9], (DEPTH, GLA_HEADS * GLA_DK), 0.02),
        "gla_out_norm": gain(ks[10], (DEPTH, GLA_HEADS * GLA_DV)),
        "ret_out_norm": gain(ks[11], (DEPTH, RET_HEADS * RET_DV)),
        "w_out": nrm(ks[12], (DEPTH, D_MIX, D_MODEL), D_MIX ** -0.5),
        "ffn_norm": gain(ks[13], (DEPTH, D_MODEL)),
        "ffn_w_up": nrm(ks[14], (DEPTH, D_MODEL, 2 * D_FF), D_MODEL ** -0.5),
        "ffn_conv_w": nrm(ks[15], (DEPTH, CONV_W, D_FF), CONV_W ** -0.5),
        "ffn_conv_b": nrm(ks[16], (DEPTH, D_FF), 0.02),
        "ffn_w_down": nrm(ks[17], (DEPTH, D_FF, D_MODEL), D_FF ** -0.5),
        "final_norm": gain(ks[18], (D_MODEL,)),
    }


def reference(x, attn_norm, w_in, mla_q_norm, mla_w_uq, mla_kv_norm, mla_w_ukv, mla_out_norm,
              gla_w_gate, gla_b_gate, gla_out_norm, ret_out_norm, w_out, ffn_norm, ffn_w_up,
              ffn_conv_w, ffn_conv_b, ffn_w_down, final_norm):
    B, S, _ = x.shape
    mla_cos, mla_sin = rope_tables(S, MLA_ROPE, MLA_ROPE_BASE)
    ret_cos, ret_sin = rope_tables(S, RET_DK, RET_ROPE_BASE)
    split_points = []
    acc = 0
    for sz in IN_SIZES[:-1]:
        acc += sz
        split_points.append(acc)

    for l in range(DEPTH):
        h = rmsnorm(x, attn_norm[l])
        z = h @ w_in[l]
        (c_q, c_kv, k_rope, g_q, g_k, g_v, g_lr, g_g, r_q, r_k, r_v, r_g) = jnp.split(z, split_points, axis=-1)
        y_a = mla_branch(c_q, c_kv, k_rope, mla_q_norm[l], mla_w_uq[l], mla_kv_norm[l], mla_w_ukv[l],
                         mla_cos, mla_sin)
        y_a = head_rmsnorm(y_a, mla_out_norm[l]).reshape(B, S, MLA_HEADS * MLA_V)
        y_b = gla_branch(g_q, g_k, g_v, g_lr, g_g, gla_w_gate[l], gla_b_gate[l], gla_out_norm[l])
        y_c = retention_branch(r_q, r_k, r_v, r_g, ret_out_norm[l], ret_cos, ret_sin)
        x = x + jnp.concatenate([y_a, y_b, y_c], axis=-1) @ w_out[l]
        h = rmsnorm(x, ffn_norm[l])
        u = h @ ffn_w_up[l]
        a, bv = u[..., :D_FF], u[..., D_FF:]
        a = causal_dwconv(a, ffn_conv_w[l], ffn_conv_b[l])
        x = x + (jax.nn.silu(a) * bv) @ ffn_w_down[l]

    return rmsnorm(x, final_norm)
```

```python
import numpy as np
import ml_dtypes
import concourse.bass as bass
import concourse.mybir as mybir
from concourse.bass_utils import run_bass_kernel_spmd

F32 = mybir.dt.float32
BF16 = mybir.dt.bfloat16
AF = mybir.ActivationFunctionType
ALU = mybir.AluOpType
AX = mybir.AxisListType

D = 1024
DEPTH = 4
EPS = 1e-6
DIN = 2256
DFF = 2816
NFC = DFF // 128
SCALE_MLA = 192 ** -0.5

ENGS = ("pe", "act", "dve", "pool", "sp")
KDMA = 8


class Buf:
    __slots__ = ("lw", "rd", "name")

    def __init__(self, name=""):
        self.lw = None
        self.rd = []
        self.name = name


class Op:
    __slots__ = ("eng", "fn", "deps", "kind", "signal", "tok", "idx", "dma_n")


class Prog:
    def __init__(self, nc):
        self.nc = nc
        self.ops = {e: [] for e in ENGS}
        self.all = []
        self.ndma = {e: 0 for e in ENGS}
        self.last = {e: None for e in ENGS}
        self.pending_barrier = {e: [] for e in ENGS}
        self.dmas = {e: [] for e in ENGS}
        self.lastcc = None

    def op(self, eng, fn, reads=(), writes=(), kind="c"):
        o = Op()
        o.eng, o.fn, o.kind, o.signal, o.tok = eng, fn, kind, False, None
        deps = []
        for b in reads:
            if b.lw is not None:
                deps.append(b.lw)
        for b in writes:
            if b.lw is not None:
                deps.append(b.lw)
            deps.extend(b.rd)
        deps.extend(self.pending_barrier[eng])
        self.pending_barrier[eng] = []
        dd = []
        seen = set()
        for d in deps:
            if id(d) in seen:
                continue
            seen.add(id(d))
            if d.eng == "pe" and eng == "pe" and d.kind == "c" and kind == "c":
                continue
            dd.append(d)
        o.deps = dd
        for b in writes:
            b.lw = o
            b.rd = []
        for b in reads:
            b.rd.append(o)
        if kind == "d":
            o.dma_n = self.ndma[eng]
            self.ndma[eng] += 1
            self.dmas[eng].append(o)
        if kind == "cc":
            if self.lastcc is not None and all(x is not self.lastcc for x in o.deps):
                o.deps.append(self.lastcc)
            self.lastcc = o
            self.dmas[eng].append(o)
        o.idx = len(self.ops[eng])
        self.ops[eng].append(o)
        self.all.append(o)
        self.last[eng] = o
        return o

    def barrier(self):
        lasts = [self.last[e] for e in ENGS if self.last[e] is not None]
        for e in ENGS:
            lasts.extend(self.dmas[e][-(KDMA + 2):])
        for e in ENGS:
            self.pending_barrier[e] = list(lasts)

    def dma(self, out, in_, reads=(), writes=(), eng="sp"):
        return self.op(eng, lambda e: e.dma_start(out=out, in_=in_), reads, writes, kind="d")

    def mm(self, out, lhsT, rhs, start, stop, reads=(), writes=()):
        return self.op("pe", lambda e: e.matmul(out, lhsT, rhs, start=start, stop=stop), reads, writes)

    def tr(self, out, in_, ident, reads=(), writes=()):
        return self.op("pe", lambda e: e.transpose(out, in_, ident), reads, writes)

    def act(self, out, in_, func, reads=(), writes=(), bias=None, scale=None, accum=None):
        def fn(e):
            kw = {}
            if bias is not None:
                kw["bias"] = bias
            if scale is not None:
                kw["scale"] = scale
            if accum is not None:
                kw["accum_out"] = accum
            return e.activation(out, in_, func, **kw)
        return self.op("act", fn, reads, writes)

    def tt(self, eng, out, in0, in1, op, reads=(), writes=()):
        return self.op(eng, lambda e: e.tensor_tensor(out, in0, in1, op), reads, writes)

    def ts(self, eng, out, in0, s1, s2, op0, op1=None, reads=(), writes=()):
        def fn(e):
            if op1 is None:
                return e.tensor_scalar(out, in0, s1, None, op0)
            return e.tensor_scalar(out, in0, s1, s2, op0, op1)
        return self.op(eng, fn, reads, writes)

    def stt(self, eng, out, in0, scalar, in1, op0, op1, reads=(), writes=()):
        eng = "dve"
        return self.op(eng, lambda e: e.scalar_tensor_tensor(out, in0, scalar, in1, op0, op1), reads, writes)

    def cp(self, eng, out, in_, reads=(), writes=()):
        if eng == "act":
            return self.op("act", lambda e: e.copy(out=out, in_=in_), reads, writes)
        return self.op(eng, lambda e: e.tensor_copy(out, in_), reads, writes)

    def memset(self, eng, ap, val, writes=()):
        return self.op(eng, lambda e: e.memset(ap, val), (), writes)

    def emit(self):
        nc = self.nc
        for o in self.all:
            for d in o.deps:
                d.signal = True
        import contextlib
        with contextlib.ExitStack() as es:
            csem = {e: es.enter_context(nc.semaphore("c_" + e)) for e in ("pe", "act", "dve", "pool")}
            dsem = {e: [es.enter_context(nc.semaphore("d_%s%d" % (e, i))) for i in range(KDMA)]
                    for e in ENGS if self.ndma[e] > 0}
            ccsem = es.enter_context(nc.semaphore("ccs"))
            cnt = {e: 0 for e in ENGS}
            ccn = 0
            for e in ENGS:
                for o in self.ops[e]:
                    if o.kind == "d":
                        o.signal = True
                        o.tok = (dsem[e][o.dma_n % KDMA], 16 * (o.dma_n // KDMA + 1), 16)
                    elif o.kind == "cc":
                        ccn += 1
                        o.signal = True
                        o.tok = (ccsem, ccn, 1)
                    elif o.signal:
                        cnt[e] += 1
                        o.tok = (csem[e], cnt[e], 1)
            block = es.enter_context(nc.Block())
            prog = self

            def body(ename):
                def f(eng):
                    waited = {}
                    ops = prog.ops[ename]
                    for o in ops:
                        ws = []
                        for d in o.deps:
                            ws.append((d.tok[0], d.tok[1]))
                        if o.kind == "d" and o.dma_n >= KDMA:
                            ws.append((dsem[ename][o.dma_n % KDMA], 16 * (o.dma_n // KDMA)))
                        for (s, v) in ws:
                            k = s.num
                            if waited.get(k, 0) < v:
                                eng.wait_ge(s, v)
                                waited[k] = v
                        ins = o.fn(eng)
                        if o.signal:
                            ins.then_inc(o.tok[0], o.tok[2])
                    if ename in dsem:
                        n = prog.ndma[ename]
                        for i in range(KDMA):
                            c = (n - i + KDMA - 1) // KDMA
                            if c > 0 and waited.get(dsem[ename][i].num, 0) < 16 * c:
                                eng.wait_ge(dsem[ename][i], 16 * c)
                return f

            block.tensor(body("pe"))
            block.scalar(body("act"))
            block.vector(body("dve"))
            block.gpsimd(body("pool"))
            block.sync(body("sp"))


class TL:
    __slots__ = ("t", "b")

    def __init__(self, t, name=""):
        self.t = t
        self.b = Buf(name)


class SB:
    BASE = 16512
    TOP = 229344

    def __init__(self, nc):
        self.nc = nc
        self.off = SB.BASE
        self.n = 0

    def tile(self, shape, dt, name="t"):
        per = 1
        for s in shape[1:]:
            per *= s
        nbytes = per * (2 if dt == BF16 else 4)
        nbytes = (nbytes + 31) // 32 * 32
        self.n += 1
        t = self.nc.alloc_sbuf_tensor_at("%s_%d" % (name, self.n), list(shape), dt, offset=self.off)
        self.off += nbytes
        assert self.off <= SB.TOP, ("SBUF overflow", name, self.off)
        return TL(t, name)

    def mark(self):
        return self.off

    def reset(self, m):
        self.off = m


C_ID, C_TRI, C_MASKT, C_BLK, C_B64, C_RDT, C_KDN, C_QDT, C_HM = 0, 128, 256, 384, 640, 768, 1280, 1408, 1536
C_RP = 1540


def host_consts(T):
    nch = T // 128
    ncst = C_RP + nch + 1
    c = np.zeros((128, ncst), np.float32)
    r = np.arange(128)
    c[:, C_ID:C_ID + 128] = np.eye(128, dtype=np.float32)
    c[:, C_TRI:C_TRI + 128] = (r[:, None] <= r[None, :]).astype(np.float32)
    c[:, C_MASKT:C_MASKT + 128] = np.where(r[:, None] <= r[None, :], 0.0, -30000.0)
    hh = r // 32
    for h in range(4):
        c[:, C_HM + h] = (hh == h)
    c[:, C_BLK:C_BLK + 256] = (hh[:, None] == (np.arange(256) // 64)[None, :])
    c[:, C_B64:C_B64 + 128] = ((r // 64)[:, None] == (r // 64)[None, :])
    gam = 1.0 - 2.0 ** (-5.0 - np.arange(4, dtype=np.float64))
    lg = np.log(gam)
    for h in range(4):
        dif = r[None, :] - r[:, None]
        c[:, C_RDT + h * 128:C_RDT + (h + 1) * 128] = np.where(dif >= 0, np.exp(lg[h] * np.maximum(dif, 0)), 0.0)
        c[:, C_KDN + h * 32:C_KDN + (h + 1) * 32] = np.exp(lg[h] * (127 - r))[:, None]
    c[:, C_QDT:C_QDT + 128] = np.exp(lg[hh][:, None] * (r[None, :] + 1.0))
    for n in range(nch + 1):
        c[:, C_RP + n] = np.exp(lg[hh] * 128.0 * n)
    return c


def host_rope(T, pos0):
    out = np.zeros((128, 4, T), np.float32)
    pos = (pos0 + np.arange(T)).astype(np.float32)
    inv = (10000.0 ** (-(np.arange(0, 64, 2, dtype=np.float32) / 64))).astype(np.float32)
    ang = pos[None, :] * inv[:, None]
    out[0:32, 0], out[32:64, 0] = np.cos(ang), np.cos(ang)
    out[0:32, 1], out[32:64, 1] = np.sin(ang), np.sin(ang)
    inv2 = (10000.0 ** (-(np.arange(0, 32, 2, dtype=np.float32) / 32))).astype(np.float32)
    ang2 = pos[None, :] * inv2[:, None]
    c2 = np.concatenate([np.cos(ang2), np.cos(ang2)], 0)
    s2 = np.concatenate([np.sin(ang2), np.sin(ang2)], 0)
    out[:, 2] = np.tile(c2, (4, 1))
    out[:, 3] = np.tile(s2, (4, 1))
    return out


V_AN, V_FN, V_QN, V_KVN, V_ON, V_BG, V_CW, V_CB, V_PER = 0, 8, 16, 19, 21, 29, 30, 96, 118


def host_vec(inp, nl0, nl):
    v = np.zeros((128, nl * V_PER), np.float32)

    def cols(a):
        return np.ascontiguousarray(a.reshape(-1, 128).T)
    for i in range(nl):
        l = nl0 + i
        o = i * V_PER
        v[:, o + V_AN:o + V_AN + 8] = cols(inp["attn_norm"][l])
        v[:, o + V_FN:o + V_FN + 8] = cols(inp["ffn_norm"][l])
        v[:, o + V_QN:o + V_QN + 3] = cols(inp["mla_q_norm"][l])
        v[:, o + V_KVN:o + V_KVN + 2] = cols(inp["mla_kv_norm"][l])
        v[:, o + V_ON:o + V_ON + 4] = cols(inp["mla_out_norm"][l])
        v[:, o + V_ON + 4:o + V_ON + 6] = cols(inp["gla_out_norm"][l])
        v[:, o + V_ON + 6:o + V_ON + 8] = cols(inp["ret_out_norm"][l])
        v[:, o + V_BG:o + V_BG + 1] = cols(inp["gla_b_gate"][l])
        for k in range(3):
            v[:, o + V_CW + k * NFC:o + V_CW + (k + 1) * NFC] = cols(inp["ffn_conv_w"][l, k])
        v[:, o + V_CB:o + V_CB + NFC] = cols(inp["ffn_conv_b"][l])
    return v


class Ctx:
    pass


def r3(ap, **kw):
    return ap.rearrange("p (a b) -> p a b", **kw)


def build(T, nl, final_norm=True, dbg=None, ext=(), stop_after=None, stop_layer=0):
    assert T % 512 == 0
    NT = T // 512
    NCH = T // 128
    nc = bass.Bass("TRN2", target_bir_lowering=False)
    p = Prog(nc)
    sb = SB(nc)
    c = Ctx()
    c.nc, c.p, c.sb, c.T, c.NT, c.NCH = nc, p, sb, T, NT, NCH
    c.stop = None

    def din(name, shape, dt=F32):
        return nc.dram_tensor(name, list(shape), dt, kind="ExternalInput")

    def dscr(name, shape, dt):
        if name in ext:
            return nc.dram_tensor(name, list(shape), dt, kind="ExternalOutput")
        return nc.dram_tensor(name, list(shape), dt)

    c.x = din("x", [T, D])
    c.w_in = din("w_in", [nl, D, DIN])
    c.w_uq = din("w_uq", [nl, 384, 768])
    c.w_ukv = din("w_ukv", [nl, 256, 1024])
    c.w_out = din("w_out", [nl, D, D])
    c.w_up = din("w_up", [nl, D, 2 * DFF])
    c.w_down = din("w_down", [nl, DFF, D])
    c.w_gate = din("w_gate", [nl, 16, 128])
    c.vec = din("vec", [128, nl * V_PER])
    ncst = C_RP + NCH + 1
    c.cst = din("cst", [128, ncst])
    c.rope = din("rope", [128, 4, T])
    c.flag = din("flag", [128, 1])
    c.fnb = din("fnb", [128, D])
    c.out = nc.dram_tensor("out", [T, D], F32, kind="ExternalOutput")
    c.xres = dscr("xres", [T, D], F32)
    c.dT = dscr("dT", [128, 11, T], BF16)
    c.logaT = dscr("logaT", [128, T], F32)
    c.dN = dscr("dN", [T, 512], BF16)
    c.xb1 = dscr("xb1", [128, 2 * T], BF16)
    c.xg1 = dscr("xg1", [256, 2 * T], BF16)
    c.xb2 = dscr("xb2", [128, T], BF16)
    c.xg2 = dscr("xg2", [256, T], BF16)
    c.oint = dscr("oint", [2, 128, 2, T], F32)
    c.qt = dscr("qt", [2, 128, T], BF16)
    c.sx = dscr("sx", [128, 512], F32)
    c.sgat = dscr("sgat", [256, 512], F32)
    c.yT = dscr("yT", [128, 8, T], BF16)
    c.hT = dscr("hT", [128, 8, T], BF16)
    c.halo = dscr("halo", [128, 2 * NFC], F32)
    c.hgat = dscr("hgat", [256, 2 * NFC], F32)
    c.gT = dscr("gT", [128, NFC, T], BF16)
    c.dbg = None
    if dbg is not None:
        c.dbg = nc.dram_tensor("dbg", list(dbg), F32, kind="ExternalOutput")

    c.ps = [TL(nc.alloc_psum_tensor("ps%d" % i, [128, 512], F32), "ps%d" % i) for i in range(7)]
    _sv = nc.psum_base
    c.psb = TL(nc.alloc_psum_tensor("psb", [128, 1024], BF16), "psb")
    nc.psum_base = _sv
    _ps7 = TL(nc.alloc_psum_tensor("ps7", [128, 512], F32), "ps7")
    _ps7.b = c.psb.b
    c.ps.append(_ps7)
    c.psi = 0

    def nps():
        t = c.ps[c.psi % 7]
        c.psi += 1
        return t
    c.nps = nps

    c.cstf = sb.tile([128, ncst], F32, "cstf")
    p.dma(c.cstf.t[:], c.cst[:, :], writes=[c.cstf.b])
    c.vecs = sb.tile([128, nl * V_PER], F32, "vecs")
    p.dma(c.vecs.t[:], c.vec[:, :], writes=[c.vecs.b])
    c.flg = sb.tile([128, 1], F32, "flag")
    p.dma(c.flg.t[:], c.flag[:, :], writes=[c.flg.b])
    c.cb = sb.tile([128, 1540], BF16, "cstb")
    p.cp("dve", c.cb.t[:], c.cstf.t[:, 0:1540], reads=[c.cstf.b], writes=[c.cb.b])
    c.ones = sb.tile([128, 128], BF16, "ones")
    p.memset("pool", c.ones.t[:], 1.0, writes=[c.ones.b])
    c.fones = sb.tile([128, 128], BF16, "fones")
    p.ts("dve", c.fones.t[:], c.ones.t[:], c.flg.t[:, 0:1], None, ALU.mult, reads=[c.ones.b, c.flg.b], writes=[c.fones.b])
    c.tri4 = sb.tile([128, 512], F32, "tri4")
    for h in range(4):
        p.cp("pool", c.tri4.t[:, h * 128:(h + 1) * 128], c.cstf.t[:, C_TRI:C_TRI + 128], reads=[c.cstf.b], writes=[c.tri4.b])
    c.zero = sb.tile([128, 256], F32, "zero")
    p.memset("pool", c.zero.t[:], 0.0, writes=[c.zero.b])
    zb = sb.tile([128, 512], BF16, "zb")
    p.memset("pool", zb.t[:], 0.0, writes=[zb.b])
    for a0 in range(0, T, 512):
        p.dma(c.xb2[64:128, a0:a0 + 512], zb.t[64:128, :], reads=[zb.b])
    c.persist_mark = sb.mark()

    xin = c.x
    for li in range(nl):
        c.li = li
        c.vo = li * V_PER
        if li == stop_layer:
            c.stop = stop_after
        stop_after_ = stop_after if li == stop_layer else None
        phase_A(c, xin)
        if stop_after_ == "A":
            break
        exchange(c, c.xb1, c.xg1)
        exchange(c, c.xb2, c.xg2)
        if stop_after_ == "X":
            break
        phase_B(c)
        if stop_after_ in ("B", "B1", "B2", "B2x"):
            break
        phase_C(c)
        if stop_after_ == "C":
            break
        phase_D(c, xin, last=(li == nl - 1) and final_norm)
        xin = c.xres
    if not final_norm:
        pass
    p.barrier()
    p.emit()
    return nc


def exchange(c, src, dst):
    p = c.p
    p.barrier()
    sap, dap = src.ap().opt(), dst.ap().opt()
    p.op("pool", lambda e: e.collective_compute("AllGather", ALU.bypass, replica_groups=[[0, 1], [2, 3], [4, 5], [6, 7]],
                                                ins=[sap], outs=[dap]), kind="cc")
    p.barrier()


def rstd_from_ss(c, out_ap, ss_ap, n, reads, writes):
    p = c.p
    p.act(out_ap, ss_ap, AF.Sqrt, reads=reads, writes=writes, bias=EPS, scale=1.0 / n)
    p.op("dve", lambda e: e.reciprocal(out_ap, out_ap), reads=writes, writes=writes)


class Alt:
    def __init__(self, engs):
        self.engs = engs
        self.i = 0

    def __call__(self):
        e = self.engs[self.i % len(self.engs)]
        self.i += 1
        return e


def scaled_copy(c, eng, out, in_, scal, reads, writes):
    p = c.p
    if eng == "act":
        p.act(out, in_, AF.Copy, reads=reads, writes=writes, scale=scal)
    else:
        p.ts(eng, out, in_, scal, None, ALU.mult, reads=reads, writes=writes)


WX = 2576
S32 = 32 ** -0.5


def phase_A(c, xin):
    nc, p, sb, T, NT = c.nc, c.p, c.sb, c.T, c.NT
    li, vo = c.li, c.vo
    p.barrier()
    sb.reset(c.persist_mark)
    vec = c.vecs
    gv = sb.tile([128, 4, 8], F32, "gv")
    g0 = vec.t[:, vo + V_AN:vo + V_AN + 8]
    p.cp("dve", gv.t[:, 0, :], g0, reads=[vec.b], writes=[gv.b])
    p.ts("dve", gv.t[:, 1, :], g0, -1.0, None, ALU.mult, reads=[vec.b], writes=[gv.b])
    p.ts("dve", gv.t[:, 2, :], g0, S32, None, ALU.mult, reads=[vec.b], writes=[gv.b])
    p.ts("dve", gv.t[:, 3, :], g0, -S32, None, ALU.mult, reads=[vec.b], writes=[gv.b])
    nbg = sb.tile([128, 1], F32, "nbg")
    p.ts("dve", nbg.t[:], vec.t[:, vo + V_BG:vo + V_BG + 1], -1.0, None, ALU.mult, reads=[vec.b], writes=[nbg.b])
    W = sb.tile([128, 8, WX], BF16, "winx")
    stg = [sb.tile([128, DIN], F32, "wstg") for _ in range(2)]
    alt = Alt(["act", "dve"])
    for k in range(8):
        st = stg[k % 2]
        p.dma(st.t[:], c.w_in[li, k * 128:(k + 1) * 128, :], writes=[st.b])
        g, gn, gs, gsn = (gv.t[:, j, k:k + 1] for j in range(4))

        def cv(d0, d1, s0, s1, sc):
            scaled_copy(c, alt(), W.t[:, k, d0:d1], st.t[:, s0:s1], sc, [st.b, gv.b], [W.b])

        def cvrot(d0, s0, nh, half, sp, sn):
            dv = W.t[:, k, d0:d0 + nh * 2 * half].rearrange("p (h two d) -> p h two d", h=nh, two=2)
            sv = st.t[:, s0:s0 + nh * 2 * half].rearrange("p (h two d) -> p h two d", h=nh, two=2)
            scaled_copy(c, alt(), dv[:, :, 0, :], sv[:, :, 1, :], sn, [st.b, gv.b], [W.b])
            scaled_copy(c, alt(), dv[:, :, 1, :], sv[:, :, 0, :], sp, [st.b, gv.b], [W.b])
        cv(0, 704, 0, 704, g)
        cvrot(704, 640, 1, 32, g, gn)
        cv(768, 896, 704, 832, gs)
        cv(896, 1024, 832, 960, g)
        cv(1024, 1040, 1216, 1232, g)
        cv(1040, 1296, 1232, 1488, g)
        cv(1296, 1424, 1488, 1616, g)
        cvrot(1424, 1488, 4, 16, g, gn)
        cv(1552, 1680, 1616, 1744, gs)
        cvrot(1680, 1616, 4, 16, gs, gsn)
        cv(1808, 2064, 2000, 2256, g)
        cv(2064, 2320, 960, 1216, g)
        cv(2320, 2576, 1744, 2000, g)
    wgf = sb.tile([16, 128], F32, "wgf")
    p.dma(wgf.t[:], c.w_gate[li, :, :], writes=[wgf.b])
    wg = sb.tile([16, 128], BF16, "wg")
    p.cp("dve", wg.t[:], wgf.t[:], reads=[wgf.b], writes=[wg.b])

    xt = [sb.tile([128, D], F32, "xt") for _ in range(2)]
    junk = sb.tile([128, D], BF16, "junk")
    ssq = [sb.tile([128, 2], F32, "ssq") for _ in range(2)]
    hN = [sb.tile([128, D], BF16, "hN") for _ in range(2)]
    hT = [sb.tile([128, 8, 512], BF16, "hT") for _ in range(2)]
    zf = [sb.tile([128, 3, 512], F32, "zf") for _ in range(2)]
    sq = [sb.tile([128, 3, 512], BF16, "sq") for _ in range(2)]
    rs = [sb.tile([128, 512], F32, "rs") for _ in range(2)]
    zn = [sb.tile([128, 3, 512], BF16, "zn") for _ in range(2)]
    rp = sb.tile([128, 4, 512], F32, "ropeA")
    tA = [sb.tile([128, 512], F32, "tA") for _ in range(2)]
    tB = [sb.tile([128, 512], F32, "tB") for _ in range(2)]
    ob = [sb.tile([128, 512], BF16, "obA") for _ in range(4)]
    glr = sb.tile([16, 512], BF16, "glr")
    ex = sb.tile([128, 512], F32, "exA")
    la = [sb.tile([128, 512], F32, "laA") for _ in range(2)]
    vN = [sb.tile([128, 512], BF16, "vN") for _ in range(2)]
    cnt = {"ob": 0, "sub": 0, "g": 0}
    idb = c.cb.t[:, C_ID:C_ID + 128]

    def nob():
        t = ob[cnt["ob"] % 4]
        cnt["ob"] += 1
        return t

    def proj(c0, M, hTt):
        ps = c.nps()
        for k in range(8):
            p.mm(ps.t[0:M, :], W.t[:, k, c0:c0 + M], hTt.t[:, k, :], k == 0, k == 7, reads=[W.b, hTt.b], writes=[ps.b])
        return ps

    def load_x(i):
        if i < 4 * NT:
            p.dma(xt[i % 2].t[:], xin[i * 128:(i + 1) * 128, :], writes=[xt[i % 2].b])
    load_x(0)
    for tt in range(NT):
        t0 = tt * 512
        hTt = hT[tt % 2]
        p.dma(rp.t[:], c.rope[:, :, t0:t0 + 512], writes=[rp.b])
        for s in range(4):
            i = cnt["sub"]
            cnt["sub"] += 1
            x_, ss_, hN_ = xt[i % 2], ssq[i % 2], hN[i % 2]
            load_x(i + 1)
            p.act(junk.t[:], x_.t[:], AF.Square, reads=[x_.b], writes=[junk.b, ss_.b], accum=ss_.t[:, 0:1])
            rstd_from_ss(c, ss_.t[:, 1:2], ss_.t[:, 0:1], D, [ss_.b], [ss_.b])
            p.act(hN_.t[:], x_.t[:], AF.Copy, reads=[x_.b, ss_.b], writes=[hN_.b], scale=ss_.t[:, 1:2])
            for k in range(8):
                p.tr(c.psb.t[:, k * 128:(k + 1) * 128], hN_.t[:, k * 128:(k + 1) * 128], idb, reads=[hN_.b, c.cb.b], writes=[c.psb.b])
            p.cp("dve", hTt.t[:, :, s * 128:(s + 1) * 128], r3(c.psb.t[:], a=8), reads=[c.psb.b], writes=[hTt.b])

        for (c0, nchk, n, which) in ((0, 3, 384, "cq"), (384, 2, 256, "ckv")):
            gi = cnt["g"]
            cnt["g"] += 1
            zf_, sq_, rs_, zn_ = zf[gi % 2], sq[gi % 2], rs[gi % 2], zn[gi % 2]
            for j in range(nchk):
                ps = proj(c0 + j * 128, 128, hTt)
                p.cp("act", zf_.t[:, j, :], ps.t[:], reads=[ps.b], writes=[zf_.b])
                p.tt("pool", sq_.t[:, j, :], zf_.t[:, j, :], zf_.t[:, j, :], ALU.mult, reads=[zf_.b], writes=[sq_.b])
            pss = c.nps()
            for j in range(nchk):
                p.mm(pss.t[:], c.ones.t[:], sq_.t[:, j, :], j == 0, j == nchk - 1, reads=[c.ones.b, sq_.b], writes=[pss.b])
            rstd_from_ss(c, rs_.t[:], pss.t[:], n, [pss.b], [rs_.b])
            for j in range(nchk):
                p.tt("dve", zn_.t[:, j, :], zf_.t[:, j, :], rs_.t[:], ALU.mult, reads=[zf_.b, rs_.b], writes=[zn_.b])
            if which == "cq":
                p.dma(c.dT[:, 0:3, t0:t0 + 512], zn_.t[:, 0:3, :], reads=[zn_.b])
            else:
                p.dma(r3(c.xb1[:, :], a=2)[:, :, t0:t0 + 512], zn_.t[:, 0:2, :], reads=[zn_.b])

        def roped(c_raw, c_rot, M, ci, si, dst_ap):
            pr_, pt_ = proj(c_raw, M, hTt), proj(c_rot, M, hTt)
            a_, b_ = tA[cnt["g"] % 2], tB[cnt["g"] % 2]
            cnt["g"] += 1
            p.tt("dve", a_.t[0:M, :], pr_.t[0:M, :], rp.t[0:M, ci, :], ALU.mult, reads=[pr_.b, rp.b], writes=[a_.b])
            p.tt("dve", b_.t[0:M, :], pt_.t[0:M, :], rp.t[0:M, si, :], ALU.mult, reads=[pt_.b, rp.b], writes=[b_.b])
            o_ = nob()
            p.tt("pool", o_.t[0:M, :], a_.t[0:M, :], b_.t[0:M, :], ALU.add, reads=[a_.b, b_.b], writes=[o_.b])
            p.dma(dst_ap, o_.t[0:M, :], reads=[o_.b])

        roped(640, 704, 64, 0, 1, c.xb2[0:64, t0:t0 + 512])
        roped(1296, 1424, 128, 2, 3, c.dT[:, 5, t0:t0 + 512])
        roped(1552, 1680, 128, 2, 3, c.dT[:, 6, t0:t0 + 512])

        for (c0, slot) in ((768, 3), (896, 4)):
            ps = proj(c0, 128, hTt)
            o_ = nob()
            p.cp("act", o_.t[:], ps.t[:], reads=[ps.b], writes=[o_.b])
            p.dma(c.dT[:, slot, t0:t0 + 512], o_.t[:], reads=[o_.b])
        for (c0, slot) in ((1040, 7), (1168, 8), (1808, 9), (1936, 10)):
            ps = proj(c0, 128, hTt)
            o_ = nob()
            p.act(o_.t[:], ps.t[:], AF.Silu, reads=[ps.b], writes=[o_.b])
            p.dma(c.dT[:, slot, t0:t0 + 512], o_.t[:], reads=[o_.b])
        ps = proj(1024, 16, hTt)
        p.cp("act", glr.t[:], ps.t[0:16, :], reads=[ps.b], writes=[glr.b])
        ps2 = c.nps()
        p.mm(ps2.t[:], wg.t[:], glr.t[:], True, True, reads=[wg.b, glr.b], writes=[ps2.b])
        p.act(ex.t[:], ps2.t[:], AF.Exp, reads=[ps2.b, nbg.b], writes=[ex.b], bias=nbg.t[:, 0:1], scale=-1.0)
        la_ = la[tt % 2]
        p.act(la_.t[:], ex.t[:], AF.Ln, reads=[ex.b], writes=[la_.b], bias=1.0)
        p.ts("pool", la_.t[:], la_.t[:], -1.0 / 16.0, None, ALU.mult, reads=[la_.b], writes=[la_.b])
        p.dma(c.logaT[:, t0:t0 + 512], la_.t[:], reads=[la_.b])
        for s in range(4):
            ps = c.nps()
            for k in range(8):
                p.mm(ps.t[:], hTt.t[:, k, s * 128:(s + 1) * 128], W.t[:, k, 2064:2576], k == 0, k == 7, reads=[W.b, hTt.b], writes=[ps.b])
            v_ = vN[s % 2]
            p.cp("act" if s % 2 else "dve", v_.t[:], ps.t[:], reads=[ps.b], writes=[v_.b])
            p.dma(c.dN[t0 + s * 128:t0 + (s + 1) * 128, :], v_.t[:], reads=[v_.b])


def phase_B(c):
    nc, p, sb, T, NT, NCH = c.nc, c.p, c.sb, c.T, c.NT, c.NCH
    p.barrier()
    sb.reset(c.persist_mark)
    cf, cbf = c.cstf, c.cb
    idb = cbf.t[:, C_ID:C_ID + 128]
    idf = cf.t[:, C_ID:C_ID + 128]
    trif = cf.t[:, C_TRI:C_TRI + 128]
    blk = cf.t[:, C_BLK:C_BLK + 256]
    U = [sb.tile([128, NCH, 256], F32, "Uall%d" % m) for m in range(2)]
    Ub = [[Buf() for _ in range(NCH)] for m in range(2)]
    dall = sb.tile([128, NCH], F32, "dall")
    Pp = sb.tile([128, NCH], F32, "Pp")
    vpad = [sb.tile([128, 4, 128], BF16, "vpad%d" % m) for m in range(2)]
    for m in range(2):
        p.memset("pool", vpad[m].t[:], 0.0, writes=[vpad[m].b])
    inT = [sb.tile([128, 4, 128], BF16, "inT") for _ in range(2)]
    la = [sb.tile([128, 128], F32, "laB") for _ in range(2)]
    vN = [sb.tile([128, 512], BF16, "vNB") for _ in range(2)]
    laN = sb.tile([128, 128], F32, "laN")
    eneg = sb.tile([128, 128], F32, "eneg")
    epos = sb.tile([128, 128], F32, "epos")
    kt = sb.tile([128, 128], BF16, "kt")
    qtl = [sb.tile([128, 128], BF16, "qtl") for _ in range(2)]
    qtm = sb.tile([128, 4, 128], BF16, "qtm")
    ktN = sb.tile([128, 128], BF16, "ktN")
    Am = [sb.tile([128, 512], BF16, "Am%d" % m) for m in range(2)]
    oev = [sb.tile([128, 2, 128], F32, "oev") for _ in range(4)]
    rqm = sb.tile([128, 4, 128], BF16, "rqm")
    qdec = [sb.tile([128, 128], BF16, "qdec") for _ in range(2)]
    kdN = sb.tile([128, 128], BF16, "kdN")
    stmp = sb.tile([128, 256], F32, "stmp")
    mB1 = sb.mark()

    def load_B(n):
        if n < NCH:
            tk = n * 128
            p.dma(inT[n % 2].t[:], c.dT[:, 3:7, tk:tk + 128], writes=[inT[n % 2].b])
            p.dma(la[n % 2].t[:], c.logaT[:, tk:tk + 128], writes=[la[n % 2].b])
            p.dma(vN[n % 2].t[:], c.dN[tk:tk + 128, :], writes=[vN[n % 2].b])
    load_B(0)
    for n in range(NCH):
        tok = n * 128
        inT_, la_, vN_ = inT[n % 2], la[n % 2], vN[n % 2]
        load_B(n + 1)
        psl = c.nps()
        p.tr(psl.t[:, 0:128], la_.t[:], idf, reads=[la_.b, cf.b], writes=[psl.b])
        p.cp("act", laN.t[:], psl.t[:, 0:128], reads=[psl.b], writes=[laN.b])
        psB = c.nps()
        p.mm(psB.t[:, 0:128], laN.t[:], trif, True, True, reads=[laN.b, cf.b], writes=[psB.b])
        p.act(eneg.t[:], psB.t[:, 0:128], AF.Exp, reads=[psB.b], writes=[eneg.b], scale=-1.0)
        p.act(epos.t[:], psB.t[:, 0:128], AF.Exp, reads=[psB.b], writes=[epos.b])
        p.cp("dve", dall.t[:, n:n + 1], epos.t[:, 127:128], reads=[epos.b], writes=[dall.b])
        p.tt("dve", kt.t[:], inT_.t[:, 1, :], eneg.t[:], ALU.mult, reads=[inT_.b, eneg.b], writes=[kt.b])
        q_ = qtl[n % 2]
        p.tt("pool", q_.t[:], inT_.t[:, 0, :], epos.t[:], ALU.mult, reads=[inT_.b, epos.b], writes=[q_.b])
        p.dma(c.qt[0, :, tok:tok + 128], q_.t[:], reads=[q_.b])
        for h in range(4):
            p.stt("dve" if h % 2 == 0 else "pool", qtm.t[:, h, :], inT_.t[:, 0, :], cf.t[:, C_HM + h:C_HM + h + 1], epos.t[:],
                  ALU.mult, ALU.mult, reads=[inT_.b, epos.b, cf.b], writes=[qtm.b])
        p.tr(c.psb.t[:, 0:128], kt.t[:], idb, reads=[kt.b, cbf.b], writes=[c.psb.b])
        p.cp("act", ktN.t[:], c.psb.t[:, 0:128], reads=[c.psb.b], writes=[ktN.b])
        psU = c.nps()
        p.mm(psU.t[:, 0:256], ktN.t[:], vN_.t[:, 0:256], True, True, reads=[ktN.b, vN_.b], writes=[psU.b])
        p.stt("dve", U[0].t[:, n, :], psU.t[:, 0:256], dall.t[:, n:n + 1], blk, ALU.mult, ALU.mult,
              reads=[psU.b, dall.b, cf.b], writes=[Ub[0][n]])
        psA = c.nps()
        for h in range(4):
            p.mm(psA.t[:, h * 128:(h + 1) * 128], kt.t[:], qtm.t[:, h, :], True, True, reads=[kt.b, qtm.b], writes=[psA.b])
        p.tt("dve", Am[0].t[:], psA.t[:], c.tri4.t[:], ALU.mult, reads=[psA.b, c.tri4.b], writes=[Am[0].b])
        for h in range(4):
            p.ts("pool" if h % 2 == 0 else "dve", rqm.t[:, h, :], inT_.t[:, 2, :], cf.t[:, C_HM + h:C_HM + h + 1], None, ALU.mult,
                 reads=[inT_.b, cf.b], writes=[rqm.b])
        qd_ = qdec[n % 2]
        p.tt("pool", qd_.t[:], inT_.t[:, 2, :], cf.t[:, C_QDT:C_QDT + 128], ALU.mult, reads=[inT_.b, cf.b], writes=[qd_.b])
        p.dma(c.qt[1, :, tok:tok + 128], qd_.t[:], reads=[qd_.b])
        p.tr(c.psb.t[:, 128:256], inT_.t[:, 3, :], idb, reads=[inT_.b, cbf.b], writes=[c.psb.b])
        p.tt("dve", kdN.t[:], c.psb.t[:, 128:256], cf.t[:, C_KDN:C_KDN + 128], ALU.mult, reads=[c.psb.b, cf.b], writes=[kdN.b])
        psU2 = c.nps()
        p.mm(psU2.t[:, 0:256], kdN.t[:], vN_.t[:, 256:512], True, True, reads=[kdN.b, vN_.b], writes=[psU2.b])
        p.tt("dve", U[1].t[:, n, :], psU2.t[:, 0:256], blk, ALU.mult, reads=[psU2.b, cf.b], writes=[Ub[1][n]])
        psA2 = c.nps()
        for h in range(4):
            p.mm(psA2.t[:, h * 128:(h + 1) * 128], inT_.t[:, 3, :], rqm.t[:, h, :], True, True, reads=[inT_.b, rqm.b], writes=[psA2.b])
        p.tt("dve", Am[1].t[:], psA2.t[:], cf.t[:, C_RDT:C_RDT + 512], ALU.mult, reads=[psA2.b, cf.b], writes=[Am[1].b])
        for m in range(2):
            vp = vpad[m]
            src = vN_.t[:, m * 256:(m + 1) * 256].rearrange("p (a two d) -> p a two d", a=2, two=2)
            dst = vp.t[:].rearrange("p (a two) d -> p a two d", two=2)
            p.cp("pool", dst[:, :, 0, 0:64], src[:, :, 0, :], reads=[vN_.b], writes=[vp.b])
            p.cp("pool", dst[:, :, 1, 64:128], src[:, :, 1, :], reads=[vN_.b], writes=[vp.b])
            psO = c.nps()
            for pr in range(2):
                for hh in range(2):
                    h = 2 * pr + hh
                    p.mm(psO.t[:, pr * 128:(pr + 1) * 128], vp.t[:, h, :], Am[m].t[:, h * 128:(h + 1) * 128], hh == 0, hh == 1,
                         reads=[vp.b, Am[m].b], writes=[psO.b])
            o_ = oev[(2 * n + m) % 4]
            p.cp("act", o_.t[:], r3(psO.t[:, 0:256], a=2), reads=[psO.b], writes=[o_.b])
            p.dma(c.oint[m, :, :, tok:tok + 128], o_.t[:], reads=[o_.b])

    if c.stop == "B1":
        return
    p.memset("dve", Pp.t[:, 0:1], 1.0, writes=[Pp.b])
    for n in range(1, NCH):
        p.stt("dve", U[0].t[:, n, :], U[0].t[:, n - 1, :], dall.t[:, n:n + 1], U[0].t[:, n, :], ALU.mult, ALU.add,
              reads=[Ub[0][n - 1], dall.b], writes=[Ub[0][n]])
        p.stt("dve", U[1].t[:, n, :], U[1].t[:, n - 1, :], cf.t[:, C_RP + 1:C_RP + 2], U[1].t[:, n, :], ALU.mult, ALU.add,
              reads=[Ub[1][n - 1], cf.b], writes=[Ub[1][n]])
        p.tt("dve", Pp.t[:, n:n + 1], Pp.t[:, n - 1:n], dall.t[:, n - 1:n], ALU.mult, reads=[dall.b], writes=[Pp.b])
    p.dma(c.sx[:, 0:256], U[0].t[:, NCH - 1, :], reads=[Ub[0][NCH - 1]])
    p.dma(c.sx[:, 256:512], U[1].t[:, NCH - 1, :], reads=[Ub[1][NCH - 1]])
    if c.stop == "B2":
        return
    exchange(c, c.sx, c.sgat)
    if c.stop == "B2x":
        return
    sb.reset(mB1)
    sin = sb.tile([128, 512], F32, "sin")
    p.dma(sin.t[:], c.sgat[0:128, :], writes=[sin.b])
    sinf = sb.tile([128, 512], F32, "sinf")
    p.ts("dve", sinf.t[:], sin.t[:], c.flg.t[:, 0:1], None, ALU.mult, reads=[sin.b, c.flg.b], writes=[sinf.b])
    qT = [sb.tile([128, 512], BF16, "qTB") for _ in range(2)]
    oi = [sb.tile([128, 2, 512], F32, "oiB") for _ in range(2)]
    gt = [sb.tile([128, 2, 512], BF16, "gtB") for _ in range(2)]
    Sp = [sb.tile([128, 256], BF16, "Sp") for _ in range(4)]
    of = [sb.tile([128, 512], F32, "ofB") for _ in range(2)]
    sqo = [sb.tile([128, 512], BF16, "sqoB") for _ in range(2)]
    rs = [sb.tile([128, 512], F32, "rsB") for _ in range(2)]
    of2 = [sb.tile([128, 512], F32, "of2B") for _ in range(2)]
    yo = [sb.tile([128, 512], BF16, "yoB") for _ in range(2)]
    k = 0
    kk = 0
    def load_B3(kx):
        if kx < 2 * NT:
            tt_, m_ = kx // 2, kx % 2
            a0 = tt_ * 512
            p.dma(qT[kx % 2].t[:], c.qt[m_, :, a0:a0 + 512], writes=[qT[kx % 2].b])
            p.dma(oi[kx % 2].t[:], c.oint[m_, :, :, a0:a0 + 512], writes=[oi[kx % 2].b])
            p.dma(gt[kx % 2].t[:], c.dT[:, 7 + 2 * m_:9 + 2 * m_, a0:a0 + 512], writes=[gt[kx % 2].b])
    load_B3(0)
    for tt in range(NT):
        t0 = tt * 512
        for m in range(2):
            qT_, oi_, gt_ = qT[k % 2], oi[k % 2], gt[k % 2]
            k += 1
            load_B3(k)
            psI = [c.nps(), c.nps()]
            for cch in range(4):
                n = 4 * tt + cch
                Sp_ = Sp[(4 * k + cch) % 4]
                if m == 0:
                    psc, rd = Pp.t[:, n:n + 1], [Pp.b]
                else:
                    psc, rd = cf.t[:, C_RP + n:C_RP + n + 1], [cf.b]
                if n > 0:
                    prev, rd2 = U[m].t[:, n - 1, :], [Ub[m][n - 1]]
                else:
                    prev, rd2 = c.zero.t[:], [c.zero.b]
                p.stt("dve" if cch % 2 == 0 else "pool", Sp_.t[:], sinf.t[:, m * 256:(m + 1) * 256], psc, prev, ALU.mult, ALU.add,
                      reads=[sinf.b] + rd + rd2, writes=[Sp_.b])
                for pr in range(2):
                    p.mm(psI[pr].t[:, cch * 128:(cch + 1) * 128], Sp_.t[:, pr * 128:(pr + 1) * 128], qT_.t[:, cch * 128:(cch + 1) * 128],
                         True, True, reads=[Sp_.b, qT_.b], writes=[psI[pr].b])
            for pr in range(2):
                of_, sq_, rs_, of2_, yo_ = of[kk % 2], sqo[kk % 2], rs[kk % 2], of2[kk % 2], yo[kk % 2]
                kk += 1
                p.tt("dve", of_.t[:], psI[pr].t[:], oi_.t[:, pr, :], ALU.add, reads=[psI[pr].b, oi_.b], writes=[of_.b])
                p.act(sq_.t[:], of_.t[:], AF.Square, reads=[of_.b], writes=[sq_.b])
                pss = c.nps()
                p.mm(pss.t[:], cbf.t[:, C_B64:C_B64 + 128], sq_.t[:], True, True, reads=[cbf.b, sq_.b], writes=[pss.b])
                rstd_from_ss(c, rs_.t[:], pss.t[:], 64, [pss.b], [rs_.b])
                p.tt("dve", of2_.t[:], of_.t[:], rs_.t[:], ALU.mult, reads=[of_.b, rs_.b], writes=[of2_.b])
                p.tt("pool", yo_.t[:], of2_.t[:], gt_.t[:, pr, :], ALU.mult, reads=[of2_.b, gt_.b], writes=[yo_.b])
                p.dma(c.yT[:, 4 + 2 * m + pr, t0:t0 + 512], yo_.t[:], reads=[yo_.b])


def phase_C(c):
    nc, p, sb, T, NT, NCH = c.nc, c.p, c.sb, c.T, c.NT, c.NCH
    li, vo = c.li, c.vo
    p.barrier()
    sb.reset(c.persist_mark)
    vec, cf, cbf = c.vecs, c.cstf, c.cb
    idb = cbf.t[:, C_ID:C_ID + 128]
    maskb = cbf.t[:, C_MASKT:C_MASKT + 128]
    NKT = 2 * T // 512
    NKB = 2 * T // 128
    gq = sb.tile([128, 2, 3], F32, "gqC")
    p.ts("dve", gq.t[:, 0, :], vec.t[:, vo + V_QN:vo + V_QN + 3], SCALE_MLA, None, ALU.mult, reads=[vec.b], writes=[gq.b])
    p.ts("dve", gq.t[:, 1, :], vec.t[:, vo + V_QN:vo + V_QN + 3], -SCALE_MLA, None, ALU.mult, reads=[vec.b], writes=[gq.b])
    wq = sb.tile([128, 3, 1024], BF16, "wq")
    wkv = sb.tile([128, 2, 1024], BF16, "wkv")
    stg = [sb.tile([128, 1024], F32, "stgC") for _ in range(2)]
    alt = Alt(["act", "dve"])
    for k in range(3):
        st = stg[k % 2]
        p.dma(st.t[:, 0:768], c.w_uq[li, k * 128:(k + 1) * 128, :], writes=[st.b])
        scaled_copy(c, alt(), wq.t[:, k, 0:768], st.t[:, 0:768], gq.t[:, 0, k:k + 1], [st.b, gq.b], [wq.b])
        for h in range(4):
            s0 = h * 192 + 128
            scaled_copy(c, alt(), wq.t[:, k, 768 + h * 64:768 + h * 64 + 32], st.t[:, s0 + 32:s0 + 64], gq.t[:, 1, k:k + 1], [st.b, gq.b], [wq.b])
            scaled_copy(c, alt(), wq.t[:, k, 768 + h * 64 + 32:768 + h * 64 + 64], st.t[:, s0:s0 + 32], gq.t[:, 0, k:k + 1], [st.b, gq.b], [wq.b])
    for k in range(2):
        st = stg[(k + 1) % 2]
        p.dma(st.t[:], c.w_ukv[li, k * 128:(k + 1) * 128, :], writes=[st.b])
        scaled_copy(c, alt(), wkv.t[:, k, :], st.t[:], vec.t[:, vo + V_KVN + k:vo + V_KVN + k + 1], [st.b, vec.b], [wkv.b])
    cq = sb.tile([128, 3, T], BF16, "cqC")
    ckv = sb.tile([128, 2, 2 * T], BF16, "ckvC")
    KrT = sb.tile([64, 2 * T], BF16, "KrT")
    PC = 1024 if T % 1024 == 0 else 512
    for a0 in range(0, T, PC):
        p.dma(cq.t[:, :, a0:a0 + PC], c.dT[:, 0:3, a0:a0 + PC], writes=[cq.b])
        p.dma(ckv.t[:, :, a0:a0 + PC], r3(c.xg1[0:128, :], a=2)[:, :, a0:a0 + PC], writes=[ckv.b])
        p.dma(ckv.t[:, :, T + a0:T + a0 + PC], r3(c.xb1[:, :], a=2)[:, :, a0:a0 + PC], writes=[ckv.b])
        p.dma(KrT.t[:, a0:a0 + PC], c.xg2[0:64, a0:a0 + PC], writes=[KrT.b])
        p.dma(KrT.t[:, T + a0:T + a0 + PC], c.xb2[0:64, a0:a0 + PC], writes=[KrT.b])
    KnT = sb.tile([128, 2 * T], BF16, "KnT")
    V = sb.tile([128, NKB, 128], BF16, "V")
    QnT = sb.tile([128, T], BF16, "QnT")
    QrT = sb.tile([64, T], BF16, "QrT")
    sqa = [sb.tile([128, 512], BF16, "sqa") for _ in range(2)]
    rpt = [sb.tile([64, 2, 512], F32, "rptC") for _ in range(2)]
    ta = [sb.tile([64, 512], F32, "taC") for _ in range(2)]
    tb = [sb.tile([64, 512], F32, "tbC") for _ in range(2)]
    mx = sb.tile([128, 4, max(NKT, NT)], F32, "mxC")
    red = sb.tile([128, 8], F32, "redC")
    bias = sb.tile([128, NT], F32, "biasC")
    PT = [sb.tile([128, 512], BF16, "PT") for _ in range(6)]
    rden = [sb.tile([128, 512], F32, "rden") for _ in range(2)]
    uu = [sb.tile([128, 512], F32, "uu") for _ in range(2)]
    squ = [sb.tile([128, 512], BF16, "squ") for _ in range(2)]
    rsu = [sb.tile([128, 512], F32, "rsu") for _ in range(2)]
    yo = [sb.tile([128, 512], BF16, "yoC") for _ in range(2)]
    k_ = {"sq": 0, "pt": 0, "s": 0, "e": 0}

    def nsq():
        t = sqa[k_["sq"] % 2]
        k_["sq"] += 1
        return t

    def rmax(dst_ap, ps, M, rd, wr):
        p.op("dve", lambda e: e.tensor_reduce(dst_ap, ps.t[0:M, :] if M < 128 else ps.t[:], AX.X, ALU.max), reads=rd, writes=wr)

    for kt in range(NKT):
        s_ = nsq()
        p.tt("pool", s_.t[0:64, :], KrT.t[:, kt * 512:(kt + 1) * 512], KrT.t[:, kt * 512:(kt + 1) * 512], ALU.mult, reads=[KrT.b], writes=[s_.b])
        ps = c.nps()
        p.mm(ps.t[:], c.ones.t[0:64, :], s_.t[0:64, :], True, True, reads=[c.ones.b, s_.b], writes=[ps.b])
        rmax(mx.t[:, 1, kt:kt + 1], ps, 128, [ps.b], [mx.b])
    p.op("dve", lambda e: e.tensor_reduce(red.t[:, 1:2], mx.t[:, 1, 0:NKT], AX.X, ALU.max), reads=[mx.b], writes=[red.b])

    for h in range(4):
        for kt in range(NKT):
            ps = c.nps()
            for k in range(2):
                p.mm(ps.t[:], wkv.t[:, k, h * 256:h * 256 + 128], ckv.t[:, k, kt * 512:(kt + 1) * 512], k == 0, k == 1,
                     reads=[wkv.b, ckv.b], writes=[ps.b])
            p.cp("act", KnT.t[:, kt * 512:(kt + 1) * 512], ps.t[:], reads=[ps.b], writes=[KnT.b])
            s_ = nsq()
            p.tt("pool", s_.t[:], KnT.t[:, kt * 512:(kt + 1) * 512], KnT.t[:, kt * 512:(kt + 1) * 512], ALU.mult, reads=[KnT.b], writes=[s_.b])
            ps2 = c.nps()
            p.mm(ps2.t[:], c.ones.t[:], s_.t[:], True, True, reads=[c.ones.b, s_.b], writes=[ps2.b])
            rmax(mx.t[:, 0, kt:kt + 1], ps2, 128, [ps2.b], [mx.b])
        p.op("dve", lambda e: e.tensor_reduce(red.t[:, 0:1], mx.t[:, 0, 0:NKT], AX.X, ALU.max), reads=[mx.b], writes=[red.b])
        p.tt("dve", red.t[:, 2:3], red.t[:, 0:1], red.t[:, 1:2], ALU.add, reads=[red.b], writes=[red.b])
        for g in range(NKB // 4):
            ps = c.nps()
            for j in range(4):
                kb = 4 * g + j
                for k in range(2):
                    p.mm(ps.t[:, j * 128:(j + 1) * 128], ckv.t[:, k, kb * 128:(kb + 1) * 128], wkv.t[:, k, h * 256 + 128:(h + 1) * 256],
                         k == 0, k == 1, reads=[wkv.b, ckv.b], writes=[ps.b])
            dst = V.t[:, 4 * g:4 * g + 4, :]
            if 4 * g < NCH:
                p.ts("dve", dst, r3(ps.t[:], a=4), c.flg.t[:, 0:1], None, ALU.mult, reads=[ps.b, c.flg.b], writes=[V.b])
            else:
                p.cp("act" if g % 2 else "dve", dst, r3(ps.t[:], a=4), reads=[ps.b], writes=[V.b])
        for tt in range(NT):
            t0 = tt * 512
            rp_ = rpt[tt % 2]
            p.dma(rp_.t[:], c.rope[0:64, 0:2, t0:t0 + 512], writes=[rp_.b])
            ps = c.nps()
            for k in range(3):
                p.mm(ps.t[:], wq.t[:, k, h * 192:h * 192 + 128], cq.t[:, k, t0:t0 + 512], k == 0, k == 2, reads=[wq.b, cq.b], writes=[ps.b])
            p.cp("act", QnT.t[:, t0:t0 + 512], ps.t[:], reads=[ps.b], writes=[QnT.b])
            psr, pst = c.nps(), c.nps()
            for k in range(3):
                p.mm(psr.t[0:64, :], wq.t[:, k, h * 192 + 128:h * 192 + 192], cq.t[:, k, t0:t0 + 512], k == 0, k == 2, reads=[wq.b, cq.b], writes=[psr.b])
            for k in range(3):
                p.mm(pst.t[0:64, :], wq.t[:, k, 768 + h * 64:768 + h * 64 + 64], cq.t[:, k, t0:t0 + 512], k == 0, k == 2, reads=[wq.b, cq.b], writes=[pst.b])
            a_, b_ = ta[tt % 2], tb[tt % 2]
            p.tt("dve", a_.t[:], psr.t[0:64, :], rp_.t[:, 0, :], ALU.mult, reads=[psr.b, rp_.b], writes=[a_.b])
            p.tt("dve", b_.t[:], pst.t[0:64, :], rp_.t[:, 1, :], ALU.mult, reads=[pst.b, rp_.b], writes=[b_.b])
            p.tt("pool", QrT.t[:, t0:t0 + 512], a_.t[:], b_.t[:], ALU.add, reads=[a_.b, b_.b], writes=[QrT.b])
            s_ = nsq()
            p.tt("pool", s_.t[:], QnT.t[:, t0:t0 + 512], QnT.t[:, t0:t0 + 512], ALU.mult, reads=[QnT.b], writes=[s_.b])
            ps2 = c.nps()
            p.mm(ps2.t[:], c.ones.t[:], s_.t[:], True, True, reads=[c.ones.b, s_.b], writes=[ps2.b])
            rmax(mx.t[:, 2, tt:tt + 1], ps2, 128, [ps2.b], [mx.b])
            s2_ = nsq()
            p.tt("pool", s2_.t[0:64, :], QrT.t[:, t0:t0 + 512], QrT.t[:, t0:t0 + 512], ALU.mult, reads=[QrT.b], writes=[s2_.b])
            ps3 = c.nps()
            p.mm(ps3.t[:], c.ones.t[0:64, :], s2_.t[0:64, :], True, True, reads=[c.ones.b, s2_.b], writes=[ps3.b])
            rmax(mx.t[:, 3, tt:tt + 1], ps3, 128, [ps3.b], [mx.b])
        p.tt("dve", bias.t[:, 0:NT], mx.t[:, 2, 0:NT], mx.t[:, 3, 0:NT], ALU.add, reads=[mx.b], writes=[bias.b])
        p.ts("dve", bias.t[:, 0:NT], bias.t[:, 0:NT], red.t[:, 2:3], None, ALU.mult, reads=[bias.b, red.b], writes=[bias.b])
        p.act(bias.t[:, 0:NT], bias.t[:, 0:NT], AF.Ln, reads=[bias.b], writes=[bias.b])
        p.act(bias.t[:, 0:NT], bias.t[:, 0:NT], AF.Exp, reads=[bias.b], writes=[bias.b], scale=0.5)
        p.ts("dve", bias.t[:, 0:NT], bias.t[:, 0:NT], -1.0, None, ALU.mult, reads=[bias.b], writes=[bias.b])

        if h == 0:
            pend = {"f": None}
        for qt in range(NT):
            q0 = qt * 512
            accO, accD = c.ps[k_["e"] % 2], c.ps[2 + k_["e"] % 2]
            blocks = [(kb, -1) for kb in range(NCH)] + [(NCH + kb, (kb - 4 * qt) if kb >= 4 * qt else -1) for kb in range(4 * qt + 4)]
            nb = len(blocks)
            stiles = [None] * nb

            G = 2
            groups = [list(range(a, min(a + G, nb))) for a in range(0, nb, G)]

            def emit_Sg(g):
                idxs = groups[g]
                banks = []
                for j_, i in enumerate(idxs):
                    S = c.ps[4 + 2 * (g % 2) + j_]
                    stiles[i] = S
                    banks.append(S.b)
                first = True
                for i in idxs:
                    kb, dj = blocks[i]
                    S = stiles[i]
                    lo = 0 if dj < 0 else 128 * dj
                    p.mm(S.t[:, lo:512], KnT.t[:, kb * 128:(kb + 1) * 128], QnT.t[:, q0 + lo:q0 + 512], True, False,
                         reads=[KnT.b, QnT.b], writes=(banks if first else [S.b]))
                    first = False
                    p.mm(S.t[:, lo:512], KrT.t[:, kb * 128:(kb + 1) * 128], QrT.t[:, q0 + lo:q0 + 512], False, dj < 0,
                         reads=[KrT.b, QrT.b], writes=[S.b])
                    if dj >= 0:
                        p.mm(S.t[:, lo:lo + 128], idb, maskb, False, True, reads=[cbf.b], writes=[S.b])

            def emit_PVg(g):
                idxs = groups[g]
                Ps = []
                for i in idxs:
                    kb, dj = blocks[i]
                    S = stiles[i]
                    lo = 0 if dj < 0 else 128 * dj
                    P_ = PT[k_["pt"] % 6]
                    k_["pt"] += 1
                    Ps.append(P_)
                    p.act(P_.t[:, lo:512], S.t[:, lo:512], AF.Exp, reads=[S.b, bias.b], writes=[P_.b], bias=bias.t[:, qt:qt + 1], scale=1.0)
                first = True
                for i, P_ in zip(idxs, Ps):
                    kb, dj = blocks[i]
                    lo = 0 if dj < 0 else 128 * dj
                    on = c.fones if kb < NCH else c.ones
                    rd = [V.b, on.b] + ([x.b for x in Ps] if first else [P_.b])
                    first = False
                    p.mm(accO.t[:, lo:512], V.t[:, kb, :], P_.t[:, lo:512], i == 0, i == nb - 1, reads=rd, writes=[accO.b])
                    p.mm(accD.t[:, lo:512], on.t[:], P_.t[:, lo:512], i == 0, i == nb - 1, reads=[on.b, P_.b], writes=[accD.b])

            emit_Sg(0)
            for g in range(len(groups)):
                if g + 1 < len(groups):
                    emit_Sg(g + 1)
                emit_PVg(g)
                if g == 3 and pend["f"] is not None:
                    pend["f"]()
                    pend["f"] = None
            e = k_["e"]
            k_["e"] += 1
            rd_, u_, sq_, rs_, yo_ = rden[e % 2], uu[e % 2], squ[e % 2], rsu[e % 2], yo[e % 2]
            p.op("dve", lambda e_, rd_=rd_, accD=accD: e_.reciprocal(rd_.t[:], accD.t[:]), reads=[accD.b], writes=[rd_.b])
            p.tt("dve", u_.t[:], accO.t[:], rd_.t[:], ALU.mult, reads=[accO.b, rd_.b], writes=[u_.b])
            p.tt("pool", sq_.t[:], u_.t[:], u_.t[:], ALU.mult, reads=[u_.b], writes=[sq_.b])

            def part2(u_=u_, sq_=sq_, rs_=rs_, yo_=yo_, h=h, q0=q0):
                pss = c.ps[6]
                p.mm(pss.t[:], c.ones.t[:], sq_.t[:], True, True, reads=[c.ones.b, sq_.b], writes=[pss.b])
                p.act(rs_.t[:], pss.t[:], AF.Ln, reads=[pss.b], writes=[rs_.b], bias=EPS, scale=1.0 / 128)
                p.act(rs_.t[:], rs_.t[:], AF.Exp, reads=[rs_.b], writes=[rs_.b], scale=-0.5)
                p.tt("pool", yo_.t[:], u_.t[:], rs_.t[:], ALU.mult, reads=[u_.b, rs_.b], writes=[yo_.b])
                p.dma(c.yT[:, h, q0:q0 + 512], yo_.t[:], reads=[yo_.b])
            pend["f"] = part2
    if pend["f"] is not None:
        pend["f"]()
        pend["f"] = None


def phase_D(c, xin, last):
    nc, p, sb, T, NT, NCH = c.nc, c.p, c.sb, c.T, c.NT, c.NCH
    li, vo = c.li, c.vo
    p.barrier()
    sb.reset(c.persist_mark)
    vec, cf, cbf = c.vecs, c.cstf, c.cb
    idb = cbf.t[:, C_ID:C_ID + 128]
    wo = sb.tile([128, 8, D], BF16, "wo")
    wu = sb.tile([128, 8, 2 * DFF], BF16, "wu")
    mW = sb.mark()
    stg = [sb.tile([128, DFF], F32, "stgD") for _ in range(2)]
    alt = Alt(["act", "dve"])
    si = 0
    for k in range(8):
        st = stg[si % 2]
        si += 1
        p.dma(st.t[:, 0:D], c.w_out[li, k * 128:(k + 1) * 128, :], writes=[st.b])
        scaled_copy(c, alt(), wo.t[:, k, :], st.t[:, 0:D], vec.t[:, vo + V_ON + k:vo + V_ON + k + 1], [st.b, vec.b], [wo.b])
    for k in range(8):
        for hf in range(2):
            st = stg[si % 2]
            si += 1
            p.dma(st.t[:], c.w_up[li, k * 128:(k + 1) * 128, hf * DFF:(hf + 1) * DFF], writes=[st.b])
            scaled_copy(c, alt(), wu.t[:, k, hf * DFF:(hf + 1) * DFF], st.t[:], vec.t[:, vo + V_FN + k:vo + V_FN + k + 1], [st.b, vec.b], [wu.b])
    yt = [sb.tile([128, 8, 512], BF16, "ytD") for _ in range(2)]
    xt = [sb.tile([128, D], F32, "xtD") for _ in range(2)]
    xm = [sb.tile([128, D], F32, "xmD") for _ in range(2)]
    junk = sb.tile([128, D], BF16, "junkD")
    ssq = [sb.tile([128, 2], F32, "ssqD") for _ in range(2)]
    hN = [sb.tile([128, D], BF16, "hND") for _ in range(2)]
    hTt = [sb.tile([128, 8, 512], BF16, "hTD") for _ in range(2)]
    i = 0

    def load_x1(ix):
        if ix < 4 * NT:
            p.dma(xt[ix % 2].t[:], xin[ix * 128:(ix + 1) * 128, :], writes=[xt[ix % 2].b])

    def load_y1(tx):
        if tx < NT:
            p.dma(yt[tx % 2].t[:], c.yT[:, :, tx * 512:(tx + 1) * 512], writes=[yt[tx % 2].b])
    load_y1(0)
    load_x1(0)
    for tt in range(NT):
        t0 = tt * 512
        yt_, hT_ = yt[tt % 2], hTt[tt % 2]
        load_y1(tt + 1)
        for s in range(4):
            x_, xm_, ss_, hN_ = xt[i % 2], xm[i % 2], ssq[i % 2], hN[i % 2]
            i += 1
            r0 = t0 + s * 128
            load_x1(i)
            for hf in range(2):
                ps = c.nps()
                for k in range(8):
                    p.mm(ps.t[:], yt_.t[:, k, s * 128:(s + 1) * 128], wo.t[:, k, hf * 512:(hf + 1) * 512], k == 0, k == 7,
                         reads=[yt_.b, wo.b], writes=[ps.b])
                p.tt("dve", xm_.t[:, hf * 512:(hf + 1) * 512], ps.t[:], x_.t[:, hf * 512:(hf + 1) * 512], ALU.add, reads=[ps.b, x_.b], writes=[xm_.b])
            p.dma(c.xres[r0:r0 + 128, :], xm_.t[:], reads=[xm_.b])
            p.act(junk.t[:], xm_.t[:], AF.Square, reads=[xm_.b], writes=[junk.b, ss_.b], accum=ss_.t[:, 0:1])
            rstd_from_ss(c, ss_.t[:, 1:2], ss_.t[:, 0:1], D, [ss_.b], [ss_.b])
            p.act(hN_.t[:], xm_.t[:], AF.Copy, reads=[xm_.b, ss_.b], writes=[hN_.b], scale=ss_.t[:, 1:2])
            for k in range(8):
                p.tr(c.psb.t[:, k * 128:(k + 1) * 128], hN_.t[:, k * 128:(k + 1) * 128], idb, reads=[hN_.b, cbf.b], writes=[c.psb.b])
            p.cp("pool" if False else "dve", hT_.t[:, :, s * 128:(s + 1) * 128], r3(c.psb.t[:], a=8), reads=[c.psb.b], writes=[hT_.b])
        p.dma(c.hT[:, :, t0:t0 + 512], hT_.t[:], reads=[hT_.b])
    if c.stop == "D1":
        return
    hl = hTt[(NT - 1) % 2]
    ps = c.nps()
    for cc in range(NFC):
        for k in range(8):
            p.mm(ps.t[:, 2 * cc:2 * cc + 2], wu.t[:, k, cc * 128:(cc + 1) * 128], hl.t[:, k, 510:512], k == 0, k == 7,
                 reads=[wu.b, hl.b], writes=[ps.b])
    hout = sb.tile([128, 2 * NFC], F32, "hout")
    p.cp("act", hout.t[:], ps.t[:, 0:2 * NFC], reads=[ps.b], writes=[hout.b])
    p.dma(c.halo[:, :], hout.t[:], reads=[hout.b])
    exchange(c, c.halo, c.hgat)
    if c.stop == "Dh":
        return
    sb.reset(mW)
    hin = sb.tile([128, 2 * NFC], F32, "hin")
    p.dma(hin.t[:], c.hgat[0:128, :], writes=[hin.b])
    hal = sb.tile([128, NFC, 2], F32, "hal")
    halb = [Buf() for _ in range(NFC)]
    p.ts("dve", hal.t[:], r3(hin.t[:], a=NFC), c.flg.t[:, 0:1], None, ALU.mult, reads=[hin.b, c.flg.b], writes=halb)
    hT2 = [sb.tile([128, 8, 512], BF16, "hT2") for _ in range(2)]
    asb = [sb.tile([128, 514], F32, "asb") for _ in range(3)]
    t1 = [sb.tile([128, 512], F32, "t1D") for _ in range(2)]
    t2 = [sb.tile([128, 512], F32, "t2D") for _ in range(2)]
    t3 = [sb.tile([128, 512], F32, "t3D") for _ in range(2)]
    sl = [sb.tile([128, 512], F32, "slD") for _ in range(2)]
    gTt = [sb.tile([128, NFC, 512], BF16, "gTt") for _ in range(2)]
    cw = lambda k_, cc: vec.t[:, vo + V_CW + k_ * NFC + cc:vo + V_CW + k_ * NFC + cc + 1]
    cbv = lambda cc: vec.t[:, vo + V_CB + cc:vo + V_CB + cc + 1]
    j = 0

    def load_h2(tx):
        if tx < NT:
            p.dma(hT2[tx % 2].t[:], c.hT[:, :, tx * 512:(tx + 1) * 512], writes=[hT2[tx % 2].b])
    load_h2(0)
    for tt in range(NT):
        t0 = tt * 512
        h_ = hT2[tt % 2]
        g_ = gTt[tt % 2]
        load_h2(tt + 1)
        for cc in range(NFC):
            psa, psg = c.nps(), c.nps()
            for k in range(8):
                p.mm(psa.t[:], wu.t[:, k, cc * 128:(cc + 1) * 128], h_.t[:, k, :], k == 0, k == 7, reads=[wu.b, h_.b], writes=[psa.b])
            for k in range(8):
                p.mm(psg.t[:], wu.t[:, k, DFF + cc * 128:DFF + (cc + 1) * 128], h_.t[:, k, :], k == 0, k == 7, reads=[wu.b, h_.b], writes=[psg.b])
            a_ = asb[j % 3]
            t1_, t2_, t3_, s_ = t1[j % 2], t2[j % 2], t3[j % 2], sl[j % 2]
            j += 1
            p.cp("act", a_.t[:, 2:514], psa.t[:], reads=[psa.b], writes=[a_.b])
            p.cp("pool", a_.t[:, 0:2], hal.t[:, cc, :], reads=[halb[cc]], writes=[a_.b])
            p.cp("pool", hal.t[:, cc, :], a_.t[:, 512:514], reads=[a_.b], writes=[halb[cc]])
            p.ts("pool", t1_.t[:], a_.t[:, 2:514], cw(2, cc), cbv(cc), ALU.mult, ALU.add, reads=[a_.b, vec.b], writes=[t1_.b])
            p.stt("dve", t2_.t[:], a_.t[:, 1:513], cw(1, cc), t1_.t[:], ALU.mult, ALU.add, reads=[a_.b, vec.b, t1_.b], writes=[t2_.b])
            p.stt("dve", t3_.t[:], a_.t[:, 0:512], cw(0, cc), t2_.t[:], ALU.mult, ALU.add, reads=[a_.b, vec.b, t2_.b], writes=[t3_.b])
            p.act(s_.t[:], t3_.t[:], AF.Silu, reads=[t3_.b], writes=[s_.b])
            p.tt("dve", g_.t[:, cc, :], s_.t[:], psg.t[:], ALU.mult, reads=[s_.b, psg.b], writes=[g_.b])
        p.dma(c.gT[:, :, t0:t0 + 512], g_.t[:], reads=[g_.b])
    if c.stop == "D2":
        return
    p.barrier()
    sb.reset(c.persist_mark)
    wd = sb.tile([128, NFC, D], BF16, "wd")
    stg = [sb.tile([128, D], F32, "stgD3") for _ in range(2)]
    for cc in range(NFC):
        st = stg[cc % 2]
        p.dma(st.t[:], c.w_down[li, cc * 128:(cc + 1) * 128, :], writes=[st.b])
        p.cp(alt(), wd.t[:, cc, :], st.t[:], reads=[st.b], writes=[wd.b])
    fnb = None
    if last:
        fnb = sb.tile([128, D], F32, "fnb")
        p.dma(fnb.t[:], c.fnb[:, :], writes=[fnb.b])
    g3 = [sb.tile([128, NFC, 512], BF16, "g3") for _ in range(2)]
    xt = [sb.tile([128, D], F32, "xt3") for _ in range(2)]
    xo = [sb.tile([128, D], F32, "xo3") for _ in range(2)]
    junk = sb.tile([128, D], BF16, "junk3")
    ssq = [sb.tile([128, 2], F32, "ssq3") for _ in range(2)]
    yn = [sb.tile([128, D], F32, "yn3") for _ in range(2)]
    i = 0

    def load_g3(tx):
        if tx < NT:
            p.dma(g3[tx % 2].t[:], c.gT[:, :, tx * 512:(tx + 1) * 512], writes=[g3[tx % 2].b])

    def load_x3(ix):
        if ix < 4 * NT:
            p.dma(xt[ix % 2].t[:], c.xres[ix * 128:(ix + 1) * 128, :], writes=[xt[ix % 2].b])
    load_g3(0)
    load_x3(0)
    for tt in range(NT):
        t0 = tt * 512
        g_ = g3[tt % 2]
        load_g3(tt + 1)
        for s in range(4):
            x_, xo_, ss_, yn_ = xt[i % 2], xo[i % 2], ssq[i % 2], yn[i % 2]
            i += 1
            r0 = t0 + s * 128
            load_x3(i)
            for hf in range(2):
                ps = c.nps()
                for cc in range(NFC):
                    p.mm(ps.t[:], g_.t[:, cc, s * 128:(s + 1) * 128], wd.t[:, cc, hf * 512:(hf + 1) * 512], cc == 0, cc == NFC - 1,
                         reads=[g_.b, wd.b], writes=[ps.b])
                p.tt("dve", xo_.t[:, hf * 512:(hf + 1) * 512], ps.t[:], x_.t[:, hf * 512:(hf + 1) * 512], ALU.add, reads=[ps.b, x_.b], writes=[xo_.b])
            if not last:
                p.dma(c.xres[r0:r0 + 128, :], xo_.t[:], reads=[xo_.b])
            else:
                p.act(junk.t[:], xo_.t[:], AF.Square, reads=[xo_.b], writes=[junk.b, ss_.b], accum=ss_.t[:, 0:1])
                rstd_from_ss(c, ss_.t[:, 1:2], ss_.t[:, 0:1], D, [ss_.b], [ss_.b])
                p.act(yn_.t[:], xo_.t[:], AF.Copy, reads=[xo_.b, ss_.b], writes=[yn_.b], scale=ss_.t[:, 1:2])
                p.tt("pool", yn_.t[:], yn_.t[:], fnb.t[:], ALU.mult, reads=[yn_.b, fnb.b], writes=[yn_.b])
                p.dma(c.out[r0:r0 + 128, :], yn_.t[:], reads=[yn_.b])


_NC_CACHE = {}


def kernel(x, attn_norm, w_in, mla_q_norm, mla_w_uq, mla_kv_norm, mla_w_ukv, mla_out_norm,
           gla_w_gate, gla_b_gate, gla_out_norm, ret_out_norm, w_out, ffn_norm, ffn_w_up,
           ffn_conv_w, ffn_conv_b, ffn_w_down, final_norm):
    f = lambda a: np.ascontiguousarray(np.asarray(a, dtype=np.float32))
    x = f(x)
    B, S, _ = x.shape
    T = S // 2
    nl = int(np.asarray(w_in).shape[0])
    assert B * 2 == 8
    inp = dict(attn_norm=f(attn_norm), ffn_norm=f(ffn_norm), mla_q_norm=f(mla_q_norm), mla_kv_norm=f(mla_kv_norm),
               mla_out_norm=f(mla_out_norm), gla_out_norm=f(gla_out_norm), ret_out_norm=f(ret_out_norm),
               gla_b_gate=f(gla_b_gate), ffn_conv_w=f(ffn_conv_w), ffn_conv_b=f(ffn_conv_b))
    key = (T, nl)
    if key not in _NC_CACHE:
        _NC_CACHE[key] = build(T, nl)
    nc = _NC_CACHE[key]
    cst = host_consts(T)
    vec = host_vec(inp, 0, nl)
    fnb = np.ascontiguousarray(np.broadcast_to(f(final_norm)[None, :], (128, D)))
    shared = dict(w_in=f(w_in), w_uq=f(mla_w_uq), w_ukv=f(mla_w_ukv), w_out=f(w_out), w_up=f(ffn_w_up),
                  w_down=f(ffn_w_down), w_gate=f(gla_w_gate), vec=vec, cst=cst, fnb=fnb)
    ropes = [host_rope(T, r * T) for r in range(2)]
    in_maps = []
    for core in range(8):
        b, r = core // 2, core % 2
        m = dict(shared)
        m["x"] = np.ascontiguousarray(x[b, r * T:(r + 1) * T])
        m["rope"] = ropes[r]
        m["flag"] = np.full((128, 1), float(r), np.float32)
        in_maps.append(m)
    res = run_bass_kernel_spmd(nc, in_maps, core_ids=list(range(8)))
    out = np.empty((B, S, D), np.float32)
    for core in range(8):
        b, r = core // 2, core % 2
        out[b, r * T:(r + 1) * T] = np.asarray(res.results[core]["out"])
    return out
```

```python
import numpy as np
import ml_dtypes
import concourse.bass as bass
import concourse.mybir as mybir
from concourse.bass_utils import run_bass_kernel_spmd

F32 = mybir.dt.float32
BF16 = mybir.dt.bfloat16
AF = mybir.ActivationFunctionType
ALU = mybir.AluOpType
AX = mybir.AxisListType

D = 1024
DEPTH = 4
EPS = 1e-6
DIN = 2256
DFF = 2816
NFC = DFF // 128
SCALE_MLA = 192 ** -0.5

ENGS = ("pe", "act", "dve", "pool", "sp")
KDMA = 8


class Buf:
    __slots__ = ("lw", "rd", "name")

    def __init__(self, name=""):
        self.lw = None
        self.rd = []
        self.name = name


class Op:
    __slots__ = ("eng", "fn", "deps", "kind", "signal", "tok", "idx", "dma_n")


class Prog:
    def __init__(self, nc):
        self.nc = nc
        self.ops = {e: [] for e in ENGS}
        self.all = []
        self.ndma = {e: 0 for e in ENGS}
        self.last = {e: None for e in ENGS}
        self.pending_barrier = {e: [] for e in ENGS}
        self.dmas = {e: [] for e in ENGS}
        self.lastcc = None

    def op(self, eng, fn, reads=(), writes=(), kind="c"):
        o = Op()
        o.eng, o.fn, o.kind, o.signal, o.tok = eng, fn, kind, False, None
        deps = []
        for b in reads:
            if b.lw is not None:
                deps.append(b.lw)
        for b in writes:
            if b.lw is not None:
                deps.append(b.lw)
            deps.extend(b.rd)
        deps.extend(self.pending_barrier[eng])
        self.pending_barrier[eng] = []
        dd = []
        seen = set()
        for d in deps:
            if id(d) in seen:
                continue
            seen.add(id(d))
            if d.eng == "pe" and eng == "pe" and d.kind == "c" and kind == "c":
                continue
            dd.append(d)
        o.deps = dd
        for b in writes:
            b.lw = o
            b.rd = []
        for b in reads:
            b.rd.append(o)
        if kind == "d":
            o.dma_n = self.ndma[eng]
            self.ndma[eng] += 1
            self.dmas[eng].append(o)
        if kind == "cc":
            if self.lastcc is not None and all(x is not self.lastcc for x in o.deps):
                o.deps.append(self.lastcc)
            self.lastcc = o
            self.dmas[eng].append(o)
        o.idx = len(self.ops[eng])
        self.ops[eng].append(o)
        self.all.append(o)
        self.last[eng] = o
        return o

    def barrier(self):
        lasts = [self.last[e] for e in ENGS if self.last[e] is not None]
        for e in ENGS:
            lasts.extend(self.dmas[e][-(KDMA + 2):])
        for e in ENGS:
            self.pending_barrier[e] = list(lasts)

    def dma(self, out, in_, reads=(), writes=(), eng="sp"):
        return self.op(eng, lambda e: e.dma_start(out=out, in_=in_), reads, writes, kind="d")

    def mm(self, out, lhsT, rhs, start, stop, reads=(), writes=()):
        return self.op("pe", lambda e: e.matmul(out, lhsT, rhs, start=start, stop=stop), reads, writes)

    def tr(self, out, in_, ident, reads=(), writes=()):
        return self.op("pe", lambda e: e.transpose(out, in_, ident), reads, writes)

    def act(self, out, in_, func, reads=(), writes=(), bias=None, scale=None, accum=None):
        def fn(e):
            kw = {}
            if bias is not None:
                kw["bias"] = bias
            if scale is not None:
                kw["scale"] = scale
            if accum is not None:
                kw["accum_out"] = accum
            return e.activation(out, in_, func, **kw)
        return self.op("act", fn, reads, writes)

    def tt(self, eng, out, in0, in1, op, reads=(), writes=()):
        return self.op(eng, lambda e: e.tensor_tensor(out, in0, in1, op), reads, writes)

    def ts(self, eng, out, in0, s1, s2, op0, op1=None, reads=(), writes=()):
        def fn(e):
            if op1 is None:
                return e.tensor_scalar(out, in0, s1, None, op0)
            return e.tensor_scalar(out, in0, s1, s2, op0, op1)
        return self.op(eng, fn, reads, writes)

    def stt(self, eng, out, in0, scalar, in1, op0, op1, reads=(), writes=()):
        eng = "dve"
        return self.op(eng, lambda e: e.scalar_tensor_tensor(out, in0, scalar, in1, op0, op1), reads, writes)

    def cp(self, eng, out, in_, reads=(), writes=()):
        if eng == "act":
            return self.op("act", lambda e: e.copy(out=out, in_=in_), reads, writes)
        return self.op(eng, lambda e: e.tensor_copy(out, in_), reads, writes)

    def memset(self, eng, ap, val, writes=()):
        return self.op(eng, lambda e: e.memset(ap, val), (), writes)

    def emit(self):
        nc = self.nc
        for o in self.all:
            for d in o.deps:
                d.signal = True
        import contextlib
        with contextlib.ExitStack() as es:
            csem = {e: es.enter_context(nc.semaphore("c_" + e)) for e in ("pe", "act", "dve", "pool")}
            dsem = {e: [es.enter_context(nc.semaphore("d_%s%d" % (e, i))) for i in range(KDMA)]
                    for e in ENGS if self.ndma[e] > 0}
            ccsem = es.enter_context(nc.semaphore("ccs"))
            cnt = {e: 0 for e in ENGS}
            ccn = 0
            for e in ENGS:
                for o in self.ops[e]:
                    if o.kind == "d":
                        o.signal = True
                        o.tok = (dsem[e][o.dma_n % KDMA], 16 * (o.dma_n // KDMA + 1), 16)
                    elif o.kind == "cc":
                        ccn += 1
                        o.signal = True
                        o.tok = (ccsem, ccn, 1)
                    elif o.signal:
                        cnt[e] += 1
                        o.tok = (csem[e], cnt[e], 1)
            block = es.enter_context(nc.Block())
            prog = self

            def body(ename):
                def f(eng):
                    waited = {}
                    ops = prog.ops[ename]
                    for o in ops:
                        ws = []
                        for d in o.deps:
                            ws.append((d.tok[0], d.tok[1]))
                        if o.kind == "d" and o.dma_n >= KDMA:
                            ws.append((dsem[ename][o.dma_n % KDMA], 16 * (o.dma_n // KDMA)))
                        for (s, v) in ws:
                            k = s.num
                            if waited.get(k, 0) < v:
                                eng.wait_ge(s, v)
                                waited[k] = v
                        ins = o.fn(eng)
                        if o.signal:
                            ins.then_inc(o.tok[0], o.tok[2])
                    if ename in dsem:
                        n = prog.ndma[ename]
                        for i in range(KDMA):
                            c = (n - i + KDMA - 1) // KDMA
                            if c > 0 and waited.get(dsem[ename][i].num, 0) < 16 * c:
                                eng.wait_ge(dsem[ename][i], 16 * c)
                return f

            block.tensor(body("pe"))
            block.scalar(body("act"))
            block.vector(body("dve"))
            block.gpsimd(body("pool"))
            block.sync(body("sp"))


class TL:
    __slots__ = ("t", "b")

    def __init__(self, t, name=""):
        self.t = t
        self.b = Buf(name)


class SB:
    BASE = 16512
    TOP = 229344

    def __init__(self, nc):
        self.nc = nc
        self.off = SB.BASE
        self.n = 0

    def tile(self, shape, dt, name="t"):
        per = 1
        for s in shape[1:]:
            per *= s
        nbytes = per * (2 if dt == BF16 else 4)
        nbytes = (nbytes + 31) // 32 * 32
        self.n += 1
        t = self.nc.alloc_sbuf_tensor_at("%s_%d" % (name, self.n), list(shape), dt, offset=self.off)
        self.off += nbytes
        assert self.off <= SB.TOP, ("SBUF overflow", name, self.off)
        return TL(t, name)

    def mark(self):
        return self.off

    def reset(self, m):
        self.off = m


C_ID, C_TRI, C_MASKT, C_BLK, C_B64, C_RDT, C_KDN, C_QDT, C_HM = 0, 128, 256, 384, 640, 768, 1280, 1408, 1536
C_RP = 1540


def host_consts(T):
    nch = T // 128
    ncst = C_RP + nch + 1
    c = np.zeros((128, ncst), np.float32)
    r = np.arange(128)
    c[:, C_ID:C_ID + 128] = np.eye(128, dtype=np.float32)
    c[:, C_TRI:C_TRI + 128] = (r[:, None] <= r[None, :]).astype(np.float32)
    c[:, C_MASKT:C_MASKT + 128] = np.where(r[:, None] <= r[None, :], 0.0, -30000.0)
    hh = r // 32
    for h in range(4):
        c[:, C_HM + h] = (hh == h)
    c[:, C_BLK:C_BLK + 256] = (hh[:, None] == (np.arange(256) // 64)[None, :])
    c[:, C_B64:C_B64 + 128] = ((r // 64)[:, None] == (r // 64)[None, :])
    gam = 1.0 - 2.0 ** (-5.0 - np.arange(4, dtype=np.float64))
    lg = np.log(gam)
    for h in range(4):
        dif = r[None, :] - r[:, None]
        c[:, C_RDT + h * 128:C_RDT + (h + 1) * 128] = np.where(dif >= 0, np.exp(lg[h] * np.maximum(dif, 0)), 0.0)
        c[:, C_KDN + h * 32:C_KDN + (h + 1) * 32] = np.exp(lg[h] * (127 - r))[:, None]
    c[:, C_QDT:C_QDT + 128] = np.exp(lg[hh][:, None] * (r[None, :] + 1.0))
    for n in range(nch + 1):
        c[:, C_RP + n] = np.exp(lg[hh] * 128.0 * n)
    return c


def host_rope(T, pos0):
    out = np.zeros((128, 4, T), np.float32)
    pos = (pos0 + np.arange(T)).astype(np.float32)
    inv = (10000.0 ** (-(np.arange(0, 64, 2, dtype=np.float32) / 64))).astype(np.float32)
    ang = pos[None, :] * inv[:, None]
    out[0:32, 0], out[32:64, 0] = np.cos(ang), np.cos(ang)
    out[0:32, 1], out[32:64, 1] = np.sin(ang), np.sin(ang)
    inv2 = (10000.0 ** (-(np.arange(0, 32, 2, dtype=np.float32) / 32))).astype(np.float32)
    ang2 = pos[None, :] * inv2[:, None]
    c2 = np.concatenate([np.cos(ang2), np.cos(ang2)], 0)
    s2 = np.concatenate([np.sin(ang2), np.sin(ang2)], 0)
    out[:, 2] = np.tile(c2, (4, 1))
    out[:, 3] = np.tile(s2, (4, 1))
    return out


V_AN, V_FN, V_QN, V_KVN, V_ON, V_BG, V_CW, V_CB, V_PER = 0, 8, 16, 19, 21, 29, 30, 96, 118


def host_vec(inp, nl0, nl):
    v = np.zeros((128, nl * V_PER), np.float32)

    def cols(a):
        return np.ascontiguousarray(a.reshape(-1, 128).T)
    for i in range(nl):
        l = nl0 + i
        o = i * V_PER
        v[:, o + V_AN:o + V_AN + 8] = cols(inp["attn_norm"][l])
        v[:, o + V_FN:o + V_FN + 8] = cols(inp["ffn_norm"][l])
        v[:, o + V_QN:o + V_QN + 3] = cols(inp["mla_q_norm"][l])
        v[:, o + V_KVN:o + V_KVN + 2] = cols(inp["mla_kv_norm"][l])
        v[:, o + V_ON:o + V_ON + 4] = cols(inp["mla_out_norm"][l])
        v[:, o + V_ON + 4:o + V_ON + 6] = cols(inp["gla_out_norm"][l])
        v[:, o + V_ON + 6:o + V_ON + 8] = cols(inp["ret_out_norm"][l])
        v[:, o + V_BG:o + V_BG + 1] = cols(inp["gla_b_gate"][l])
        for k in range(3):
            v[:, o + V_CW + k * NFC:o + V_CW + (k + 1) * NFC] = cols(inp["ffn_conv_w"][l, k])
        v[:, o + V_CB:o + V_CB + NFC] = cols(inp["ffn_conv_b"][l])
    return v


class Ctx:
    pass


def r3(ap, **kw):
    return ap.rearrange("p (a b) -> p a b", **kw)


def build(T, nl, final_norm=True, dbg=None, ext=(), stop_after=None, stop_layer=0):
    assert T % 512 == 0
    NT = T // 512
    NCH = T // 128
    nc = bass.Bass("TRN2", target_bir_lowering=False)
    p = Prog(nc)
    sb = SB(nc)
    c = Ctx()
    c.nc, c.p, c.sb, c.T, c.NT, c.NCH = nc, p, sb, T, NT, NCH
    c.stop = None

    def din(name, shape, dt=F32):
        return nc.dram_tensor(name, list(shape), dt, kind="ExternalInput")

    def dscr(name, shape, dt):
        if name in ext:
            return nc.dram_tensor(name, list(shape), dt, kind="ExternalOutput")
        return nc.dram_tensor(name, list(shape), dt)

    c.x = din("x", [T, D])
    c.w_in = din("w_in", [nl, D, DIN])
    c.w_uq = din("w_uq", [nl, 384, 768])
    c.w_ukv = din("w_ukv", [nl, 256, 1024])
    c.w_out = din("w_out", [nl, D, D])
    c.w_up = din("w_up", [nl, D, 2 * DFF])
    c.w_down = din("w_down", [nl, DFF, D])
    c.w_gate = din("w_gate", [nl, 16, 128])
    c.vec = din("vec", [128, nl * V_PER])
    ncst = C_RP + NCH + 1
    c.cst = din("cst", [128, ncst])
    c.rope = din("rope", [128, 4, T])
    c.flag = din("flag", [128, 1])
    c.fnb = din("fnb", [128, D])
    c.out = nc.dram_tensor("out", [T, D], F32, kind="ExternalOutput")
    c.xres = dscr("xres", [T, D], F32)
    c.dT = dscr("dT", [128, 11, T], BF16)
    c.logaT = dscr("logaT", [128, T], F32)
    c.dN = dscr("dN", [T, 512], BF16)
    c.xb1 = dscr("xb1", [128, 2 * T], BF16)
    c.xg1 = dscr("xg1", [256, 2 * T], BF16)
    c.xb2 = dscr("xb2", [128, T], BF16)
    c.xg2 = dscr("xg2", [256, T], BF16)
    c.oint = dscr("oint", [2, 128, 2, T], F32)
    c.qt = dscr("qt", [2, 128, T], BF16)
    c.sx = dscr("sx", [128, 512], F32)
    c.sgat = dscr("sgat", [256, 512], F32)
    c.yT = dscr("yT", [128, 8, T], BF16)
    c.hT = dscr("hT", [128, 8, T], BF16)
    c.halo = dscr("halo", [128, 2 * NFC], F32)
    c.hgat = dscr("hgat", [256, 2 * NFC], F32)
    c.gT = dscr("gT", [128, NFC, T], BF16)
    c.dbg = None
    if dbg is not None:
        c.dbg = nc.dram_tensor("dbg", list(dbg), F32, kind="ExternalOutput")

    c.ps = [TL(nc.alloc_psum_tensor("ps%d" % i, [128, 512], F32), "ps%d" % i) for i in range(7)]
    _sv = nc.psum_base
    c.psb = TL(nc.alloc_psum_tensor("psb", [128, 1024], BF16), "psb")
    nc.psum_base = _sv
    _ps7 = TL(nc.alloc_psum_tensor("ps7", [128, 512], F32), "ps7")
    _ps7.b = c.psb.b
    c.ps.append(_ps7)
    c.psi = 0

    def nps():
        t = c.ps[c.psi % 7]
        c.psi += 1
        return t
    c.nps = nps

    c.cstf = sb.tile([128, ncst], F32, "cstf")
    p.dma(c.cstf.t[:], c.cst[:, :], writes=[c.cstf.b])
    c.vecs = sb.tile([128, nl * V_PER], F32, "vecs")
    p.dma(c.vecs.t[:], c.vec[:, :], writes=[c.vecs.b])
    c.flg = sb.tile([128, 1], F32, "flag")
    p.dma(c.flg.t[:], c.flag[:, :], writes=[c.flg.b])
    c.cb = sb.tile([128, 1540], BF16, "cstb")
    p.cp("dve", c.cb.t[:], c.cstf.t[:, 0:1540], reads=[c.cstf.b], writes=[c.cb.b])
    c.ones = sb.tile([128, 128], BF16, "ones")
    p.memset("pool", c.ones.t[:], 1.0, writes=[c.ones.b])
    c.fones = sb.tile([128, 128], BF16, "fones")
    p.ts("dve", c.fones.t[:], c.ones.t[:], c.flg.t[:, 0:1], None, ALU.mult, reads=[c.ones.b, c.flg.b], writes=[c.fones.b])
    c.tri4 = sb.tile([128, 512], F32, "tri4")
    for h in range(4):
        p.cp("pool", c.tri4.t[:, h * 128:(h + 1) * 128], c.cstf.t[:, C_TRI:C_TRI + 128], reads=[c.cstf.b], writes=[c.tri4.b])
    c.zero = sb.tile([128, 256], F32, "zero")
    p.memset("pool", c.zero.t[:], 0.0, writes=[c.zero.b])
    zb = sb.tile([128, 512], BF16, "zb")
    p.memset("pool", zb.t[:], 0.0, writes=[zb.b])
    for a0 in range(0, T, 512):
        p.dma(c.xb2[64:128, a0:a0 + 512], zb.t[64:128, :], reads=[zb.b])
    c.persist_mark = sb.mark()

    xin = c.x
    for li in range(nl):
        c.li = li
        c.vo = li * V_PER
        if li == stop_layer:
            c.stop = stop_after
        stop_after_ = stop_after if li == stop_layer else None
        phase_A(c, xin)
        if stop_after_ == "A":
            break
        exchange(c, c.xb1, c.xg1)
        exchange(c, c.xb2, c.xg2)
        if stop_after_ == "X":
            break
        phase_B(c)
        if stop_after_ in ("B", "B1", "B2", "B2x"):
            break
        phase_C(c)
        if stop_after_ == "C":
            break
        phase_D(c, xin, last=(li == nl - 1) and final_norm)
        xin = c.xres
    if not final_norm:
        pass
    p.barrier()
    p.emit()
    return nc


def exchange(c, src, dst):
    p = c.p
    p.barrier()
    sap, dap = src.ap().opt(), dst.ap().opt()
    p.op("pool", lambda e: e.collective_compute("AllGather", ALU.bypass, replica_groups=[[0, 1], [2, 3], [4, 5], [6, 7]],
                                                ins=[sap], outs=[dap]), kind="cc")
    p.barrier()


def rstd_from_ss(c, out_ap, ss_ap, n, reads, writes):
    p = c.p
    p.act(out_ap, ss_ap, AF.Sqrt, reads=reads, writes=writes, bias=EPS, scale=1.0 / n)
    p.op("dve", lambda e: e.reciprocal(out_ap, out_ap), reads=writes, writes=writes)


class Alt:
    def __init__(self, engs):
        self.engs = engs
        self.i = 0

    def __call__(self):
        e = self.engs[self.i % len(self.engs)]
        self.i += 1
        return e


def scaled_copy(c, eng, out, in_, scal, reads, writes):
    p = c.p
    if eng == "act":
        p.act(out, in_, AF.Copy, reads=reads, writes=writes, scale=scal)
    else:
        p.ts(eng, out, in_, scal, None, ALU.mult, reads=reads, writes=writes)


WX = 2576
S32 = 32 ** -0.5


def phase_A(c, xin):
    nc, p, sb, T, NT = c.nc, c.p, c.sb, c.T, c.NT
    li, vo = c.li, c.vo
    p.barrier()
    sb.reset(c.persist_mark)
    vec = c.vecs
    gv = sb.tile([128, 4, 8], F32, "gv")
    g0 = vec.t[:, vo + V_AN:vo + V_AN + 8]
    p.cp("dve", gv.t[:, 0, :], g0, reads=[vec.b], writes=[gv.b])
    p.ts("dve", gv.t[:, 1, :], g0, -1.0, None, ALU.mult, reads=[vec.b], writes=[gv.b])
    p.ts("dve", gv.t[:, 2, :], g0, S32, None, ALU.mult, reads=[vec.b], writes=[gv.b])
    p.ts("dve", gv.t[:, 3, :], g0, -S32, None, ALU.mult, reads=[vec.b], writes=[gv.b])
    nbg = sb.tile([128, 1], F32, "nbg")
    p.ts("dve", nbg.t[:], vec.t[:, vo + V_BG:vo + V_BG + 1], -1.0, None, ALU.mult, reads=[vec.b], writes=[nbg.b])
    W = sb.tile([128, 8, WX], BF16, "winx")
    stg = [sb.tile([128, DIN], F32, "wstg") for _ in range(2)]
    alt = Alt(["act", "dve"])
    for k in range(8):
        st = stg[k % 2]
        p.dma(st.t[:], c.w_in[li, k * 128:(k + 1) * 128, :], writes=[st.b])
        g, gn, gs, gsn = (gv.t[:, j, k:k + 1] for j in range(4))

        def cv(d0, d1, s0, s1, sc):
            scaled_copy(c, alt(), W.t[:, k, d0:d1], st.t[:, s0:s1], sc, [st.b, gv.b], [W.b])

        def cvrot(d0, s0, nh, half, sp, sn):
            dv = W.t[:, k, d0:d0 + nh * 2 * half].rearrange("p (h two d) -> p h two d", h=nh, two=2)
            sv = st.t[:, s0:s0 + nh * 2 * half].rearrange("p (h two d) -> p h two d", h=nh, two=2)
            scaled_copy(c, alt(), dv[:, :, 0, :], sv[:, :, 1, :], sn, [st.b, gv.b], [W.b])
            scaled_copy(c, alt(), dv[:, :, 1, :], sv[:, :, 0, :], sp, [st.b, gv.b], [W.b])
        cv(0, 704, 0, 704, g)
        cvrot(704, 640, 1, 32, g, gn)
        cv(768, 896, 704, 832, gs)
        cv(896, 1024, 832, 960, g)
        cv(1024, 1040, 1216, 1232, g)
        cv(1040, 1296, 1232, 1488, g)
        cv(1296, 1424, 1488, 1616, g)
        cvrot(1424, 1488, 4, 16, g, gn)
        cv(1552, 1680, 1616, 1744, gs)
        cvrot(1680, 1616, 4, 16, gs, gsn)
        cv(1808, 2064, 2000, 2256, g)
        cv(2064, 2320, 960, 1216, g)
        cv(2320, 2576, 1744, 2000, g)
    wgf = sb.tile([16, 128], F32, "wgf")
    p.dma(wgf.t[:], c.w_gate[li, :, :], writes=[wgf.b])
    wg = sb.tile([16, 128], BF16, "wg")
    p.cp("dve", wg.t[:], wgf.t[:], reads=[wgf.b], writes=[wg.b])

    xt = [sb.tile([128, D], F32, "xt") for _ in range(2)]
    junk = sb.tile([128, D], BF16, "junk")
    ssq = [sb.tile([128, 2], F32, "ssq") for _ in range(2)]
    hN = [sb.tile([128, D], BF16, "hN") for _ in range(2)]
    hT = [sb.tile([128, 8, 512], BF16, "hT") for _ in range(2)]
    zf = [sb.tile([128, 3, 512], F32, "zf") for _ in range(2)]
    sq = [sb.tile([128, 3, 512], BF16, "sq") for _ in range(2)]
    rs = [sb.tile([128, 512], F32, "rs") for _ in range(2)]
    zn = [sb.tile([128, 3, 512], BF16, "zn") for _ in range(2)]
    rp = sb.tile([128, 4, 512], F32, "ropeA")
    tA = [sb.tile([128, 512], F32, "tA") for _ in range(2)]
    tB = [sb.tile([128, 512], F32, "tB") for _ in range(2)]
    ob = [sb.tile([128, 512], BF16, "obA") for _ in range(4)]
    glr = sb.tile([16, 512], BF16, "glr")
    ex = sb.tile([128, 512], F32, "exA")
    la = [sb.tile([128, 512], F32, "laA") for _ in range(2)]
    vN = [sb.tile([128, 512], BF16, "vN") for _ in range(2)]
    cnt = {"ob": 0, "sub": 0, "g": 0}
    idb = c.cb.t[:, C_ID:C_ID + 128]

    def nob():
        t = ob[cnt["ob"] % 4]
        cnt["ob"] += 1
        return t

    def proj(c0, M, hTt):
        ps = c.nps()
        for k in range(8):
            p.mm(ps.t[0:M, :], W.t[:, k, c0:c0 + M], hTt.t[:, k, :], k == 0, k == 7, reads=[W.b, hTt.b], writes=[ps.b])
        return ps

    def load_x(i):
        if i < 4 * NT:
            p.dma(xt[i % 2].t[:], xin[i * 128:(i + 1) * 128, :], writes=[xt[i % 2].b])
    load_x(0)
    for tt in range(NT):
        t0 = tt * 512
        hTt = hT[tt % 2]
        p.dma(rp.t[:], c.rope[:, :, t0:t0 + 512], writes=[rp.b])
        for s in range(4):
            i = cnt["sub"]
            cnt["sub"] += 1
            x_, ss_, hN_ = xt[i % 2], ssq[i % 2], hN[i % 2]
            load_x(i + 1)
            p.act(junk.t[:], x_.t[:], AF.Square, reads=[x_.b], writes=[junk.b, ss_.b], accum=ss_.t[:, 0:1])
            rstd_from_ss(c, ss_.t[:, 1:2], ss_.t[:, 0:1], D, [ss_.b], [ss_.b])
            p.act(hN_.t[:], x_.t[:], AF.Copy, reads=[x_.b, ss_.b], writes=[hN_.b], scale=ss_.t[:, 1:2])
            for k in range(8):
                p.tr(c.psb.t[:, k * 128:(k + 1) * 128], hN_.t[:, k * 128:(k + 1) * 128], idb, reads=[hN_.b, c.cb.b], writes=[c.psb.b])
            p.cp("dve", hTt.t[:, :, s * 128:(s + 1) * 128], r3(c.psb.t[:], a=8), reads=[c.psb.b], writes=[hTt.b])

        for (c0, nchk, n, which) in ((0, 3, 384, "cq"), (384, 2, 256, "ckv")):
            gi = cnt["g"]
            cnt["g"] += 1
            zf_, sq_, rs_, zn_ = zf[gi % 2], sq[gi % 2], rs[gi % 2], zn[gi % 2]
            for j in range(nchk):
                ps = proj(c0 + j * 128, 128, hTt)
                p.cp("act", zf_.t[:, j, :], ps.t[:], reads=[ps.b], writes=[zf_.b])
                p.tt("pool", sq_.t[:, j, :], zf_.t[:, j, :], zf_.t[:, j, :], ALU.mult, reads=[zf_.b], writes=[sq_.b])
            pss = c.nps()
            for j in range(nchk):
                p.mm(pss.t[:], c.ones.t[:], sq_.t[:, j, :], j == 0, j == nchk - 1, reads=[c.ones.b, sq_.b], writes=[pss.b])
            rstd_from_ss(c, rs_.t[:], pss.t[:], n, [pss.b], [rs_.b])
            for j in range(nchk):
                p.tt("dve", zn_.t[:, j, :], zf_.t[:, j, :], rs_.t[:], ALU.mult, reads=[zf_.b, rs_.b], writes=[zn_.b])
            if which == "cq":
                p.dma(c.dT[:, 0:3, t0:t0 + 512], zn_.t[:, 0:3, :], reads=[zn_.b])
            else:
                p.dma(r3(c.xb1[:, :], a=2)[:, :, t0:t0 + 512], zn_.t[:, 0:2, :], reads=[zn_.b])

        def roped(c_raw, c_rot, M, ci, si, dst_ap):
            pr_, pt_ = proj(c_raw, M, hTt), proj(c_rot, M, hTt)
            a_, b_ = tA[cnt["g"] % 2], tB[cnt["g"] % 2]
            cnt["g"] += 1
            p.tt("dve", a_.t[0:M, :], pr_.t[0:M, :], rp.t[0:M, ci, :], ALU.mult, reads=[pr_.b, rp.b], writes=[a_.b])
            p.tt("dve", b_.t[0:M, :], pt_.t[0:M, :], rp.t[0:M, si, :], ALU.mult, reads=[pt_.b, rp.b], writes=[b_.b])
            o_ = nob()
            p.tt("pool", o_.t[0:M, :], a_.t[0:M, :], b_.t[0:M, :], ALU.add, reads=[a_.b, b_.b], writes=[o_.b])
            p.dma(dst_ap, o_.t[0:M, :], reads=[o_.b])

        roped(640, 704, 64, 0, 1, c.xb2[0:64, t0:t0 + 512])
        roped(1296, 1424, 128, 2, 3, c.dT[:, 5, t0:t0 + 512])
        roped(1552, 1680, 128, 2, 3, c.dT[:, 6, t0:t0 + 512])

        for (c0, slot) in ((768, 3), (896, 4)):
            ps = proj(c0, 128, hTt)
            o_ = nob()
            p.cp("act", o_.t[:], ps.t[:], reads=[ps.b], writes=[o_.b])
            p.dma(c.dT[:, slot, t0:t0 + 512], o_.t[:], reads=[o_.b])
        for (c0, slot) in ((1040, 7), (1168, 8), (1808, 9), (1936, 10)):
            ps = proj(c0, 128, hTt)
            o_ = nob()
            p.act(o_.t[:], ps.t[:], AF.Silu, reads=[ps.b], writes=[o_.b])
            p.dma(c.dT[:, slot, t0:t0 + 512], o_.t[:], reads=[o_.b])
        ps = proj(1024, 16, hTt)
        p.cp("act", glr.t[:], ps.t[0:16, :], reads=[ps.b], writes=[glr.b])
        ps2 = c.nps()
        p.mm(ps2.t[:], wg.t[:], glr.t[:], True, True, reads=[wg.b, glr.b], writes=[ps2.b])
        p.act(ex.t[:], ps2.t[:], AF.Exp, reads=[ps2.b, nbg.b], writes=[ex.b], bias=nbg.t[:, 0:1], scale=-1.0)
        la_ = la[tt % 2]
        p.act(la_.t[:], ex.t[:], AF.Ln, reads=[ex.b], writes=[la_.b], bias=1.0)
        p.ts("pool", la_.t[:], la_.t[:], -1.0 / 16.0, None, ALU.mult, reads=[la_.b], writes=[la_.b])
        p.dma(c.logaT[:, t0:t0 + 512], la_.t[:], reads=[la_.b])
        for s in range(4):
            ps = c.nps()
            for k in range(8):
                p.mm(ps.t[:], hTt.t[:, k, s * 128:(s + 1) * 128], W.t[:, k, 2064:2576], k == 0, k == 7, reads=[W.b, hTt.b], writes=[ps.b])
            v_ = vN[s % 2]
            p.cp("act" if s % 2 else "dve", v_.t[:], ps.t[:], reads=[ps.b], writes=[v_.b])
            p.dma(c.dN[t0 + s * 128:t0 + (s + 1) * 128, :], v_.t[:], reads=[v_.b])


def phase_B(c):
    nc, p, sb, T, NT, NCH = c.nc, c.p, c.sb, c.T, c.NT, c.NCH
    p.barrier()
    sb.reset(c.persist_mark)
    cf, cbf = c.cstf, c.cb
    idb = cbf.t[:, C_ID:C_ID + 128]
    idf = cf.t[:, C_ID:C_ID + 128]
    trif = cf.t[:, C_TRI:C_TRI + 128]
    blk = cf.t[:, C_BLK:C_BLK + 256]
    U = [sb.tile([128, NCH, 256], F32, "Uall%d" % m) for m in range(2)]
    Ub = [[Buf() for _ in range(NCH)] for m in range(2)]
    dall = sb.tile([128, NCH], F32, "dall")
    Pp = sb.tile([128, NCH], F32, "Pp")
    vpad = [sb.tile([128, 4, 128], BF16, "vpad%d" % m) for m in range(2)]
    for m in range(2):
        p.memset("pool", vpad[m].t[:], 0.0, writes=[vpad[m].b])
    inT = [sb.tile([128, 4, 128], BF16, "inT") for _ in range(2)]
    la = [sb.tile([128, 128], F32, "laB") for _ in range(2)]
    vN = [sb.tile([128, 512], BF16, "vNB") for _ in range(2)]
    laN = sb.tile([128, 128], F32, "laN")
    eneg = sb.tile([128, 128], F32, "eneg")
    epos = sb.tile([128, 128], F32, "epos")
    kt = sb.tile([128, 128], BF16, "kt")
    qtl = [sb.tile([128, 128], BF16, "qtl") for _ in range(2)]
    qtm = sb.tile([128, 4, 128], BF16, "qtm")
    ktN = sb.tile([128, 128], BF16, "ktN")
    Am = [sb.tile([128, 512], BF16, "Am%d" % m) for m in range(2)]
    oev = [sb.tile([128, 2, 128], F32, "oev") for _ in range(4)]
    rqm = sb.tile([128, 4, 128], BF16, "rqm")
    qdec = [sb.tile([128, 128], BF16, "qdec") for _ in range(2)]
    kdN = sb.tile([128, 128], BF16, "kdN")
    stmp = sb.tile([128, 256], F32, "stmp")
    mB1 = sb.mark()

    def load_B(n):
        if n < NCH:
            tk = n * 128
            p.dma(inT[n % 2].t[:], c.dT[:, 3:7, tk:tk + 128], writes=[inT[n % 2].b])
            p.dma(la[n % 2].t[:], c.logaT[:, tk:tk + 128], writes=[la[n % 2].b])
            p.dma(vN[n % 2].t[:], c.dN[tk:tk + 128, :], writes=[vN[n % 2].b])
    load_B(0)
    for n in range(NCH):
        tok = n * 128
        inT_, la_, vN_ = inT[n % 2], la[n % 2], vN[n % 2]
        load_B(n + 1)
        psl = c.nps()
        p.tr(psl.t[:, 0:128], la_.t[:], idf, reads=[la_.b, cf.b], writes=[psl.b])
        p.cp("act", laN.t[:], psl.t[:, 0:128], reads=[psl.b], writes=[laN.b])
        psB = c.nps()
        p.mm(psB.t[:, 0:128], laN.t[:], trif, True, True, reads=[laN.b, cf.b], writes=[psB.b])
        p.act(eneg.t[:], psB.t[:, 0:128], AF.Exp, reads=[psB.b], writes=[eneg.b], scale=-1.0)
        p.act(epos.t[:], psB.t[:, 0:128], AF.Exp, reads=[psB.b], writes=[epos.b])
        p.cp("dve", dall.t[:, n:n + 1], epos.t[:, 127:128], reads=[epos.b], writes=[dall.b])
        p.tt("dve", kt.t[:], inT_.t[:, 1, :], eneg.t[:], ALU.mult, reads=[inT_.b, eneg.b], writes=[kt.b])
        q_ = qtl[n % 2]
        p.tt("pool", q_.t[:], inT_.t[:, 0, :], epos.t[:], ALU.mult, reads=[inT_.b, epos.b], writes=[q_.b])
        p.dma(c.qt[0, :, tok:tok + 128], q_.t[:], reads=[q_.b])
        for h in range(4):
            p.stt("dve" if h % 2 == 0 else "pool", qtm.t[:, h, :], inT_.t[:, 0, :], cf.t[:, C_HM + h:C_HM + h + 1], epos.t[:],
                  ALU.mult, ALU.mult, reads=[inT_.b, epos.b, cf.b], writes=[qtm.b])
        p.tr(c.psb.t[:, 0:128], kt.t[:], idb, reads=[kt.b, cbf.b], writes=[c.psb.b])
        p.cp("act", ktN.t[:], c.psb.t[:, 0:128], reads=[c.psb.b], writes=[ktN.b])
        psU = c.nps()
        p.mm(psU.t[:, 0:256], ktN.t[:], vN_.t[:, 0:256], True, True, reads=[ktN.b, vN_.b], writes=[psU.b])
        p.stt("dve", U[0].t[:, n, :], psU.t[:, 0:256], dall.t[:, n:n + 1], blk, ALU.mult, ALU.mult,
              reads=[psU.b, dall.b, cf.b], writes=[Ub[0][n]])
        psA = c.nps()
        for h in range(4):
            p.mm(psA.t[:, h * 128:(h + 1) * 128], kt.t[:], qtm.t[:, h, :], True, True, reads=[kt.b, qtm.b], writes=[psA.b])
        p.tt("dve", Am[0].t[:], psA.t[:], c.tri4.t[:], ALU.mult, reads=[psA.b, c.tri4.b], writes=[Am[0].b])
        for h in range(4):
            p.ts("pool" if h % 2 == 0 else "dve", rqm.t[:, h, :], inT_.t[:, 2, :], cf.t[:, C_HM + h:C_HM + h + 1], None, ALU.mult,
                 reads=[inT_.b, cf.b], writes=[rqm.b])
        qd_ = qdec[n % 2]
        p.tt("pool", qd_.t[:], inT_.t[:, 2, :], cf.t[:, C_QDT:C_QDT + 128], ALU.mult, reads=[inT_.b, cf.b], writes=[qd_.b])
        p.dma(c.qt[1, :, tok:tok + 128], qd_.t[:], reads=[qd_.b])
        p.tr(c.psb.t[:, 128:256], inT_.t[:, 3, :], idb, reads=[inT_.b, cbf.b], writes=[c.psb.b])
        p.tt("dve", kdN.t[:], c.psb.t[:, 128:256], cf.t[:, C_KDN:C_KDN + 128], ALU.mult, reads=[c.psb.b, cf.b], writes=[kdN.b])
        psU2 = c.nps()
        p.mm(psU2.t[:, 0:256], kdN.t[:], vN_.t[:, 256:512], True, True, reads=[kdN.b, vN_.b], writes=[psU2.b])
        p.tt("dve", U[1].t[:, n, :], psU2.t[:, 0:256], blk, ALU.mult, reads=[psU2.b, cf.b], writes=[Ub[1][n]])
        psA2 = c.nps()
        for h in range(4):
            p.mm(psA2.t[:, h * 128:(h + 1) * 128], inT_.t[:, 3, :], rqm.t[:, h, :], True, True, reads=[inT_.b, rqm.b], writes=[psA2.b])
        p.tt("dve", Am[1].t[:], psA2.t[:], cf.t[:, C_RDT:C_RDT + 512], ALU.mult, reads=[psA2.b, cf.b], writes=[Am[1].b])
        for m in range(2):
            vp = vpad[m]
            src = vN_.t[:, m * 256:(m + 1) * 256].rearrange("p (a two d) -> p a two d", a=2, two=2)
            dst = vp.t[:].rearrange("p (a two) d -> p a two d", two=2)
            p.cp("pool", dst[:, :, 0, 0:64], src[:, :, 0, :], reads=[vN_.b], writes=[vp.b])
            p.cp("pool", dst[:, :, 1, 64:128], src[:, :, 1, :], reads=[vN_.b], writes=[vp.b])
            psO = c.nps()
            for pr in range(2):
                for hh in range(2):
                    h = 2 * pr + hh
                    p.mm(psO.t[:, pr * 128:(pr + 1) * 128], vp.t[:, h, :], Am[m].t[:, h * 128:(h + 1) * 128], hh == 0, hh == 1,
                         reads=[vp.b, Am[m].b], writes=[psO.b])
            o_ = oev[(2 * n + m) % 4]
            p.cp("act", o_.t[:], r3(psO.t[:, 0:256], a=2), reads=[psO.b], writes=[o_.b])
            p.dma(c.oint[m, :, :, tok:tok + 128], o_.t[:], reads=[o_.b])

    if c.stop == "B1":
        return
    p.memset("dve", Pp.t[:, 0:1], 1.0, writes=[Pp.b])
    for n in range(1, NCH):
        p.stt("dve", U[0].t[:, n, :], U[0].t[:, n - 1, :], dall.t[:, n:n + 1], U[0].t[:, n, :], ALU.mult, ALU.add,
              reads=[Ub[0][n - 1], dall.b], writes=[Ub[0][n]])
        p.stt("dve", U[1].t[:, n, :], U[1].t[:, n - 1, :], cf.t[:, C_RP + 1:C_RP + 2], U[1].t[:, n, :], ALU.mult, ALU.add,
              reads=[Ub[1][n - 1], cf.b], writes=[Ub[1][n]])
        p.tt("dve", Pp.t[:, n:n + 1], Pp.t[:, n - 1:n], dall.t[:, n - 1:n], ALU.mult, reads=[dall.b], writes=[Pp.b])
    p.dma(c.sx[:, 0:256], U[0].t[:, NCH - 1, :], reads=[Ub[0][NCH - 1]])
    p.dma(c.sx[:, 256:512], U[1].t[:, NCH - 1, :], reads=[Ub[1][NCH - 1]])
    if c.stop == "B2":
        return
    exchange(c, c.sx, c.sgat)
    if c.stop == "B2x":
        return
    sb.reset(mB1)
    sin = sb.tile([128, 512], F32, "sin")
    p.dma(sin.t[:], c.sgat[0:128, :], writes=[sin.b])
    sinf = sb.tile([128, 512], F32, "sinf")
    p.ts("dve", sinf.t[:], sin.t[:], c.flg.t[:, 0:1], None, ALU.mult, reads=[sin.b, c.flg.b], writes=[sinf.b])
    qT = [sb.tile([128, 512], BF16, "qTB") for _ in range(2)]
    oi = [sb.tile([128, 2, 512], F32, "oiB") for _ in range(2)]
    gt = [sb.tile([128, 2, 512], BF16, "gtB") for _ in range(2)]
    Sp = [sb.tile([128, 256], BF16, "Sp") for _ in range(4)]
    of = [sb.tile([128, 512], F32, "ofB") for _ in range(2)]
    sqo = [sb.tile([128, 512], BF16, "sqoB") for _ in range(2)]
    rs = [sb.tile([128, 512], F32, "rsB") for _ in range(2)]
    of2 = [sb.tile([128, 512], F32, "of2B") for _ in range(2)]
    yo = [sb.tile([128, 512], BF16, "yoB") for _ in range(2)]
    k = 0
    kk = 0
    def load_B3(kx):
        if kx < 2 * NT:
            tt_, m_ = kx // 2, kx % 2
            a0 = tt_ * 512
            p.dma(qT[kx % 2].t[:], c.qt[m_, :, a0:a0 + 512], writes=[qT[kx % 2].b])
            p.dma(oi[kx % 2].t[:], c.oint[m_, :, :, a0:a0 + 512], writes=[oi[kx % 2].b])
            p.dma(gt[kx % 2].t[:], c.dT[:, 7 + 2 * m_:9 + 2 * m_, a0:a0 + 512], writes=[gt[kx % 2].b])
    load_B3(0)
    for tt in range(NT):
        t0 = tt * 512
        for m in range(2):
            qT_, oi_, gt_ = qT[k % 2], oi[k % 2], gt[k % 2]
            k += 1
            load_B3(k)
            psI = [c.nps(), c.nps()]
            for cch in range(4):
                n = 4 * tt + cch
                Sp_ = Sp[(4 * k + cch) % 4]
                if m == 0:
                    psc, rd = Pp.t[:, n:n + 1], [Pp.b]
                else:
                    psc, rd = cf.t[:, C_RP + n:C_RP + n + 1], [cf.b]
                if n > 0:
                    prev, rd2 = U[m].t[:, n - 1, :], [Ub[m][n - 1]]
                else:
                    prev, rd2 = c.zero.t[:], [c.zero.b]
                p.stt("dve" if cch % 2 == 0 else "pool", Sp_.t[:], sinf.t[:, m * 256:(m + 1) * 256], psc, prev, ALU.mult, ALU.add,
                      reads=[sinf.b] + rd + rd2, writes=[Sp_.b])
                for pr in range(2):
                    p.mm(psI[pr].t[:, cch * 128:(cch + 1) * 128], Sp_.t[:, pr * 128:(pr + 1) * 128], qT_.t[:, cch * 128:(cch + 1) * 128],
                         True, True, reads=[Sp_.b, qT_.b], writes=[psI[pr].b])
            for pr in range(2):
                of_, sq_, rs_, of2_, yo_ = of[kk % 2], sqo[kk % 2], rs[kk % 2], of2[kk % 2], yo[kk % 2]
                kk += 1
                p.tt("dve", of_.t[:], psI[pr].t[:], oi_.t[:, pr, :], ALU.add, reads=[psI[pr].b, oi_.b], writes=[of_.b])
                p.act(sq_.t[:], of_.t[:], AF.Square, reads=[of_.b], writes=[sq_.b])
                pss = c.nps()
                p.mm(pss.t[:], cbf.t[:, C_B64:C_B64 + 128], sq_.t[:], True, True, reads=[cbf.b, sq_.b], writes=[pss.b])
                rstd_from_ss(c, rs_.t[:], pss.t[:], 64, [pss.b], [rs_.b])
                p.tt("dve", of2_.t[:], of_.t[:], rs_.t[:], ALU.mult, reads=[of_.b, rs_.b], writes=[of2_.b])
                p.tt("pool", yo_.t[:], of2_.t[:], gt_.t[:, pr, :], ALU.mult, reads=[of2_.b, gt_.b], writes=[yo_.b])
                p.dma(c.yT[:, 4 + 2 * m + pr, t0:t0 + 512], yo_.t[:], reads=[yo_.b])


def phase_C(c):
    nc, p, sb, T, NT, NCH = c.nc, c.p, c.sb, c.T, c.NT, c.NCH
    li, vo = c.li, c.vo
    p.barrier()
    sb.reset(c.persist_mark)
    vec, cf, cbf = c.vecs, c.cstf, c.cb
    idb = cbf.t[:, C_ID:C_ID + 128]
    maskb = cbf.t[:, C_MASKT:C_MASKT + 128]
    NKT = 2 * T // 512
    NKB = 2 * T // 128
    gq = sb.tile([128, 2, 3], F32, "gqC")
    p.ts("dve", gq.t[:, 0, :], vec.t[:, vo + V_QN:vo + V_QN + 3], SCALE_MLA, None, ALU.mult, reads=[vec.b], writes=[gq.b])
    p.ts("dve", gq.t[:, 1, :], vec.t[:, vo + V_QN:vo + V_QN + 3], -SCALE_MLA, None, ALU.mult, reads=[vec.b], writes=[gq.b])
    wq = sb.tile([128, 3, 1024], BF16, "wq")
    wkv = sb.tile([128, 2, 1024], BF16, "wkv")
    stg = [sb.tile([128, 1024], F32, "stgC") for _ in range(2)]
    alt = Alt(["act", "dve"])
    for k in range(3):
        st = stg[k % 2]
        p.dma(st.t[:, 0:768], c.w_uq[li, k * 128:(k + 1) * 128, :], writes=[st.b])
        scaled_copy(c, alt(), wq.t[:, k, 0:768], st.t[:, 0:768], gq.t[:, 0, k:k + 1], [st.b, gq.b], [wq.b])
        for h in range(4):
            s0 = h * 192 + 128
            scaled_copy(c, alt(), wq.t[:, k, 768 + h * 64:768 + h * 64 + 32], st.t[:, s0 + 32:s0 + 64], gq.t[:, 1, k:k + 1], [st.b, gq.b], [wq.b])
            scaled_copy(c, alt(), wq.t[:, k, 768 + h * 64 + 32:768 + h * 64 + 64], st.t[:, s0:s0 + 32], gq.t[:, 0, k:k + 1], [st.b, gq.b], [wq.b])
    for k in range(2):
        st = stg[(k + 1) % 2]
        p.dma(st.t[:], c.w_ukv[li, k * 128:(k + 1) * 128, :], writes=[st.b])
        scaled_copy(c, alt(), wkv.t[:, k, :], st.t[:], vec.t[:, vo + V_KVN + k:vo + V_KVN + k + 1], [st.b, vec.b], [wkv.b])
    cq = sb.tile([128, 3, T], BF16, "cqC")
    ckv = sb.tile([128, 2, 2 * T], BF16, "ckvC")
    KrT = sb.tile([64, 2 * T], BF16, "KrT")
    PC = 1024 if T % 1024 == 0 else 512
    for a0 in range(0, T, PC):
        p.dma(cq.t[:, :, a0:a0 + PC], c.dT[:, 0:3, a0:a0 + PC], writes=[cq.b])
        p.dma(ckv.t[:, :, a0:a0 + PC], r3(c.xg1[0:128, :], a=2)[:, :, a0:a0 + PC], writes=[ckv.b])
        p.dma(ckv.t[:, :, T + a0:T + a0 + PC], r3(c.xb1[:, :], a=2)[:, :, a0:a0 + PC], writes=[ckv.b])
        p.dma(KrT.t[:, a0:a0 + PC], c.xg2[0:64, a0:a0 + PC], writes=[KrT.b])
        p.dma(KrT.t[:, T + a0:T + a0 + PC], c.xb2[0:64, a0:a0 + PC], writes=[KrT.b])
    KnT = sb.tile([128, 2 * T], BF16, "KnT")
    V = sb.tile([128, NKB, 128], BF16, "V")
    QnT = sb.tile([128, T], BF16, "QnT")
    QrT = sb.tile([64, T], BF16, "QrT")
    sqa = [sb.tile([128, 512], BF16, "sqa") for _ in range(2)]
    rpt = [sb.tile([64, 2, 512], F32, "rptC") for _ in range(2)]
    ta = [sb.tile([64, 512], F32, "taC") for _ in range(2)]
    tb = [sb.tile([64, 512], F32, "tbC") for _ in range(2)]
    mx = sb.tile([128, 4, max(NKT, NT)], F32, "mxC")
    red = sb.tile([128, 8], F32, "redC")
    bias = sb.tile([128, NT], F32, "biasC")
    PT = [sb.tile([128, 512], BF16, "PT") for _ in range(6)]
    rden = [sb.tile([128, 512], F32, "rden") for _ in range(2)]
    uu = [sb.tile([128, 512], F32, "uu") for _ in range(2)]
    squ = [sb.tile([128, 512], BF16, "squ") for _ in range(2)]
    rsu = [sb.tile([128, 512], F32, "rsu") for _ in range(2)]
    yo = [sb.tile([128, 512], BF16, "yoC") for _ in range(2)]
    k_ = {"sq": 0, "pt": 0, "s": 0, "e": 0}

    def nsq():
        t = sqa[k_["sq"] % 2]
        k_["sq"] += 1
        return t

    def rmax(dst_ap, ps, M, rd, wr):
        p.op("dve", lambda e: e.tensor_reduce(dst_ap, ps.t[0:M, :] if M < 128 else ps.t[:], AX.X, ALU.max), reads=rd, writes=wr)

    for kt in range(NKT):
        s_ = nsq()
        p.tt("pool", s_.t[0:64, :], KrT.t[:, kt * 512:(kt + 1) * 512], KrT.t[:, kt * 512:(kt + 1) * 512], ALU.mult, reads=[KrT.b], writes=[s_.b])
        ps = c.nps()
        p.mm(ps.t[:], c.ones.t[0:64, :], s_.t[0:64, :], True, True, reads=[c.ones.b, s_.b], writes=[ps.b])
        rmax(mx.t[:, 1, kt:kt + 1], ps, 128, [ps.b], [mx.b])
    p.op("dve", lambda e: e.tensor_reduce(red.t[:, 1:2], mx.t[:, 1, 0:NKT], AX.X, ALU.max), reads=[mx.b], writes=[red.b])

    for h in range(4):
        for kt in range(NKT):
            ps = c.nps()
            for k in range(2):
                p.mm(ps.t[:], wkv.t[:, k, h * 256:h * 256 + 128], ckv.t[:, k, kt * 512:(kt + 1) * 512], k == 0, k == 1,
                     reads=[wkv.b, ckv.b], writes=[ps.b])
            p.cp("act", KnT.t[:, kt * 512:(kt + 1) * 512], ps.t[:], reads=[ps.b], writes=[KnT.b])
            s_ = nsq()
            p.tt("pool", s_.t[:], KnT.t[:, kt * 512:(kt + 1) * 512], KnT.t[:, kt * 512:(kt + 1) * 512], ALU.mult, reads=[KnT.b], writes=[s_.b])
            ps2 = c.nps()
            p.mm(ps2.t[:], c.ones.t[:], s_.t[:], True, True, reads=[c.ones.b, s_.b], writes=[ps2.b])
            rmax(mx.t[:, 0, kt:kt + 1], ps2, 128, [ps2.b], [mx.b])
        p.op("dve", lambda e: e.tensor_reduce(red.t[:, 0:1], mx.t[:, 0, 0:NKT], AX.X, ALU.max), reads=[mx.b], writes=[red.b])
        p.tt("dve", red.t[:, 2:3], red.t[:, 0:1], red.t[:, 1:2], ALU.add, reads=[red.b], writes=[red.b])
        for g in range(NKB // 4):
            ps = c.nps()
            for j in range(4):
                kb = 4 * g + j
                for k in range(2):
                    p.mm(ps.t[:, j * 128:(j + 1) * 128], ckv.t[:, k, kb * 128:(kb + 1) * 128], wkv.t[:, k, h * 256 + 128:(h + 1) * 256],
                         k == 0, k == 1, reads=[wkv.b, ckv.b], writes=[ps.b])
            dst = V.t[:, 4 * g:4 * g + 4, :]
            if 4 * g < NCH:
                p.ts("dve", dst, r3(ps.t[:], a=4), c.flg.t[:, 0:1], None, ALU.mult, reads=[ps.b, c.flg.b], writes=[V.b])
            else:
                p.cp("act" if g % 2 else "dve", dst, r3(ps.t[:], a=4), reads=[ps.b], writes=[V.b])
        for tt in range(NT):
            t0 = tt * 512
            rp_ = rpt[tt % 2]
            p.dma(rp_.t[:], c.rope[0:64, 0:2, t0:t0 + 512], writes=[rp_.b])
            ps = c.nps()
            for k in range(3):
                p.mm(ps.t[:], wq.t[:, k, h * 192:h * 192 + 128], cq.t[:, k, t0:t0 + 512], k == 0, k == 2, reads=[wq.b, cq.b], writes=[ps.b])
            p.cp("act", QnT.t[:, t0:t0 + 512], ps.t[:], reads=[ps.b], writes=[QnT.b])
            psr, pst = c.nps(), c.nps()
            for k in range(3):
                p.mm(psr.t[0:64, :], wq.t[:, k, h * 192 + 128:h * 192 + 192], cq.t[:, k, t0:t0 + 512], k == 0, k == 2, reads=[wq.b, cq.b], writes=[psr.b])
            for k in range(3):
                p.mm(pst.t[0:64, :], wq.t[:, k, 768 + h * 64:768 + h * 64 + 64], cq.t[:, k, t0:t0 + 512], k == 0, k == 2, reads=[wq.b, cq.b], writes=[pst.b])
            a_, b_ = ta[tt % 2], tb[tt % 2]
            p.tt("dve", a_.t[:], psr.t[0:64, :], rp_.t[:, 0, :], ALU.mult, reads=[psr.b, rp_.b], writes=[a_.b])
            p.tt("dve", b_.t[:], pst.t[0:64, :], rp_.t[:, 1, :], ALU.mult, reads=[pst.b, rp_.b], writes=[b_.b])
            p.tt("pool", QrT.t[:, t0:t0 + 512], a_.t[:], b_.t[:], ALU.add, reads=[a_.b, b_.b], writes=[QrT.b])
            s_ = nsq()
            p.tt("pool", s_.t[:], QnT.t[:, t0:t0 + 512], QnT.t[:, t0:t0 + 512], ALU.mult, reads=[QnT.b], writes=[s_.b])
            ps2 = c.nps()
            p.mm(ps2.t[:], c.ones.t[:], s_.t[:], True, True, reads=[c.ones.b, s_.b], writes=[ps2.b])
            rmax(mx.t[:, 2, tt:tt + 1], ps2, 128, [ps2.b], [mx.b])
            s2_ = nsq()
            p.tt("pool", s2_.t[0:64, :], QrT.t[:, t0:t0 + 512], QrT.t[:, t0:t0 + 512], ALU.mult, reads=[QrT.b], writes=[s2_.b])
            ps3 = c.nps()
            p.mm(ps3.t[:], c.ones.t[0:64, :], s2_.t[0:64, :], True, True, reads=[c.ones.b, s2_.b], writes=[ps3.b])
            rmax(mx.t[:, 3, tt:tt + 1], ps3, 128, [ps3.b], [mx.b])
        p.tt("dve", bias.t[:, 0:NT], mx.t[:, 2, 0:NT], mx.t[:, 3, 0:NT], ALU.add, reads=[mx.b], writes=[bias.b])
        p.ts("dve", bias.t[:, 0:NT], bias.t[:, 0:NT], red.t[:, 2:3], None, ALU.mult, reads=[bias.b, red.b], writes=[bias.b])
        p.act(bias.t[:, 0:NT], bias.t[:, 0:NT], AF.Ln, reads=[bias.b], writes=[bias.b])
        p.act(bias.t[:, 0:NT], bias.t[:, 0:NT], AF.Exp, reads=[bias.b], writes=[bias.b], scale=0.5)
        p.ts("dve", bias.t[:, 0:NT], bias.t[:, 0:NT], -1.0, None, ALU.mult, reads=[bias.b], writes=[bias.b])

        if h == 0:
            pend = {"f": None}
        for qt in range(NT):
            q0 = qt * 512
            accO, accD = c.ps[k_["e"] % 2], c.ps[2 + k_["e"] % 2]
            blocks = [(kb, -1) for kb in range(NCH)] + [(NCH + kb, (kb - 4 * qt) if kb >= 4 * qt else -1) for kb in range(4 * qt + 4)]
            nb = len(blocks)
            stiles = [None] * nb

            G = 2
            groups = [list(range(a, min(a + G, nb))) for a in range(0, nb, G)]

            def emit_Sg(g):
                idxs = groups[g]
                banks = []
                for j_, i in enumerate(idxs):
                    S = c.ps[4 + 2 * (g % 2) + j_]
                    stiles[i] = S
                    banks.append(S.b)
                first = True
                for i in idxs:
                    kb, dj = blocks[i]
                    S = stiles[i]
                    lo = 0 if dj < 0 else 128 * dj
                    p.mm(S.t[:, lo:512], KnT.t[:, kb * 128:(kb + 1) * 128], QnT.t[:, q0 + lo:q0 + 512], True, False,
                         reads=[KnT.b, QnT.b], writes=(banks if first else [S.b]))
                    first = False
                    p.mm(S.t[:, lo:512], KrT.t[:, kb * 128:(kb + 1) * 128], QrT.t[:, q0 + lo:q0 + 512], False, dj < 0,
                         reads=[KrT.b, QrT.b], writes=[S.b])
                    if dj >= 0:
                        p.mm(S.t[:, lo:lo + 128], idb, maskb, False, True, reads=[cbf.b], writes=[S.b])

            def emit_PVg(g):
                idxs = groups[g]
                Ps = []
                for i in idxs:
                    kb, dj = blocks[i]
                    S = stiles[i]
                    lo = 0 if dj < 0 else 128 * dj
                    P_ = PT[k_["pt"] % 6]
                    k_["pt"] += 1
                    Ps.append(P_)
                    p.act(P_.t[:, lo:512], S.t[:, lo:512], AF.Exp, reads=[S.b, bias.b], writes=[P_.b], bias=bias.t[:, qt:qt + 1], scale=1.0)
                first = True
                for i, P_ in zip(idxs, Ps):
                    kb, dj = blocks[i]
                    lo = 0 if dj < 0 else 128 * dj
                    on = c.fones if kb < NCH else c.ones
                    rd = [V.b, on.b] + ([x.b for x in Ps] if first else [P_.b])
                    first = False
                    p.mm(accO.t[:, lo:512], V.t[:, kb, :], P_.t[:, lo:512], i == 0, i == nb - 1, reads=rd, writes=[accO.b])
                    p.mm(accD.t[:, lo:512], on.t[:], P_.t[:, lo:512], i == 0, i == nb - 1, reads=[on.b, P_.b], writes=[accD.b])

            emit_Sg(0)
            for g in range(len(groups)):
                if g + 1 < len(groups):
                    emit_Sg(g + 1)
                emit_PVg(g)
                if g == 3 and pend["f"] is not None:
                    pend["f"]()
                    pend["f"] = None
            e = k_["e"]
            k_["e"] += 1
            rd_, u_, sq_, rs_, yo_ = rden[e % 2], uu[e % 2], squ[e % 2], rsu[e % 2], yo[e % 2]
            p.op("dve", lambda e_, rd_=rd_, accD=accD: e_.reciprocal(rd_.t[:], accD.t[:]), reads=[accD.b], writes=[rd_.b])
            p.tt("dve", u_.t[:], accO.t[:], rd_.t[:], ALU.mult, reads=[accO.b, rd_.b], writes=[u_.b])
            p.tt("pool", sq_.t[:], u_.t[:], u_.t[:], ALU.mult, reads=[u_.b], writes=[sq_.b])

            def part2(u_=u_, sq_=sq_, rs_=rs_, yo_=yo_, h=h, q0=q0):
                pss = c.ps[6]
                p.mm(pss.t[:], c.ones.t[:], sq_.t[:], True, True, reads=[c.ones.b, sq_.b], writes=[pss.b])
                p.act(rs_.t[:], pss.t[:], AF.Ln, reads=[pss.b], writes=[rs_.b], bias=EPS, scale=1.0 / 128)
                p.act(rs_.t[:], rs_.t[:], AF.Exp, reads=[rs_.b], writes=[rs_.b], scale=-0.5)
                p.tt("pool", yo_.t[:], u_.t[:], rs_.t[:], ALU.mult, reads=[u_.b, rs_.b], writes=[yo_.b])
                p.dma(c.yT[:, h, q0:q0 + 512], yo_.t[:], reads=[yo_.b])
            pend["f"] = part2
    if pend["f"] is not None:
        pend["f"]()
        pend["f"] = None


def phase_D(c, xin, last):
    nc, p, sb, T, NT, NCH = c.nc, c.p, c.sb, c.T, c.NT, c.NCH
    li, vo = c.li, c.vo
    p.barrier()
    sb.reset(c.persist_mark)
    vec, cf, cbf = c.vecs, c.cstf, c.cb
    idb = cbf.t[:, C_ID:C_ID + 128]
    wo = sb.tile([128, 8, D], BF16, "wo")
    wu = sb.tile([128, 8, 2 * DFF], BF16, "wu")
    mW = sb.mark()
    stg = [sb.tile([128, DFF], F32, "stgD") for _ in range(3)]
    alt = Alt(["act", "dve"])
    for k in range(8):
        st = stg[k % 3]
        p.dma(st.t[:, 0:D], c.w_out[li, k * 128:(k + 1) * 128, :], writes=[st.b])
        scaled_copy(c, alt(), wo.t[:, k, :], st.t[:, 0:D], vec.t[:, vo + V_ON + k:vo + V_ON + k + 1], [st.b, vec.b], [wo.b])
    pieces = [(k, hf) for hf in range(2) for k in range(8)]
    wub = [[Buf() for _ in range(2)] for _ in range(8)]

    def piece_dma(j):
        if j < len(pieces):
            k, hf = pieces[j]
            st = stg[j % 3]
            p.dma(st.t[:], c.w_up[li, k * 128:(k + 1) * 128, hf * DFF:(hf + 1) * DFF], writes=[st.b])

    def piece_cvt(j):
        if j < len(pieces):
            k, hf = pieces[j]
            st = stg[j % 3]
            scaled_copy(c, alt(), wu.t[:, k, hf * DFF:(hf + 1) * DFF], st.t[:], vec.t[:, vo + V_FN + k:vo + V_FN + k + 1], [st.b, vec.b], [wub[k][hf]])
    piece_dma(0)
    piece_dma(1)
    pj = {"j": 0}

    def piece_step():
        j = pj["j"]
        if j < len(pieces):
            piece_dma(j + 2)
            piece_cvt(j)
            pj["j"] += 1
    wu_a = [wub[k][0] for k in range(8)]
    wu_all = [wub[k][hf] for k in range(8) for hf in range(2)]
    yt = [sb.tile([128, 8, 512], BF16, "ytD") for _ in range(2)]
    xt = [sb.tile([128, D], F32, "xtD") for _ in range(2)]
    xm = [sb.tile([128, D], F32, "xmD") for _ in range(2)]
    junk = sb.tile([128, D], BF16, "junkD")
    ssq = [sb.tile([128, 2], F32, "ssqD") for _ in range(2)]
    hN = [sb.tile([128, D], BF16, "hND") for _ in range(2)]
    hTt = [sb.tile([128, 8, 512], BF16, "hTD") for _ in range(2)]
    i = 0

    def load_x1(ix):
        if ix < 4 * NT:
            p.dma(xt[ix % 2].t[:], xin[ix * 128:(ix + 1) * 128, :], writes=[xt[ix % 2].b])

    def load_y1(tx):
        if tx < NT:
            p.dma(yt[tx % 2].t[:], c.yT[:, :, tx * 512:(tx + 1) * 512], writes=[yt[tx % 2].b])
    load_y1(0)
    load_x1(0)
    for tt in range(NT):
        t0 = tt * 512
        yt_, hT_ = yt[tt % 2], hTt[tt % 2]
        load_y1(tt + 1)
        for s in range(4):
            x_, xm_, ss_, hN_ = xt[i % 2], xm[i % 2], ssq[i % 2], hN[i % 2]
            i += 1
            r0 = t0 + s * 128
            load_x1(i)
            for hf in range(2):
                ps = c.nps()
                for k in range(8):
                    p.mm(ps.t[:], yt_.t[:, k, s * 128:(s + 1) * 128], wo.t[:, k, hf * 512:(hf + 1) * 512], k == 0, k == 7,
                         reads=[yt_.b, wo.b], writes=[ps.b])
                p.tt("dve", xm_.t[:, hf * 512:(hf + 1) * 512], ps.t[:], x_.t[:, hf * 512:(hf + 1) * 512], ALU.add, reads=[ps.b, x_.b], writes=[xm_.b])
            p.dma(c.xres[r0:r0 + 128, :], xm_.t[:], reads=[xm_.b])
            p.act(junk.t[:], xm_.t[:], AF.Square, reads=[xm_.b], writes=[junk.b, ss_.b], accum=ss_.t[:, 0:1])
            rstd_from_ss(c, ss_.t[:, 1:2], ss_.t[:, 0:1], D, [ss_.b], [ss_.b])
            p.act(hN_.t[:], xm_.t[:], AF.Copy, reads=[xm_.b, ss_.b], writes=[hN_.b], scale=ss_.t[:, 1:2])
            for k in range(8):
                p.tr(c.psb.t[:, k * 128:(k + 1) * 128], hN_.t[:, k * 128:(k + 1) * 128], idb, reads=[hN_.b, cbf.b], writes=[c.psb.b])
            p.cp("pool" if False else "dve", hT_.t[:, :, s * 128:(s + 1) * 128], r3(c.psb.t[:], a=8), reads=[c.psb.b], writes=[hT_.b])
            piece_step()
        p.dma(c.hT[:, :, t0:t0 + 512], hT_.t[:], reads=[hT_.b])
    while pj["j"] < len(pieces):
        piece_step()
    if c.stop == "D1":
        return
    hl = hTt[(NT - 1) % 2]
    ps = c.nps()
    for cc in range(NFC):
        for k in range(8):
            p.mm(ps.t[:, 2 * cc:2 * cc + 2], wu.t[:, k, cc * 128:(cc + 1) * 128], hl.t[:, k, 510:512], k == 0, k == 7,
                 reads=[wub[k][0], hl.b], writes=[ps.b])
    hout = sb.tile([128, 2 * NFC], F32, "hout")
    p.cp("act", hout.t[:], ps.t[:, 0:2 * NFC], reads=[ps.b], writes=[hout.b])
    p.dma(c.halo[:, :], hout.t[:], reads=[hout.b])
    exchange(c, c.halo, c.hgat)
    if c.stop == "Dh":
        return
    sb.reset(mW)
    hin = sb.tile([128, 2 * NFC], F32, "hin")
    p.dma(hin.t[:], c.hgat[0:128, :], writes=[hin.b])
    hal = sb.tile([128, NFC, 2], F32, "hal")
    halb = [Buf() for _ in range(NFC)]
    p.ts("dve", hal.t[:], r3(hin.t[:], a=NFC), c.flg.t[:, 0:1], None, ALU.mult, reads=[hin.b, c.flg.b], writes=halb)
    hT2 = [sb.tile([128, 8, 512], BF16, "hT2") for _ in range(2)]
    asb = [sb.tile([128, 514], F32, "asb") for _ in range(3)]
    t1 = [sb.tile([128, 512], F32, "t1D") for _ in range(2)]
    t2 = [sb.tile([128, 512], F32, "t2D") for _ in range(2)]
    t3 = [sb.tile([128, 512], F32, "t3D") for _ in range(2)]
    sl = [sb.tile([128, 512], F32, "slD") for _ in range(2)]
    gTt = [sb.tile([128, NFC, 512], BF16, "gTt") for _ in range(2)]
    cw = lambda k_, cc: vec.t[:, vo + V_CW + k_ * NFC + cc:vo + V_CW + k_ * NFC + cc + 1]
    cbv = lambda cc: vec.t[:, vo + V_CB + cc:vo + V_CB + cc + 1]
    j = 0

    def load_h2(tx):
        if tx < NT:
            p.dma(hT2[tx % 2].t[:], c.hT[:, :, tx * 512:(tx + 1) * 512], writes=[hT2[tx % 2].b])
    load_h2(0)
    for tt in range(NT):
        t0 = tt * 512
        h_ = hT2[tt % 2]
        g_ = gTt[tt % 2]
        load_h2(tt + 1)
        for cc in range(NFC):
            psa, psg = c.nps(), c.nps()
            for k in range(8):
                p.mm(psa.t[:], wu.t[:, k, cc * 128:(cc + 1) * 128], h_.t[:, k, :], k == 0, k == 7, reads=[wub[k][0], h_.b], writes=[psa.b])
            for k in range(8):
                p.mm(psg.t[:], wu.t[:, k, DFF + cc * 128:DFF + (cc + 1) * 128], h_.t[:, k, :], k == 0, k == 7, reads=[wub[k][1], h_.b], writes=[psg.b])
            a_ = asb[j % 3]
            t1_, t2_, t3_, s_ = t1[j % 2], t2[j % 2], t3[j % 2], sl[j % 2]
            j += 1
            p.cp("act", a_.t[:, 2:514], psa.t[:], reads=[psa.b], writes=[a_.b])
            p.cp("pool", a_.t[:, 0:2], hal.t[:, cc, :], reads=[halb[cc]], writes=[a_.b])
            p.cp("pool", hal.t[:, cc, :], a_.t[:, 512:514], reads=[a_.b], writes=[halb[cc]])
            p.ts("pool", t1_.t[:], a_.t[:, 2:514], cw(2, cc), cbv(cc), ALU.mult, ALU.add, reads=[a_.b, vec.b], writes=[t1_.b])
            p.stt("dve", t2_.t[:], a_.t[:, 1:513], cw(1, cc), t1_.t[:], ALU.mult, ALU.add, reads=[a_.b, vec.b, t1_.b], writes=[t2_.b])
            p.stt("dve", t3_.t[:], a_.t[:, 0:512], cw(0, cc), t2_.t[:], ALU.mult, ALU.add, reads=[a_.b, vec.b, t2_.b], writes=[t3_.b])
            p.act(s_.t[:], t3_.t[:], AF.Silu, reads=[t3_.b], writes=[s_.b])
            p.tt("dve", g_.t[:, cc, :], s_.t[:], psg.t[:], ALU.mult, reads=[s_.b, psg.b], writes=[g_.b])
        p.dma(c.gT[:, :, t0:t0 + 512], g_.t[:], reads=[g_.b])
    if c.stop == "D2":
        return
    p.barrier()
    sb.reset(c.persist_mark)
    wd = sb.tile([128, NFC, D], BF16, "wd")
    stg = [sb.tile([128, D], F32, "stgD3") for _ in range(2)]
    for cc in range(NFC):
        st = stg[cc % 2]
        p.dma(st.t[:], c.w_down[li, cc * 128:(cc + 1) * 128, :], writes=[st.b])
        p.cp(alt(), wd.t[:, cc, :], st.t[:], reads=[st.b], writes=[wd.b])
    fnb = None
    if last:
        fnb = sb.tile([128, D], F32, "fnb")
        p.dma(fnb.t[:], c.fnb[:, :], writes=[fnb.b])
    g3 = [sb.tile([128, NFC, 512], BF16, "g3") for _ in range(2)]
    xt = [sb.tile([128, D], F32, "xt3") for _ in range(2)]
    xo = [sb.tile([128, D], F32, "xo3") for _ in range(2)]
    junk = sb.tile([128, D], BF16, "junk3")
    ssq = [sb.tile([128, 2], F32, "ssq3") for _ in range(2)]
    yn = [sb.tile([128, D], F32, "yn3") for _ in range(2)]
    i = 0

    def load_g3(tx):
        if tx < NT:
            p.dma(g3[tx % 2].t[:], c.gT[:, :, tx * 512:(tx + 1) * 512], writes=[g3[tx % 2].b])

    def load_x3(ix):
        if ix < 4 * NT:
            p.dma(xt[ix % 2].t[:], c.xres[ix * 128:(ix + 1) * 128, :], writes=[xt[ix % 2].b])
    load_g3(0)
    load_x3(0)
    for tt in range(NT):
        t0 = tt * 512
        g_ = g3[tt % 2]
        load_g3(tt + 1)
        for s in range(4):
            x_, xo_, ss_, yn_ = xt[i % 2], xo[i % 2], ssq[i % 2], yn[i % 2]
            i += 1
            r0 = t0 + s * 128
            load_x3(i)
            for hf in range(2):
                ps = c.nps()
                for cc in range(NFC):
                    p.mm(ps.t[:], g_.t[:, cc, s * 128:(s + 1) * 128], wd.t[:, cc, hf * 512:(hf + 1) * 512], cc == 0, cc == NFC - 1,
                         reads=[g_.b, wd.b], writes=[ps.b])
                p.tt("dve", xo_.t[:, hf * 512:(hf + 1) * 512], ps.t[:], x_.t[:, hf * 512:(hf + 1) * 512], ALU.add, reads=[ps.b, x_.b], writes=[xo_.b])
            if not last:
                p.dma(c.xres[r0:r0 + 128, :], xo_.t[:], reads=[xo_.b])
            else:
                p.act(junk.t[:], xo_.t[:], AF.Square, reads=[xo_.b], writes=[junk.b, ss_.b], accum=ss_.t[:, 0:1])
                rstd_from_ss(c, ss_.t[:, 1:2], ss_.t[:, 0:1], D, [ss_.b], [ss_.b])
                p.act(yn_.t[:], xo_.t[:], AF.Copy, reads=[xo_.b, ss_.b], writes=[yn_.b], scale=ss_.t[:, 1:2])
                p.tt("pool", yn_.t[:], yn_.t[:], fnb.t[:], ALU.mult, reads=[yn_.b, fnb.b], writes=[yn_.b])
                p.dma(c.out[r0:r0 + 128, :], yn_.t[:], reads=[yn_.b])


_NC_CACHE = {}


def kernel(x, attn_norm, w_in, mla_q_norm, mla_w_uq, mla_kv_norm, mla_w_ukv, mla_out_norm,
           gla_w_gate, gla_b_gate, gla_out_norm, ret_out_norm, w_out, ffn_norm, ffn_w_up,
           ffn_conv_w, ffn_conv_b, ffn_w_down, final_norm):
    f = lambda a: np.ascontiguousarray(np.asarray(a, dtype=np.float32))
    x = f(x)
    B, S, _ = x.shape
    T = S // 2
    nl = int(np.asarray(w_in).shape[0])
    assert B * 2 == 8
    inp = dict(attn_norm=f(attn_norm), ffn_norm=f(ffn_norm), mla_q_norm=f(mla_q_norm), mla_kv_norm=f(mla_kv_norm),
               mla_out_norm=f(mla_out_norm), gla_out_norm=f(gla_out_norm), ret_out_norm=f(ret_out_norm),
               gla_b_gate=f(gla_b_gate), ffn_conv_w=f(ffn_conv_w), ffn_conv_b=f(ffn_conv_b))
    key = (T, nl)
    if key not in _NC_CACHE:
        _NC_CACHE[key] = build(T, nl)
    nc = _NC_CACHE[key]
    cst = host_consts(T)
    vec = host_vec(inp, 0, nl)
    fnb = np.ascontiguousarray(np.broadcast_to(f(final_norm)[None, :], (128, D)))
    shared = dict(w_in=f(w_in), w_uq=f(mla_w_uq), w_ukv=f(mla_w_ukv), w_out=f(w_out), w_up=f(ffn_w_up),
                  w_down=f(ffn_w_down), w_gate=f(gla_w_gate), vec=vec, cst=cst, fnb=fnb)
    ropes = [host_rope(T, r * T) for r in range(2)]
    in_maps = []
    for core in range(8):
        b, r = core // 2, core % 2
        m = dict(shared)
        m["x"] = np.ascontiguousarray(x[b, r * T:(r + 1) * T])
        m["rope"] = ropes[r]
        m["flag"] = np.full((128, 1), float(r), np.float32)
        in_maps.append(m)
    res = run_bass_kernel_spmd(nc, in_maps, core_ids=list(range(8)))
    out = np.empty((B, S, D), np.float32)
    for core in range(8):
        b, r = core // 2, core % 2
        out[b, r * T:(r + 1) * T] = np.asarray(res.results[core]["out"])
    return out
```

```python
import numpy as np
import ml_dtypes
import concourse.bass as bass
import concourse.mybir as mybir
from concourse.bass_utils import run_bass_kernel_spmd

F32 = mybir.dt.float32
BF16 = mybir.dt.bfloat16
AF = mybir.ActivationFunctionType
ALU = mybir.AluOpType
AX = mybir.AxisListType

D = 1024
DEPTH = 4
EPS = 1e-6
DIN = 2256
DFF = 2816
NFC = DFF // 128
SCALE_MLA = 192 ** -0.5

ENGS = ("pe", "act", "dve", "pool", "sp")
KDMA = 8


class Buf:
    __slots__ = ("lw", "rd", "name")

    def __init__(self, name=""):
        self.lw = None
        self.rd = []
        self.name = name


class Op:
    __slots__ = ("eng", "fn", "deps", "kind", "signal", "tok", "idx", "dma_n")


class Prog:
    def __init__(self, nc):
        self.nc = nc
        self.ops = {e: [] for e in ENGS}
        self.all = []
        self.ndma = {e: 0 for e in ENGS}
        self.last = {e: None for e in ENGS}
        self.pending_barrier = {e: [] for e in ENGS}
        self.dmas = {e: [] for e in ENGS}
        self.lastcc = None

    def op(self, eng, fn, reads=(), writes=(), kind="c"):
        o = Op()
        o.eng, o.fn, o.kind, o.signal, o.tok = eng, fn, kind, False, None
        deps = []
        for b in reads:
            if b.lw is not None:
                deps.append(b.lw)
        for b in writes:
            if b.lw is not None:
                deps.append(b.lw)
            deps.extend(b.rd)
        deps.extend(self.pending_barrier[eng])
        self.pending_barrier[eng] = []
        dd = []
        seen = set()
        for d in deps:
            if id(d) in seen:
                continue
            seen.add(id(d))
            if d.eng == "pe" and eng == "pe" and d.kind == "c" and kind == "c":
                continue
            dd.append(d)
        o.deps = dd
        for b in writes:
            b.lw = o
            b.rd = []
        for b in reads:
            b.rd.append(o)
        if kind == "d":
            o.dma_n = self.ndma[eng]
            self.ndma[eng] += 1
            self.dmas[eng].append(o)
        if kind == "cc":
            if self.lastcc is not None and all(x is not self.lastcc for x in o.deps):
                o.deps.append(self.lastcc)
            self.lastcc = o
            self.dmas[eng].append(o)
        o.idx = len(self.ops[eng])
        self.ops[eng].append(o)
        self.all.append(o)
        self.last[eng] = o
        return o

    def barrier(self):
        lasts = [self.last[e] for e in ENGS if self.last[e] is not None]
        for e in ENGS:
            lasts.extend(self.dmas[e][-(KDMA + 2):])
        for e in ENGS:
            self.pending_barrier[e] = list(lasts)

    def dma(self, out, in_, reads=(), writes=(), eng="sp"):
        return self.op(eng, lambda e: e.dma_start(out=out, in_=in_), reads, writes, kind="d")

    def mm(self, out, lhsT, rhs, start, stop, reads=(), writes=()):
        return self.op("pe", lambda e: e.matmul(out, lhsT, rhs, start=start, stop=stop), reads, writes)

    def tr(self, out, in_, ident, reads=(), writes=()):
        return self.op("pe", lambda e: e.transpose(out, in_, ident), reads, writes)

    def act(self, out, in_, func, reads=(), writes=(), bias=None, scale=None, accum=None):
        def fn(e):
            kw = {}
            if bias is not None:
                kw["bias"] = bias
            if scale is not None:
                kw["scale"] = scale
            if accum is not None:
                kw["accum_out"] = accum
            return e.activation(out, in_, func, **kw)
        return self.op("act", fn, reads, writes)

    def tt(self, eng, out, in0, in1, op, reads=(), writes=()):
        return self.op(eng, lambda e: e.tensor_tensor(out, in0, in1, op), reads, writes)

    def ts(self, eng, out, in0, s1, s2, op0, op1=None, reads=(), writes=()):
        def fn(e):
            if op1 is None:
                return e.tensor_scalar(out, in0, s1, None, op0)
            return e.tensor_scalar(out, in0, s1, s2, op0, op1)
        return self.op(eng, fn, reads, writes)

    def stt(self, eng, out, in0, scalar, in1, op0, op1, reads=(), writes=()):
        eng = "dve"
        return self.op(eng, lambda e: e.scalar_tensor_tensor(out, in0, scalar, in1, op0, op1), reads, writes)

    def cp(self, eng, out, in_, reads=(), writes=()):
        if eng == "act":
            return self.op("act", lambda e: e.copy(out=out, in_=in_), reads, writes)
        return self.op(eng, lambda e: e.tensor_copy(out, in_), reads, writes)

    def memset(self, eng, ap, val, writes=()):
        return self.op(eng, lambda e: e.memset(ap, val), (), writes)

    def emit(self):
        nc = self.nc
        for o in self.all:
            for d in o.deps:
                d.signal = True
        import contextlib
        with contextlib.ExitStack() as es:
            csem = {e: es.enter_context(nc.semaphore("c_" + e)) for e in ("pe", "act", "dve", "pool")}
            dsem = {e: [es.enter_context(nc.semaphore("d_%s%d" % (e, i))) for i in range(KDMA)]
                    for e in ENGS if self.ndma[e] > 0}
            ccsem = es.enter_context(nc.semaphore("ccs"))
            cnt = {e: 0 for e in ENGS}
            ccn = 0
            for e in ENGS:
                for o in self.ops[e]:
                    if o.kind == "d":
                        o.signal = True
                        o.tok = (dsem[e][o.dma_n % KDMA], 16 * (o.dma_n // KDMA + 1), 16)
                    elif o.kind == "cc":
                        ccn += 1
                        o.signal = True
                        o.tok = (ccsem, ccn, 1)
                    elif o.signal:
                        cnt[e] += 1
                        o.tok = (csem[e], cnt[e], 1)
            block = es.enter_context(nc.Block())
            prog = self

            def body(ename):
                def f(eng):
                    waited = {}
                    ops = prog.ops[ename]
                    for o in ops:
                        ws = []
                        for d in o.deps:
                            ws.append((d.tok[0], d.tok[1]))
                        if o.kind == "d" and o.dma_n >= KDMA:
                            ws.append((dsem[ename][o.dma_n % KDMA], 16 * (o.dma_n // KDMA)))
                        for (s, v) in ws:
                            k = s.num
                            if waited.get(k, 0) < v:
                                eng.wait_ge(s, v)
                                waited[k] = v
                        ins = o.fn(eng)
                        if o.signal:
                            ins.then_inc(o.tok[0], o.tok[2])
                    if ename in dsem:
                        n = prog.ndma[ename]
                        for i in range(KDMA):
                            c = (n - i + KDMA - 1) // KDMA
                            if c > 0 and waited.get(dsem[ename][i].num, 0) < 16 * c:
                                eng.wait_ge(dsem[ename][i], 16 * c)
                return f

            block.tensor(body("pe"))
            block.scalar(body("act"))
            block.vector(body("dve"))
            block.gpsimd(body("pool"))
            block.sync(body("sp"))


class TL:
    __slots__ = ("t", "b")

    def __init__(self, t, name=""):
        self.t = t
        self.b = Buf(name)


class SB:
    BASE = 16512
    TOP = 229344

    def __init__(self, nc):
        self.nc = nc
        self.off = SB.BASE
        self.n = 0

    def tile(self, shape, dt, name="t"):
        per = 1
        for s in shape[1:]:
            per *= s
        nbytes = per * (2 if dt == BF16 else 4)
        nbytes = (nbytes + 31) // 32 * 32
        self.n += 1
        t = self.nc.alloc_sbuf_tensor_at("%s_%d" % (name, self.n), list(shape), dt, offset=self.off)
        self.off += nbytes
        assert self.off <= SB.TOP, ("SBUF overflow", name, self.off)
        return TL(t, name)

    def mark(self):
        return self.off

    def reset(self, m):
        self.off = m


C_ID, C_TRI, C_MASKT, C_BLK, C_B64, C_RDT, C_KDN, C_QDT, C_HM = 0, 128, 256, 384, 640, 768, 1280, 1408, 1536
C_RP = 1540


def host_consts(T):
    nch = T // 128
    ncst = C_RP + nch + 1
    c = np.zeros((128, ncst), np.float32)
    r = np.arange(128)
    c[:, C_ID:C_ID + 128] = np.eye(128, dtype=np.float32)
    c[:, C_TRI:C_TRI + 128] = (r[:, None] <= r[None, :]).astype(np.float32)
    c[:, C_MASKT:C_MASKT + 128] = np.where(r[:, None] <= r[None, :], 0.0, -30000.0)
    hh = r // 32
    for h in range(4):
        c[:, C_HM + h] = (hh == h)
    c[:, C_BLK:C_BLK + 256] = (hh[:, None] == (np.arange(256) // 64)[None, :])
    c[:, C_B64:C_B64 + 128] = ((r // 64)[:, None] == (r // 64)[None, :])
    gam = 1.0 - 2.0 ** (-5.0 - np.arange(4, dtype=np.float64))
    lg = np.log(gam)
    for h in range(4):
        dif = r[None, :] - r[:, None]
        c[:, C_RDT + h * 128:C_RDT + (h + 1) * 128] = np.where(dif >= 0, np.exp(lg[h] * np.maximum(dif, 0)), 0.0)
        c[:, C_KDN + h * 32:C_KDN + (h + 1) * 32] = np.exp(lg[h] * (127 - r))[:, None]
    c[:, C_QDT:C_QDT + 128] = np.exp(lg[hh][:, None] * (r[None, :] + 1.0))
    for n in range(nch + 1):
        c[:, C_RP + n] = np.exp(lg[hh] * 128.0 * n)
    return c


def host_rope(T, pos0):
    out = np.zeros((128, 4, T), np.float32)
    pos = (pos0 + np.arange(T)).astype(np.float32)
    inv = (10000.0 ** (-(np.arange(0, 64, 2, dtype=np.float32) / 64))).astype(np.float32)
    ang = pos[None, :] * inv[:, None]
    out[0:32, 0], out[32:64, 0] = np.cos(ang), np.cos(ang)
    out[0:32, 1], out[32:64, 1] = np.sin(ang), np.sin(ang)
    inv2 = (10000.0 ** (-(np.arange(0, 32, 2, dtype=np.float32) / 32))).astype(np.float32)
    ang2 = pos[None, :] * inv2[:, None]
    c2 = np.concatenate([np.cos(ang2), np.cos(ang2)], 0)
    s2 = np.concatenate([np.sin(ang2), np.sin(ang2)], 0)
    out[:, 2] = np.tile(c2, (4, 1))
    out[:, 3] = np.tile(s2, (4, 1))
    return out


V_AN, V_FN, V_QN, V_KVN, V_ON, V_BG, V_CW, V_CB, V_PER = 0, 8, 16, 19, 21, 29, 30, 96, 118


def host_vec(inp, nl0, nl):
    v = np.zeros((128, nl * V_PER), np.float32)

    def cols(a):
        return np.ascontiguousarray(a.reshape(-1, 128).T)
    for i in range(nl):
        l = nl0 + i
        o = i * V_PER
        v[:, o + V_AN:o + V_AN + 8] = cols(inp["attn_norm"][l])
        v[:, o + V_FN:o + V_FN + 8] = cols(inp["ffn_norm"][l])
        v[:, o + V_QN:o + V_QN + 3] = cols(inp["mla_q_norm"][l])
        v[:, o + V_KVN:o + V_KVN + 2] = cols(inp["mla_kv_norm"][l])
        v[:, o + V_ON:o + V_ON + 4] = cols(inp["mla_out_norm"][l])
        v[:, o + V_ON + 4:o + V_ON + 6] = cols(inp["gla_out_norm"][l])
        v[:, o + V_ON + 6:o + V_ON + 8] = cols(inp["ret_out_norm"][l])
        v[:, o + V_BG:o + V_BG + 1] = cols(inp["gla_b_gate"][l])
        for k in range(3):
            v[:, o + V_CW + k * NFC:o + V_CW + (k + 1) * NFC] = cols(inp["ffn_conv_w"][l, k])
        v[:, o + V_CB:o + V_CB + NFC] = cols(inp["ffn_conv_b"][l])
    return v


class Ctx:
    pass


def r3(ap, **kw):
    return ap.rearrange("p (a b) -> p a b", **kw)


def build(T, nl, final_norm=True, dbg=None, ext=(), stop_after=None, stop_layer=0):
    assert T % 512 == 0
    NT = T // 512
    NCH = T // 128
    nc = bass.Bass("TRN2", target_bir_lowering=False)
    p = Prog(nc)
    sb = SB(nc)
    c = Ctx()
    c.nc, c.p, c.sb, c.T, c.NT, c.NCH = nc, p, sb, T, NT, NCH
    c.stop = None

    def din(name, shape, dt=F32):
        return nc.dram_tensor(name, list(shape), dt, kind="ExternalInput")

    def dscr(name, shape, dt):
        if name in ext:
            return nc.dram_tensor(name, list(shape), dt, kind="ExternalOutput")
        return nc.dram_tensor(name, list(shape), dt)

    c.x = din("x", [T, D])
    c.w_in = din("w_in", [nl, D, DIN])
    c.w_uq = din("w_uq", [nl, 384, 768])
    c.w_ukv = din("w_ukv", [nl, 256, 1024])
    c.w_out = din("w_out", [nl, D, D])
    c.w_up = din("w_up", [nl, D, 2 * DFF])
    c.w_down = din("w_down", [nl, DFF, D])
    c.w_gate = din("w_gate", [nl, 16, 128])
    c.vec = din("vec", [128, nl * V_PER])
    ncst = C_RP + NCH + 1
    c.cst = din("cst", [128, ncst])
    c.rope = din("rope", [128, 4, T])
    c.flag = din("flag", [128, 1])
    c.fnb = din("fnb", [128, D])
    c.out = nc.dram_tensor("out", [T, D], F32, kind="ExternalOutput")
    c.xres = dscr("xres", [T, D], F32)
    c.dT = dscr("dT", [128, 11, T], BF16)
    c.logaT = dscr("logaT", [128, T], F32)
    c.dN = dscr("dN", [T, 512], BF16)
    c.xb1 = dscr("xb1", [128, 2 * T], BF16)
    c.xg1 = dscr("xg1", [256, 2 * T], BF16)
    c.xb2 = dscr("xb2", [128, T], BF16)
    c.xg2 = dscr("xg2", [256, T], BF16)
    c.oint = dscr("oint", [2, 128, 2, T], F32)
    c.qt = dscr("qt", [2, 128, T], BF16)
    c.sx = dscr("sx", [128, 512], F32)
    c.sgat = dscr("sgat", [256, 512], F32)
    c.yT = dscr("yT", [128, 8, T], BF16)
    c.hT = dscr("hT", [128, 8, T], BF16)
    c.halo = dscr("halo", [128, 2 * NFC], F32)
    c.hgat = dscr("hgat", [256, 2 * NFC], F32)
    c.gT = dscr("gT", [128, NFC, T], BF16)
    c.dbg = None
    if dbg is not None:
        c.dbg = nc.dram_tensor("dbg", list(dbg), F32, kind="ExternalOutput")

    c.ps = [TL(nc.alloc_psum_tensor("ps%d" % i, [128, 512], F32), "ps%d" % i) for i in range(7)]
    _sv = nc.psum_base
    c.psb = TL(nc.alloc_psum_tensor("psb", [128, 1024], BF16), "psb")
    nc.psum_base = _sv
    _ps7 = TL(nc.alloc_psum_tensor("ps7", [128, 512], F32), "ps7")
    _ps7.b = c.psb.b
    c.ps.append(_ps7)
    c.psi = 0

    def nps():
        t = c.ps[c.psi % 7]
        c.psi += 1
        return t
    c.nps = nps

    c.cstf = sb.tile([128, ncst], F32, "cstf")
    p.dma(c.cstf.t[:], c.cst[:, :], writes=[c.cstf.b])
    c.vecs = sb.tile([128, nl * V_PER], F32, "vecs")
    p.dma(c.vecs.t[:], c.vec[:, :], writes=[c.vecs.b])
    c.flg = sb.tile([128, 1], F32, "flag")
    p.dma(c.flg.t[:], c.flag[:, :], writes=[c.flg.b])
    c.cb = sb.tile([128, 1540], BF16, "cstb")
    p.cp("dve", c.cb.t[:], c.cstf.t[:, 0:1540], reads=[c.cstf.b], writes=[c.cb.b])
    c.ones = sb.tile([128, 128], BF16, "ones")
    p.memset("pool", c.ones.t[:], 1.0, writes=[c.ones.b])
    c.fones = sb.tile([128, 128], BF16, "fones")
    p.ts("dve", c.fones.t[:], c.ones.t[:], c.flg.t[:, 0:1], None, ALU.mult, reads=[c.ones.b, c.flg.b], writes=[c.fones.b])
    c.tri4 = sb.tile([128, 512], F32, "tri4")
    for h in range(4):
        p.cp("pool", c.tri4.t[:, h * 128:(h + 1) * 128], c.cstf.t[:, C_TRI:C_TRI + 128], reads=[c.cstf.b], writes=[c.tri4.b])
    c.zero = sb.tile([128, 256], F32, "zero")
    p.memset("pool", c.zero.t[:], 0.0, writes=[c.zero.b])
    zb = sb.tile([128, 512], BF16, "zb")
    p.memset("pool", zb.t[:], 0.0, writes=[zb.b])
    for a0 in range(0, T, 512):
        p.dma(c.xb2[64:128, a0:a0 + 512], zb.t[64:128, :], reads=[zb.b])
    c.persist_mark = sb.mark()

    xin = c.x
    for li in range(nl):
        c.li = li
        c.vo = li * V_PER
        if li == stop_layer:
            c.stop = stop_after
        stop_after_ = stop_after if li == stop_layer else None
        phase_A(c, xin)
        if stop_after_ == "A":
            break
        exchange(c, c.xb1, c.xg1)
        exchange(c, c.xb2, c.xg2)
        if stop_after_ == "X":
            break
        phase_B(c)
        if stop_after_ in ("B", "B1", "B2", "B2x"):
            break
        phase_C(c)
        if stop_after_ == "C":
            break
        phase_D(c, xin, last=(li == nl - 1) and final_norm)
        xin = c.xres
    if not final_norm:
        pass
    p.barrier()
    p.emit()
    return nc


def exchange(c, src, dst):
    p = c.p
    p.barrier()
    sap, dap = src.ap().opt(), dst.ap().opt()
    p.op("pool", lambda e: e.collective_compute("AllGather", ALU.bypass, replica_groups=[[0, 1], [2, 3], [4, 5], [6, 7]],
                                                ins=[sap], outs=[dap]), kind="cc")
    p.barrier()


def rstd_from_ss(c, out_ap, ss_ap, n, reads, writes):
    p = c.p
    p.act(out_ap, ss_ap, AF.Sqrt, reads=reads, writes=writes, bias=EPS, scale=1.0 / n)
    p.op("dve", lambda e: e.reciprocal(out_ap, out_ap), reads=writes, writes=writes)


class Alt:
    def __init__(self, engs):
        self.engs = engs
        self.i = 0

    def __call__(self):
        e = self.engs[self.i % len(self.engs)]
        self.i += 1
        return e


def scaled_copy(c, eng, out, in_, scal, reads, writes):
    p = c.p
    if eng == "act":
        p.act(out, in_, AF.Copy, reads=reads, writes=writes, scale=scal)
    else:
        p.ts(eng, out, in_, scal, None, ALU.mult, reads=reads, writes=writes)


WX = 2576
S32 = 32 ** -0.5


def phase_A(c, xin):
    nc, p, sb, T, NT = c.nc, c.p, c.sb, c.T, c.NT
    li, vo = c.li, c.vo
    p.barrier()
    sb.reset(c.persist_mark)
    vec = c.vecs
    gv = sb.tile([128, 4, 8], F32, "gv")
    g0 = vec.t[:, vo + V_AN:vo + V_AN + 8]
    p.cp("dve", gv.t[:, 0, :], g0, reads=[vec.b], writes=[gv.b])
    p.ts("dve", gv.t[:, 1, :], g0, -1.0, None, ALU.mult, reads=[vec.b], writes=[gv.b])
    p.ts("dve", gv.t[:, 2, :], g0, S32, None, ALU.mult, reads=[vec.b], writes=[gv.b])
    p.ts("dve", gv.t[:, 3, :], g0, -S32, None, ALU.mult, reads=[vec.b], writes=[gv.b])
    nbg = sb.tile([128, 1], F32, "nbg")
    p.ts("dve", nbg.t[:], vec.t[:, vo + V_BG:vo + V_BG + 1], -1.0, None, ALU.mult, reads=[vec.b], writes=[nbg.b])
    W = sb.tile([128, 8, WX], BF16, "winx")
    stg = [sb.tile([128, DIN], F32, "wstg") for _ in range(2)]
    alt = Alt(["act", "dve"])
    for k in range(8):
        st = stg[k % 2]
        p.dma(st.t[:], c.w_in[li, k * 128:(k + 1) * 128, :], writes=[st.b])
        g, gn, gs, gsn = (gv.t[:, j, k:k + 1] for j in range(4))

        def cv(d0, d1, s0, s1, sc):
            scaled_copy(c, alt(), W.t[:, k, d0:d1], st.t[:, s0:s1], sc, [st.b, gv.b], [W.b])

        def cvrot(d0, s0, nh, half, sp, sn):
            dv = W.t[:, k, d0:d0 + nh * 2 * half].rearrange("p (h two d) -> p h two d", h=nh, two=2)
            sv = st.t[:, s0:s0 + nh * 2 * half].rearrange("p (h two d) -> p h two d", h=nh, two=2)
            scaled_copy(c, alt(), dv[:, :, 0, :], sv[:, :, 1, :], sn, [st.b, gv.b], [W.b])
            scaled_copy(c, alt(), dv[:, :, 1, :], sv[:, :, 0, :], sp, [st.b, gv.b], [W.b])
        cv(0, 704, 0, 704, g)
        cvrot(704, 640, 1, 32, g, gn)
        cv(768, 896, 704, 832, gs)
        cv(896, 1024, 832, 960, g)
        cv(1024, 1040, 1216, 1232, g)
        cv(1040, 1296, 1232, 1488, g)
        cv(1296, 1424, 1488, 1616, g)
        cvrot(1424, 1488, 4, 16, g, gn)
        cv(1552, 1680, 1616, 1744, gs)
        cvrot(1680, 1616, 4, 16, gs, gsn)
        cv(1808, 2064, 2000, 2256, g)
        cv(2064, 2320, 960, 1216, g)
        cv(2320, 2576, 1744, 2000, g)
    wgf = sb.tile([16, 128], F32, "wgf")
    p.dma(wgf.t[:], c.w_gate[li, :, :], writes=[wgf.b])
    wg = sb.tile([16, 128], BF16, "wg")
    p.cp("dve", wg.t[:], wgf.t[:], reads=[wgf.b], writes=[wg.b])

    xt = [sb.tile([128, D], F32, "xt") for _ in range(2)]
    junk = sb.tile([128, D], BF16, "junk")
    ssq = [sb.tile([128, 2], F32, "ssq") for _ in range(2)]
    hN = [sb.tile([128, D], BF16, "hN") for _ in range(2)]
    hT = [sb.tile([128, 8, 512], BF16, "hT") for _ in range(2)]
    zf = [sb.tile([128, 3, 512], F32, "zf") for _ in range(2)]
    sq = [sb.tile([128, 3, 512], BF16, "sq") for _ in range(2)]
    rs = [sb.tile([128, 512], F32, "rs") for _ in range(2)]
    zn = [sb.tile([128, 3, 512], BF16, "zn") for _ in range(2)]
    rp = sb.tile([128, 4, 512], F32, "ropeA")
    tA = [sb.tile([128, 512], F32, "tA") for _ in range(2)]
    tB = [sb.tile([128, 512], F32, "tB") for _ in range(2)]
    ob = [sb.tile([128, 512], BF16, "obA") for _ in range(4)]
    glr = sb.tile([16, 512], BF16, "glr")
    ex = sb.tile([128, 512], F32, "exA")
    la = [sb.tile([128, 512], F32, "laA") for _ in range(2)]
    vN = [sb.tile([128, 512], BF16, "vN") for _ in range(2)]
    cnt = {"ob": 0, "sub": 0, "g": 0}
    idb = c.cb.t[:, C_ID:C_ID + 128]

    def nob():
        t = ob[cnt["ob"] % 4]
        cnt["ob"] += 1
        return t

    def proj(c0, M, hTt):
        ps = c.nps()
        for k in range(8):
            p.mm(ps.t[0:M, :], W.t[:, k, c0:c0 + M], hTt.t[:, k, :], k == 0, k == 7, reads=[W.b, hTt.b], writes=[ps.b])
        return ps

    def load_x(i):
        if i < 4 * NT:
            p.dma(xt[i % 2].t[:], xin[i * 128:(i + 1) * 128, :], writes=[xt[i % 2].b])
    load_x(0)
    for tt in range(NT):
        t0 = tt * 512
        hTt = hT[tt % 2]
        p.dma(rp.t[:], c.rope[:, :, t0:t0 + 512], writes=[rp.b])
        for s in range(4):
            i = cnt["sub"]
            cnt["sub"] += 1
            x_, ss_, hN_ = xt[i % 2], ssq[i % 2], hN[i % 2]
            load_x(i + 1)
            p.act(junk.t[:], x_.t[:], AF.Square, reads=[x_.b], writes=[junk.b, ss_.b], accum=ss_.t[:, 0:1])
            rstd_from_ss(c, ss_.t[:, 1:2], ss_.t[:, 0:1], D, [ss_.b], [ss_.b])
            p.act(hN_.t[:], x_.t[:], AF.Copy, reads=[x_.b, ss_.b], writes=[hN_.b], scale=ss_.t[:, 1:2])
            for k in range(8):
                p.tr(c.psb.t[:, k * 128:(k + 1) * 128], hN_.t[:, k * 128:(k + 1) * 128], idb, reads=[hN_.b, c.cb.b], writes=[c.psb.b])
            p.cp("dve", hTt.t[:, :, s * 128:(s + 1) * 128], r3(c.psb.t[:], a=8), reads=[c.psb.b], writes=[hTt.b])

        for (c0, nchk, n, which) in ((0, 3, 384, "cq"), (384, 2, 256, "ckv")):
            gi = cnt["g"]
            cnt["g"] += 1
            zf_, sq_, rs_, zn_ = zf[gi % 2], sq[gi % 2], rs[gi % 2], zn[gi % 2]
            for j in range(nchk):
                ps = proj(c0 + j * 128, 128, hTt)
                p.cp("act", zf_.t[:, j, :], ps.t[:], reads=[ps.b], writes=[zf_.b])
                p.tt("pool", sq_.t[:, j, :], zf_.t[:, j, :], zf_.t[:, j, :], ALU.mult, reads=[zf_.b], writes=[sq_.b])
            pss = c.nps()
            for j in range(nchk):
                p.mm(pss.t[:], c.ones.t[:], sq_.t[:, j, :], j == 0, j == nchk - 1, reads=[c.ones.b, sq_.b], writes=[pss.b])
            rstd_from_ss(c, rs_.t[:], pss.t[:], n, [pss.b], [rs_.b])
            for j in range(nchk):
                p.tt("dve", zn_.t[:, j, :], zf_.t[:, j, :], rs_.t[:], ALU.mult, reads=[zf_.b, rs_.b], writes=[zn_.b])
            if which == "cq":
                p.dma(c.dT[:, 0:3, t0:t0 + 512], zn_.t[:, 0:3, :], reads=[zn_.b])
            else:
                p.dma(r3(c.xb1[:, :], a=2)[:, :, t0:t0 + 512], zn_.t[:, 0:2, :], reads=[zn_.b])

        def roped(c_raw, c_rot, M, ci, si, dst_ap):
            pr_, pt_ = proj(c_raw, M, hTt), proj(c_rot, M, hTt)
            a_, b_ = tA[cnt["g"] % 2], tB[cnt["g"] % 2]
            cnt["g"] += 1
            p.tt("dve", a_.t[0:M, :], pr_.t[0:M, :], rp.t[0:M, ci, :], ALU.mult, reads=[pr_.b, rp.b], writes=[a_.b])
            p.tt("dve", b_.t[0:M, :], pt_.t[0:M, :], rp.t[0:M, si, :], ALU.mult, reads=[pt_.b, rp.b], writes=[b_.b])
            o_ = nob()
            p.tt("pool", o_.t[0:M, :], a_.t[0:M, :], b_.t[0:M, :], ALU.add, reads=[a_.b, b_.b], writes=[o_.b])
            p.dma(dst_ap, o_.t[0:M, :], reads=[o_.b])

        roped(640, 704, 64, 0, 1, c.xb2[0:64, t0:t0 + 512])
        roped(1296, 1424, 128, 2, 3, c.dT[:, 5, t0:t0 + 512])
        roped(1552, 1680, 128, 2, 3, c.dT[:, 6, t0:t0 + 512])

        for (c0, slot) in ((768, 3), (896, 4)):
            ps = proj(c0, 128, hTt)
            o_ = nob()
            p.cp("act", o_.t[:], ps.t[:], reads=[ps.b], writes=[o_.b])
            p.dma(c.dT[:, slot, t0:t0 + 512], o_.t[:], reads=[o_.b])
        for (c0, slot) in ((1040, 7), (1168, 8), (1808, 9), (1936, 10)):
            ps = proj(c0, 128, hTt)
            o_ = nob()
            p.act(o_.t[:], ps.t[:], AF.Silu, reads=[ps.b], writes=[o_.b])
            p.dma(c.dT[:, slot, t0:t0 + 512], o_.t[:], reads=[o_.b])
        ps = proj(1024, 16, hTt)
        p.cp("act", glr.t[:], ps.t[0:16, :], reads=[ps.b], writes=[glr.b])
        ps2 = c.nps()
        p.mm(ps2.t[:], wg.t[:], glr.t[:], True, True, reads=[wg.b, glr.b], writes=[ps2.b])
        p.act(ex.t[:], ps2.t[:], AF.Exp, reads=[ps2.b, nbg.b], writes=[ex.b], bias=nbg.t[:, 0:1], scale=-1.0)
        la_ = la[tt % 2]
        p.act(la_.t[:], ex.t[:], AF.Ln, reads=[ex.b], writes=[la_.b], bias=1.0)
        p.ts("pool", la_.t[:], la_.t[:], -1.0 / 16.0, None, ALU.mult, reads=[la_.b], writes=[la_.b])
        p.dma(c.logaT[:, t0:t0 + 512], la_.t[:], reads=[la_.b])
        for s in range(4):
            ps = c.nps()
            for k in range(8):
                p.mm(ps.t[:], hTt.t[:, k, s * 128:(s + 1) * 128], W.t[:, k, 2064:2576], k == 0, k == 7, reads=[W.b, hTt.b], writes=[ps.b])
            v_ = vN[s % 2]
            p.cp("act" if s % 2 else "dve", v_.t[:], ps.t[:], reads=[ps.b], writes=[v_.b])
            p.dma(c.dN[t0 + s * 128:t0 + (s + 1) * 128, :], v_.t[:], reads=[v_.b])


def phase_B(c):
    nc, p, sb, T, NT, NCH = c.nc, c.p, c.sb, c.T, c.NT, c.NCH
    p.barrier()
    sb.reset(c.persist_mark)
    cf, cbf = c.cstf, c.cb
    idb = cbf.t[:, C_ID:C_ID + 128]
    idf = cf.t[:, C_ID:C_ID + 128]
    trif = cf.t[:, C_TRI:C_TRI + 128]
    blk = cf.t[:, C_BLK:C_BLK + 256]
    U = [sb.tile([128, NCH, 256], F32, "Uall%d" % m) for m in range(2)]
    Ub = [[Buf() for _ in range(NCH)] for m in range(2)]
    dall = sb.tile([128, NCH], F32, "dall")
    Pp = sb.tile([128, NCH], F32, "Pp")
    vpad = [sb.tile([128, 4, 128], BF16, "vpad%d" % m) for m in range(2)]
    for m in range(2):
        p.memset("pool", vpad[m].t[:], 0.0, writes=[vpad[m].b])
    inT = [sb.tile([128, 4, 128], BF16, "inT") for _ in range(2)]
    la = [sb.tile([128, 128], F32, "laB") for _ in range(2)]
    vN = [sb.tile([128, 512], BF16, "vNB") for _ in range(2)]
    laN = sb.tile([128, 128], F32, "laN")
    eneg = sb.tile([128, 128], F32, "eneg")
    epos = sb.tile([128, 128], F32, "epos")
    kt = sb.tile([128, 128], BF16, "kt")
    qtl = [sb.tile([128, 128], BF16, "qtl") for _ in range(2)]
    qtm = sb.tile([128, 4, 128], BF16, "qtm")
    ktN = sb.tile([128, 128], BF16, "ktN")
    Am = [sb.tile([128, 512], BF16, "Am%d" % m) for m in range(2)]
    oev = [sb.tile([128, 2, 128], F32, "oev") for _ in range(4)]
    rqm = sb.tile([128, 4, 128], BF16, "rqm")
    qdec = [sb.tile([128, 128], BF16, "qdec") for _ in range(2)]
    kdN = sb.tile([128, 128], BF16, "kdN")
    stmp = sb.tile([128, 256], F32, "stmp")
    mB1 = sb.mark()

    def load_B(n):
        if n < NCH:
            tk = n * 128
            p.dma(inT[n % 2].t[:], c.dT[:, 3:7, tk:tk + 128], writes=[inT[n % 2].b])
            p.dma(la[n % 2].t[:], c.logaT[:, tk:tk + 128], writes=[la[n % 2].b])
            p.dma(vN[n % 2].t[:], c.dN[tk:tk + 128, :], writes=[vN[n % 2].b])
    load_B(0)
    for n in range(NCH):
        tok = n * 128
        inT_, la_, vN_ = inT[n % 2], la[n % 2], vN[n % 2]
        load_B(n + 1)
        psl = c.nps()
        p.tr(psl.t[:, 0:128], la_.t[:], idf, reads=[la_.b, cf.b], writes=[psl.b])
        p.cp("act", laN.t[:], psl.t[:, 0:128], reads=[psl.b], writes=[laN.b])
        psB = c.nps()
        p.mm(psB.t[:, 0:128], laN.t[:], trif, True, True, reads=[laN.b, cf.b], writes=[psB.b])
        p.act(eneg.t[:], psB.t[:, 0:128], AF.Exp, reads=[psB.b], writes=[eneg.b], scale=-1.0)
        p.act(epos.t[:], psB.t[:, 0:128], AF.Exp, reads=[psB.b], writes=[epos.b])
        p.cp("dve", dall.t[:, n:n + 1], epos.t[:, 127:128], reads=[epos.b], writes=[dall.b])
        p.tt("dve", kt.t[:], inT_.t[:, 1, :], eneg.t[:], ALU.mult, reads=[inT_.b, eneg.b], writes=[kt.b])
        q_ = qtl[n % 2]
        p.tt("pool", q_.t[:], inT_.t[:, 0, :], epos.t[:], ALU.mult, reads=[inT_.b, epos.b], writes=[q_.b])
        p.dma(c.qt[0, :, tok:tok + 128], q_.t[:], reads=[q_.b])
        for h in range(4):
            p.stt("dve" if h % 2 == 0 else "pool", qtm.t[:, h, :], inT_.t[:, 0, :], cf.t[:, C_HM + h:C_HM + h + 1], epos.t[:],
                  ALU.mult, ALU.mult, reads=[inT_.b, epos.b, cf.b], writes=[qtm.b])
        p.tr(c.psb.t[:, 0:128], kt.t[:], idb, reads=[kt.b, cbf.b], writes=[c.psb.b])
        p.cp("act", ktN.t[:], c.psb.t[:, 0:128], reads=[c.psb.b], writes=[ktN.b])
        psU = c.nps()
        p.mm(psU.t[:, 0:256], ktN.t[:], vN_.t[:, 0:256], True, True, reads=[ktN.b, vN_.b], writes=[psU.b])
        p.stt("dve", U[0].t[:, n, :], psU.t[:, 0:256], dall.t[:, n:n + 1], blk, ALU.mult, ALU.mult,
              reads=[psU.b, dall.b, cf.b], writes=[Ub[0][n]])
        psA = c.nps()
        for h in range(4):
            p.mm(psA.t[:, h * 128:(h + 1) * 128], kt.t[:], qtm.t[:, h, :], True, True, reads=[kt.b, qtm.b], writes=[psA.b])
        p.tt("dve", Am[0].t[:], psA.t[:], c.tri4.t[:], ALU.mult, reads=[psA.b, c.tri4.b], writes=[Am[0].b])
        for h in range(4):
            p.ts("pool" if h % 2 == 0 else "dve", rqm.t[:, h, :], inT_.t[:, 2, :], cf.t[:, C_HM + h:C_HM + h + 1], None, ALU.mult,
                 reads=[inT_.b, cf.b], writes=[rqm.b])
        qd_ = qdec[n % 2]
        p.tt("pool", qd_.t[:], inT_.t[:, 2, :], cf.t[:, C_QDT:C_QDT + 128], ALU.mult, reads=[inT_.b, cf.b], writes=[qd_.b])
        p.dma(c.qt[1, :, tok:tok + 128], qd_.t[:], reads=[qd_.b])
        p.tr(c.psb.t[:, 128:256], inT_.t[:, 3, :], idb, reads=[inT_.b, cbf.b], writes=[c.psb.b])
        p.tt("dve", kdN.t[:], c.psb.t[:, 128:256], cf.t[:, C_KDN:C_KDN + 128], ALU.mult, reads=[c.psb.b, cf.b], writes=[kdN.b])
        psU2 = c.nps()
        p.mm(psU2.t[:, 0:256], kdN.t[:], vN_.t[:, 256:512], True, True, reads=[kdN.b, vN_.b], writes=[psU2.b])
        p.tt("dve", U[1].t[:, n, :], psU2.t[:, 0:256], blk, ALU.mult, reads=[psU2.b, cf.b], writes=[Ub[1][n]])
        psA2 = c.nps()
        for h in range(4):
            p.mm(psA2.t[:, h * 128:(h + 1) * 128], inT_.t[:, 3, :], rqm.t[:, h, :], True, True, reads=[inT_.b, rqm.b], writes=[psA2.b])
        p.tt("dve", Am[1].t[:], psA2.t[:], cf.t[:, C_RDT:C_RDT + 512], ALU.mult, reads=[psA2.b, cf.b], writes=[Am[1].b])
        for m in range(2):
            vp = vpad[m]
            src = vN_.t[:, m * 256:(m + 1) * 256].rearrange("p (a two d) -> p a two d", a=2, two=2)
            dst = vp.t[:].rearrange("p (a two) d -> p a two d", two=2)
            p.cp("pool", dst[:, :, 0, 0:64], src[:, :, 0, :], reads=[vN_.b], writes=[vp.b])
            p.cp("pool", dst[:, :, 1, 64:128], src[:, :, 1, :], reads=[vN_.b], writes=[vp.b])
            psO = c.nps()
            for pr in range(2):
                for hh in range(2):
                    h = 2 * pr + hh
                    p.mm(psO.t[:, pr * 128:(pr + 1) * 128], vp.t[:, h, :], Am[m].t[:, h * 128:(h + 1) * 128], hh == 0, hh == 1,
                         reads=[vp.b, Am[m].b], writes=[psO.b])
            o_ = oev[(2 * n + m) % 4]
            p.cp("act", o_.t[:], r3(psO.t[:, 0:256], a=2), reads=[psO.b], writes=[o_.b])
            p.dma(c.oint[m, :, :, tok:tok + 128], o_.t[:], reads=[o_.b])

    if c.stop == "B1":
        return
    p.memset("dve", Pp.t[:, 0:1], 1.0, writes=[Pp.b])
    for n in range(1, NCH):
        p.stt("dve", U[0].t[:, n, :], U[0].t[:, n - 1, :], dall.t[:, n:n + 1], U[0].t[:, n, :], ALU.mult, ALU.add,
              reads=[Ub[0][n - 1], dall.b], writes=[Ub[0][n]])
        p.stt("dve", U[1].t[:, n, :], U[1].t[:, n - 1, :], cf.t[:, C_RP + 1:C_RP + 2], U[1].t[:, n, :], ALU.mult, ALU.add,
              reads=[Ub[1][n - 1], cf.b], writes=[Ub[1][n]])
        p.tt("dve", Pp.t[:, n:n + 1], Pp.t[:, n - 1:n], dall.t[:, n - 1:n], ALU.mult, reads=[dall.b], writes=[Pp.b])
    p.dma(c.sx[:, 0:256], U[0].t[:, NCH - 1, :], reads=[Ub[0][NCH - 1]])
    p.dma(c.sx[:, 256:512], U[1].t[:, NCH - 1, :], reads=[Ub[1][NCH - 1]])
    if c.stop == "B2":
        return
    exchange(c, c.sx, c.sgat)
    if c.stop == "B2x":
        return
    sb.reset(mB1)
    sin = sb.tile([128, 512], F32, "sin")
    p.dma(sin.t[:], c.sgat[0:128, :], writes=[sin.b])
    sinf = sb.tile([128, 512], F32, "sinf")
    p.ts("dve", sinf.t[:], sin.t[:], c.flg.t[:, 0:1], None, ALU.mult, reads=[sin.b, c.flg.b], writes=[sinf.b])
    qT = [sb.tile([128, 512], BF16, "qTB") for _ in range(2)]
    oi = [sb.tile([128, 2, 512], F32, "oiB") for _ in range(2)]
    gt = [sb.tile([128, 2, 512], BF16, "gtB") for _ in range(2)]
    Sp = [sb.tile([128, 256], BF16, "Sp") for _ in range(4)]
    of = [sb.tile([128, 512], F32, "ofB") for _ in range(2)]
    sqo = [sb.tile([128, 512], BF16, "sqoB") for _ in range(2)]
    rs = [sb.tile([128, 512], F32, "rsB") for _ in range(2)]
    of2 = [sb.tile([128, 512], F32, "of2B") for _ in range(2)]
    yo = [sb.tile([128, 512], BF16, "yoB") for _ in range(2)]
    k = 0
    kk = 0
    def load_B3(kx):
        if kx < 2 * NT:
            tt_, m_ = kx // 2, kx % 2
            a0 = tt_ * 512
            p.dma(qT[kx % 2].t[:], c.qt[m_, :, a0:a0 + 512], writes=[qT[kx % 2].b])
            p.dma(oi[kx % 2].t[:], c.oint[m_, :, :, a0:a0 + 512], writes=[oi[kx % 2].b])
            p.dma(gt[kx % 2].t[:], c.dT[:, 7 + 2 * m_:9 + 2 * m_, a0:a0 + 512], writes=[gt[kx % 2].b])
    load_B3(0)
    for tt in range(NT):
        t0 = tt * 512
        for m in range(2):
            qT_, oi_, gt_ = qT[k % 2], oi[k % 2], gt[k % 2]
            k += 1
            load_B3(k)
            psI = [c.nps(), c.nps()]
            for cch in range(4):
                n = 4 * tt + cch
                Sp_ = Sp[(4 * k + cch) % 4]
                if m == 0:
                    psc, rd = Pp.t[:, n:n + 1], [Pp.b]
                else:
                    psc, rd = cf.t[:, C_RP + n:C_RP + n + 1], [cf.b]
                if n > 0:
                    prev, rd2 = U[m].t[:, n - 1, :], [Ub[m][n - 1]]
                else:
                    prev, rd2 = c.zero.t[:], [c.zero.b]
                p.stt("dve" if cch % 2 == 0 else "pool", Sp_.t[:], sinf.t[:, m * 256:(m + 1) * 256], psc, prev, ALU.mult, ALU.add,
                      reads=[sinf.b] + rd + rd2, writes=[Sp_.b])
                for pr in range(2):
                    p.mm(psI[pr].t[:, cch * 128:(cch + 1) * 128], Sp_.t[:, pr * 128:(pr + 1) * 128], qT_.t[:, cch * 128:(cch + 1) * 128],
                         True, True, reads=[Sp_.b, qT_.b], writes=[psI[pr].b])
            for pr in range(2):
                of_, sq_, rs_, of2_, yo_ = of[kk % 2], sqo[kk % 2], rs[kk % 2], of2[kk % 2], yo[kk % 2]
                kk += 1
                p.tt("dve", of_.t[:], psI[pr].t[:], oi_.t[:, pr, :], ALU.add, reads=[psI[pr].b, oi_.b], writes=[of_.b])
                p.act(sq_.t[:], of_.t[:], AF.Square, reads=[of_.b], writes=[sq_.b])
                pss = c.nps()
                p.mm(pss.t[:], cbf.t[:, C_B64:C_B64 + 128], sq_.t[:], True, True, reads=[cbf.b, sq_.b], writes=[pss.b])
                rstd_from_ss(c, rs_.t[:], pss.t[:], 64, [pss.b], [rs_.b])
                p.tt("dve", of2_.t[:], of_.t[:], rs_.t[:], ALU.mult, reads=[of_.b, rs_.b], writes=[of2_.b])
                p.tt("pool", yo_.t[:], of2_.t[:], gt_.t[:, pr, :], ALU.mult, reads=[of2_.b, gt_.b], writes=[yo_.b])
                p.dma(c.yT[:, 4 + 2 * m + pr, t0:t0 + 512], yo_.t[:], reads=[yo_.b])


def phase_C(c):
    nc, p, sb, T, NT, NCH = c.nc, c.p, c.sb, c.T, c.NT, c.NCH
    li, vo = c.li, c.vo
    p.barrier()
    sb.reset(c.persist_mark)
    vec, cf, cbf = c.vecs, c.cstf, c.cb
    idb = cbf.t[:, C_ID:C_ID + 128]
    maskb = cbf.t[:, C_MASKT:C_MASKT + 128]
    NKT = 2 * T // 512
    NKB = 2 * T // 128
    gq = sb.tile([128, 2, 3], F32, "gqC")
    p.ts("dve", gq.t[:, 0, :], vec.t[:, vo + V_QN:vo + V_QN + 3], SCALE_MLA, None, ALU.mult, reads=[vec.b], writes=[gq.b])
    p.ts("dve", gq.t[:, 1, :], vec.t[:, vo + V_QN:vo + V_QN + 3], -SCALE_MLA, None, ALU.mult, reads=[vec.b], writes=[gq.b])
    wq = sb.tile([128, 3, 1024], BF16, "wq")
    wkv = sb.tile([128, 2, 1024], BF16, "wkv")
    stg = [sb.tile([128, 1024], F32, "stgC") for _ in range(2)]
    alt = Alt(["act", "dve"])
    for k in range(3):
        st = stg[k % 2]
        p.dma(st.t[:, 0:768], c.w_uq[li, k * 128:(k + 1) * 128, :], writes=[st.b])
        scaled_copy(c, alt(), wq.t[:, k, 0:768], st.t[:, 0:768], gq.t[:, 0, k:k + 1], [st.b, gq.b], [wq.b])
        for h in range(4):
            s0 = h * 192 + 128
            scaled_copy(c, alt(), wq.t[:, k, 768 + h * 64:768 + h * 64 + 32], st.t[:, s0 + 32:s0 + 64], gq.t[:, 1, k:k + 1], [st.b, gq.b], [wq.b])
            scaled_copy(c, alt(), wq.t[:, k, 768 + h * 64 + 32:768 + h * 64 + 64], st.t[:, s0:s0 + 32], gq.t[:, 0, k:k + 1], [st.b, gq.b], [wq.b])
    for k in range(2):
        st = stg[(k + 1) % 2]
        p.dma(st.t[:], c.w_ukv[li, k * 128:(k + 1) * 128, :], writes=[st.b])
        scaled_copy(c, alt(), wkv.t[:, k, :], st.t[:], vec.t[:, vo + V_KVN + k:vo + V_KVN + k + 1], [st.b, vec.b], [wkv.b])
    cq = sb.tile([128, 3, T], BF16, "cqC")
    ckv = sb.tile([128, 2, 2 * T], BF16, "ckvC")
    KrT = sb.tile([64, 2 * T], BF16, "KrT")
    PC = 1024 if T % 1024 == 0 else 512
    for a0 in range(0, T, PC):
        p.dma(cq.t[:, :, a0:a0 + PC], c.dT[:, 0:3, a0:a0 + PC], writes=[cq.b])
        p.dma(ckv.t[:, :, a0:a0 + PC], r3(c.xg1[0:128, :], a=2)[:, :, a0:a0 + PC], writes=[ckv.b])
        p.dma(ckv.t[:, :, T + a0:T + a0 + PC], r3(c.xb1[:, :], a=2)[:, :, a0:a0 + PC], writes=[ckv.b])
        p.dma(KrT.t[:, a0:a0 + PC], c.xg2[0:64, a0:a0 + PC], writes=[KrT.b])
        p.dma(KrT.t[:, T + a0:T + a0 + PC], c.xb2[0:64, a0:a0 + PC], writes=[KrT.b])
    KnT = sb.tile([128, 2 * T], BF16, "KnT")
    V = sb.tile([128, NKB, 128], BF16, "V")
    QnT = sb.tile([128, T], BF16, "QnT")
    QrT = sb.tile([64, T], BF16, "QrT")
    sqa = [sb.tile([128, 512], BF16, "sqa") for _ in range(2)]
    rpt = [sb.tile([64, 2, 512], F32, "rptC") for _ in range(2)]
    ta = [sb.tile([64, 512], F32, "taC") for _ in range(2)]
    tb = [sb.tile([64, 512], F32, "tbC") for _ in range(2)]
    mx = sb.tile([128, 4, max(NKT, NT)], F32, "mxC")
    red = sb.tile([128, 8], F32, "redC")
    bias = sb.tile([128, NT], F32, "biasC")
    PT = [sb.tile([128, 512], BF16, "PT") for _ in range(6)]
    rden = [sb.tile([128, 512], F32, "rden") for _ in range(2)]
    uu = [sb.tile([128, 512], F32, "uu") for _ in range(2)]
    squ = [sb.tile([128, 512], BF16, "squ") for _ in range(2)]
    rsu = [sb.tile([128, 512], F32, "rsu") for _ in range(2)]
    yo = [sb.tile([128, 512], BF16, "yoC") for _ in range(2)]
    k_ = {"sq": 0, "pt": 0, "s": 0, "e": 0}

    def nsq():
        t = sqa[k_["sq"] % 2]
        k_["sq"] += 1
        return t

    def rmax(dst_ap, ps, M, rd, wr):
        p.op("dve", lambda e: e.tensor_reduce(dst_ap, ps.t[0:M, :] if M < 128 else ps.t[:], AX.X, ALU.max), reads=rd, writes=wr)

    for kt in range(NKT):
        s_ = nsq()
        p.tt("pool", s_.t[0:64, :], KrT.t[:, kt * 512:(kt + 1) * 512], KrT.t[:, kt * 512:(kt + 1) * 512], ALU.mult, reads=[KrT.b], writes=[s_.b])
        ps = c.nps()
        p.mm(ps.t[:], c.ones.t[0:64, :], s_.t[0:64, :], True, True, reads=[c.ones.b, s_.b], writes=[ps.b])
        rmax(mx.t[:, 1, kt:kt + 1], ps, 128, [ps.b], [mx.b])
    p.op("dve", lambda e: e.tensor_reduce(red.t[:, 1:2], mx.t[:, 1, 0:NKT], AX.X, ALU.max), reads=[mx.b], writes=[red.b])

    for h in range(4):
        for kt in range(NKT):
            ps = c.nps()
            for k in range(2):
                p.mm(ps.t[:], wkv.t[:, k, h * 256:h * 256 + 128], ckv.t[:, k, kt * 512:(kt + 1) * 512], k == 0, k == 1,
                     reads=[wkv.b, ckv.b], writes=[ps.b])
            p.cp("act", KnT.t[:, kt * 512:(kt + 1) * 512], ps.t[:], reads=[ps.b], writes=[KnT.b])
            s_ = nsq()
            p.tt("pool", s_.t[:], KnT.t[:, kt * 512:(kt + 1) * 512], KnT.t[:, kt * 512:(kt + 1) * 512], ALU.mult, reads=[KnT.b], writes=[s_.b])
            ps2 = c.nps()
            p.mm(ps2.t[:], c.ones.t[:], s_.t[:], True, True, reads=[c.ones.b, s_.b], writes=[ps2.b])
            rmax(mx.t[:, 0, kt:kt + 1], ps2, 128, [ps2.b], [mx.b])
        p.op("dve", lambda e: e.tensor_reduce(red.t[:, 0:1], mx.t[:, 0, 0:NKT], AX.X, ALU.max), reads=[mx.b], writes=[red.b])
        p.tt("dve", red.t[:, 2:3], red.t[:, 0:1], red.t[:, 1:2], ALU.add, reads=[red.b], writes=[red.b])
        for g in range(NKB // 4):
            ps = c.nps()
            for j in range(4):
                kb = 4 * g + j
                for k in range(2):
                    p.mm(ps.t[:, j * 128:(j + 1) * 128], ckv.t[:, k, kb * 128:(kb + 1) * 128], wkv.t[:, k, h * 256 + 128:(h + 1) * 256],
                         k == 0, k == 1, reads=[wkv.b, ckv.b], writes=[ps.b])
            dst = V.t[:, 4 * g:4 * g + 4, :]
            if 4 * g < NCH:
                p.ts("dve", dst, r3(ps.t[:], a=4), c.flg.t[:, 0:1], None, ALU.mult, reads=[ps.b, c.flg.b], writes=[V.b])
            else:
                p.cp("act" if g % 2 else "dve", dst, r3(ps.t[:], a=4), reads=[ps.b], writes=[V.b])
        for tt in range(NT):
            t0 = tt * 512
            rp_ = rpt[tt % 2]
            p.dma(rp_.t[:], c.rope[0:64, 0:2, t0:t0 + 512], writes=[rp_.b])
            ps = c.nps()
            for k in range(3):
                p.mm(ps.t[:], wq.t[:, k, h * 192:h * 192 + 128], cq.t[:, k, t0:t0 + 512], k == 0, k == 2, reads=[wq.b, cq.b], writes=[ps.b])
            p.cp("act", QnT.t[:, t0:t0 + 512], ps.t[:], reads=[ps.b], writes=[QnT.b])
            psr, pst = c.nps(), c.nps()
            for k in range(3):
                p.mm(psr.t[0:64, :], wq.t[:, k, h * 192 + 128:h * 192 + 192], cq.t[:, k, t0:t0 + 512], k == 0, k == 2, reads=[wq.b, cq.b], writes=[psr.b])
            for k in range(3):
                p.mm(pst.t[0:64, :], wq.t[:, k, 768 + h * 64:768 + h * 64 + 64], cq.t[:, k, t0:t0 + 512], k == 0, k == 2, reads=[wq.b, cq.b], writes=[pst.b])
            a_, b_ = ta[tt % 2], tb[tt % 2]
            p.tt("dve", a_.t[:], psr.t[0:64, :], rp_.t[:, 0, :], ALU.mult, reads=[psr.b, rp_.b], writes=[a_.b])
            p.tt("dve", b_.t[:], pst.t[0:64, :], rp_.t[:, 1, :], ALU.mult, reads=[pst.b, rp_.b], writes=[b_.b])
            p.tt("pool", QrT.t[:, t0:t0 + 512], a_.t[:], b_.t[:], ALU.add, reads=[a_.b, b_.b], writes=[QrT.b])
            s_ = nsq()
            p.tt("pool", s_.t[:], QnT.t[:, t0:t0 + 512], QnT.t[:, t0:t0 + 512], ALU.mult, reads=[QnT.b], writes=[s_.b])
            ps2 = c.nps()
            p.mm(ps2.t[:], c.ones.t[:], s_.t[:], True, True, reads=[c.ones.b, s_.b], writes=[ps2.b])
            rmax(mx.t[:, 2, tt:tt + 1], ps2, 128, [ps2.b], [mx.b])
            s2_ = nsq()
            p.tt("pool", s2_.t[0:64, :], QrT.t[:, t0:t0 + 512], QrT.t[:, t0:t0 + 512], ALU.mult, reads=[QrT.b], writes=[s2_.b])
            ps3 = c.nps()
            p.mm(ps3.t[:], c.ones.t[0:64, :], s2_.t[0:64, :], True, True, reads=[c.ones.b, s2_.b], writes=[ps3.b])
            rmax(mx.t[:, 3, tt:tt + 1], ps3, 128, [ps3.b], [mx.b])
        p.tt("dve", bias.t[:, 0:NT], mx.t[:, 2, 0:NT], mx.t[:, 3, 0:NT], ALU.add, reads=[mx.b], writes=[bias.b])
        p.ts("dve", bias.t[:, 0:NT], bias.t[:, 0:NT], red.t[:, 2:3], None, ALU.mult, reads=[bias.b, red.b], writes=[bias.b])
        p.act(bias.t[:, 0:NT], bias.t[:, 0:NT], AF.Ln, reads=[bias.b], writes=[bias.b])
        p.act(bias.t[:, 0:NT], bias.t[:, 0:NT], AF.Exp, reads=[bias.b], writes=[bias.b], scale=0.5)
        p.ts("dve", bias.t[:, 0:NT], bias.t[:, 0:NT], -1.0, None, ALU.mult, reads=[bias.b], writes=[bias.b])

        if h == 0:
            pend = {"f": None}
        for qt in range(NT):
            q0 = qt * 512
            accO, accD = c.ps[k_["e"] % 2], c.ps[2 + k_["e"] % 2]
            blocks = [(kb, -1) for kb in range(NCH)] + [(NCH + kb, (kb - 4 * qt) if kb >= 4 * qt else -1) for kb in range(4 * qt + 4)]
            nb = len(blocks)
            stiles = [None] * nb

            G = 2
            groups = [list(range(a, min(a + G, nb))) for a in range(0, nb, G)]

            def emit_Sg(g):
                idxs = groups[g]
                banks = []
                for j_, i in enumerate(idxs):
                    S = c.ps[4 + 2 * (g % 2) + j_]
                    stiles[i] = S
                    banks.append(S.b)
                first = True
                for i in idxs:
                    kb, dj = blocks[i]
                    S = stiles[i]
                    lo = 0 if dj < 0 else 128 * dj
                    p.mm(S.t[:, lo:512], KnT.t[:, kb * 128:(kb + 1) * 128], QnT.t[:, q0 + lo:q0 + 512], True, False,
                         reads=[KnT.b, QnT.b], writes=(banks if first else [S.b]))
                    first = False
                for i in idxs:
                    kb, dj = blocks[i]
                    S = stiles[i]
                    lo = 0 if dj < 0 else 128 * dj
                    p.mm(S.t[:, lo:512], KrT.t[:, kb * 128:(kb + 1) * 128], QrT.t[:, q0 + lo:q0 + 512], False, dj < 0,
                         reads=[KrT.b, QrT.b], writes=[S.b])
                for i in idxs:
                    kb, dj = blocks[i]
                    S = stiles[i]
                    lo = 0 if dj < 0 else 128 * dj
                    if dj >= 0:
                        p.mm(S.t[:, lo:lo + 128], idb, maskb, False, True, reads=[cbf.b], writes=[S.b])

            def emit_PVg(g):
                idxs = groups[g]
                Ps = []
                for i in idxs:
                    kb, dj = blocks[i]
                    S = stiles[i]
                    lo = 0 if dj < 0 else 128 * dj
                    P_ = PT[k_["pt"] % 6]
                    k_["pt"] += 1
                    Ps.append(P_)
                    p.act(P_.t[:, lo:512], S.t[:, lo:512], AF.Exp, reads=[S.b, bias.b], writes=[P_.b], bias=bias.t[:, qt:qt + 1], scale=1.0)
                first = True
                for i, P_ in zip(idxs, Ps):
                    kb, dj = blocks[i]
                    lo = 0 if dj < 0 else 128 * dj
                    on = c.fones if kb < NCH else c.ones
                    rd = [V.b, on.b] + ([x.b for x in Ps] if first else [P_.b])
                    first = False
                    p.mm(accO.t[:, lo:512], V.t[:, kb, :], P_.t[:, lo:512], i == 0, i == nb - 1, reads=rd, writes=[accO.b])
                    p.mm(accD.t[:, lo:512], on.t[:], P_.t[:, lo:512], i == 0, i == nb - 1, reads=[on.b, P_.b], writes=[accD.b])

            emit_Sg(0)
            for g in range(len(groups)):
                if g + 1 < len(groups):
                    emit_Sg(g + 1)
                emit_PVg(g)
                if g == 3 and pend["f"] is not None:
                    pend["f"]()
                    pend["f"] = None
            e = k_["e"]
            k_["e"] += 1
            rd_, u_, sq_, rs_, yo_ = rden[e % 2], uu[e % 2], squ[e % 2], rsu[e % 2], yo[e % 2]
            p.op("dve", lambda e_, rd_=rd_, accD=accD: e_.reciprocal(rd_.t[:], accD.t[:]), reads=[accD.b], writes=[rd_.b])
            p.tt("dve", u_.t[:], accO.t[:], rd_.t[:], ALU.mult, reads=[accO.b, rd_.b], writes=[u_.b])
            p.tt("pool", sq_.t[:], u_.t[:], u_.t[:], ALU.mult, reads=[u_.b], writes=[sq_.b])

            def part2(u_=u_, sq_=sq_, rs_=rs_, yo_=yo_, h=h, q0=q0):
                pss = c.ps[6]
                p.mm(pss.t[:], c.ones.t[:], sq_.t[:], True, True, reads=[c.ones.b, sq_.b], writes=[pss.b])
                p.act(rs_.t[:], pss.t[:], AF.Ln, reads=[pss.b], writes=[rs_.b], bias=EPS, scale=1.0 / 128)
                p.act(rs_.t[:], rs_.t[:], AF.Exp, reads=[rs_.b], writes=[rs_.b], scale=-0.5)
                p.tt("pool", yo_.t[:], u_.t[:], rs_.t[:], ALU.mult, reads=[u_.b, rs_.b], writes=[yo_.b])
                p.dma(c.yT[:, h, q0:q0 + 512], yo_.t[:], reads=[yo_.b])
            pend["f"] = part2
    if pend["f"] is not None:
        pend["f"]()
        pend["f"] = None


def phase_D(c, xin, last):
    nc, p, sb, T, NT, NCH = c.nc, c.p, c.sb, c.T, c.NT, c.NCH
    li, vo = c.li, c.vo
    p.barrier()
    sb.reset(c.persist_mark)
    vec, cf, cbf = c.vecs, c.cstf, c.cb
    idb = cbf.t[:, C_ID:C_ID + 128]
    wo = sb.tile([128, 8, D], BF16, "wo")
    wu = sb.tile([128, 8, 2 * DFF], BF16, "wu")
    mW = sb.mark()
    stg = [sb.tile([128, DFF], F32, "stgD") for _ in range(3)]
    alt = Alt(["act", "dve"])
    for k in range(8):
        st = stg[k % 3]
        p.dma(st.t[:, 0:D], c.w_out[li, k * 128:(k + 1) * 128, :], writes=[st.b])
        scaled_copy(c, alt(), wo.t[:, k, :], st.t[:, 0:D], vec.t[:, vo + V_ON + k:vo + V_ON + k + 1], [st.b, vec.b], [wo.b])
    pieces = [(k, hf) for hf in range(2) for k in range(8)]
    wub = [[Buf() for _ in range(2)] for _ in range(8)]

    def piece_dma(j):
        if j < len(pieces):
            k, hf = pieces[j]
            st = stg[j % 3]
            p.dma(st.t[:], c.w_up[li, k * 128:(k + 1) * 128, hf * DFF:(hf + 1) * DFF], writes=[st.b])

    def piece_cvt(j):
        if j < len(pieces):
            k, hf = pieces[j]
            st = stg[j % 3]
            scaled_copy(c, alt(), wu.t[:, k, hf * DFF:(hf + 1) * DFF], st.t[:], vec.t[:, vo + V_FN + k:vo + V_FN + k + 1], [st.b, vec.b], [wub[k][hf]])
    piece_dma(0)
    piece_dma(1)
    pj = {"j": 0}

    def piece_step():
        j = pj["j"]
        if j < len(pieces):
            piece_dma(j + 2)
            piece_cvt(j)
            pj["j"] += 1
    wu_a = [wub[k][0] for k in range(8)]
    wu_all = [wub[k][hf] for k in range(8) for hf in range(2)]
    yt = [sb.tile([128, 8, 512], BF16, "ytD") for _ in range(2)]
    xt = [sb.tile([128, D], F32, "xtD") for _ in range(2)]
    xm = [sb.tile([128, D], F32, "xmD") for _ in range(2)]
    junk = sb.tile([128, D], BF16, "junkD")
    ssq = [sb.tile([128, 2], F32, "ssqD") for _ in range(2)]
    hN = [sb.tile([128, D], BF16, "hND") for _ in range(2)]
    hTt = [sb.tile([128, 8, 512], BF16, "hTD") for _ in range(2)]
    i = 0

    def load_x1(ix):
        if ix < 4 * NT:
            p.dma(xt[ix % 2].t[:], xin[ix * 128:(ix + 1) * 128, :], writes=[xt[ix % 2].b])

    def load_y1(tx):
        if tx < NT:
            p.dma(yt[tx % 2].t[:], c.yT[:, :, tx * 512:(tx + 1) * 512], writes=[yt[tx % 2].b])
    load_y1(0)
    load_x1(0)
    for tt in range(NT):
        t0 = tt * 512
        yt_, hT_ = yt[tt % 2], hTt[tt % 2]
        load_y1(tt + 1)
        for s in range(4):
            x_, xm_, ss_, hN_ = xt[i % 2], xm[i % 2], ssq[i % 2], hN[i % 2]
            i += 1
            r0 = t0 + s * 128
            load_x1(i)
            for hf in range(2):
                ps = c.nps()
                for k in range(8):
                    p.mm(ps.t[:], yt_.t[:, k, s * 128:(s + 1) * 128], wo.t[:, k, hf * 512:(hf + 1) * 512], k == 0, k == 7,
                         reads=[yt_.b, wo.b], writes=[ps.b])
                p.tt("dve", xm_.t[:, hf * 512:(hf + 1) * 512], ps.t[:], x_.t[:, hf * 512:(hf + 1) * 512], ALU.add, reads=[ps.b, x_.b], writes=[xm_.b])
            p.dma(c.xres[r0:r0 + 128, :], xm_.t[:], reads=[xm_.b])
            p.act(junk.t[:], xm_.t[:], AF.Square, reads=[xm_.b], writes=[junk.b, ss_.b], accum=ss_.t[:, 0:1])
            rstd_from_ss(c, ss_.t[:, 1:2], ss_.t[:, 0:1], D, [ss_.b], [ss_.b])
            p.act(hN_.t[:], xm_.t[:], AF.Copy, reads=[xm_.b, ss_.b], writes=[hN_.b], scale=ss_.t[:, 1:2])
            for k in range(8):
                p.tr(c.psb.t[:, k * 128:(k + 1) * 128], hN_.t[:, k * 128:(k + 1) * 128], idb, reads=[hN_.b, cbf.b], writes=[c.psb.b])
            p.cp("pool" if False else "dve", hT_.t[:, :, s * 128:(s + 1) * 128], r3(c.psb.t[:], a=8), reads=[c.psb.b], writes=[hT_.b])
            piece_step()
        p.dma(c.hT[:, :, t0:t0 + 512], hT_.t[:], reads=[hT_.b])
    while pj["j"] < len(pieces):
        piece_step()
    if c.stop == "D1":
        return
    hl = hTt[(NT - 1) % 2]
    ps = c.nps()
    for cc in range(NFC):
        for k in range(8):
            p.mm(ps.t[:, 2 * cc:2 * cc + 2], wu.t[:, k, cc * 128:(cc + 1) * 128], hl.t[:, k, 510:512], k == 0, k == 7,
                 reads=[wub[k][0], hl.b], writes=[ps.b])
    hout = sb.tile([128, 2 * NFC], F32, "hout")
    p.cp("act", hout.t[:], ps.t[:, 0:2 * NFC], reads=[ps.b], writes=[hout.b])
    p.dma(c.halo[:, :], hout.t[:], reads=[hout.b])
    exchange(c, c.halo, c.hgat)
    if c.stop == "Dh":
        return
    sb.reset(mW)
    hin = sb.tile([128, 2 * NFC], F32, "hin")
    p.dma(hin.t[:], c.hgat[0:128, :], writes=[hin.b])
    hal = sb.tile([128, NFC, 2], F32, "hal")
    halb = [Buf() for _ in range(NFC)]
    p.ts("dve", hal.t[:], r3(hin.t[:], a=NFC), c.flg.t[:, 0:1], None, ALU.mult, reads=[hin.b, c.flg.b], writes=halb)
    hT2 = [sb.tile([128, 8, 512], BF16, "hT2") for _ in range(2)]
    asb = [sb.tile([128, 514], F32, "asb") for _ in range(3)]
    t1 = [sb.tile([128, 512], F32, "t1D") for _ in range(2)]
    t2 = [sb.tile([128, 512], F32, "t2D") for _ in range(2)]
    t3 = [sb.tile([128, 512], F32, "t3D") for _ in range(2)]
    sl = [sb.tile([128, 512], F32, "slD") for _ in range(2)]
    gTt = [sb.tile([128, NFC, 512], BF16, "gTt") for _ in range(2)]
    cw = lambda k_, cc: vec.t[:, vo + V_CW + k_ * NFC + cc:vo + V_CW + k_ * NFC + cc + 1]
    cbv = lambda cc: vec.t[:, vo + V_CB + cc:vo + V_CB + cc + 1]
    j = 0

    def load_h2(tx):
        if tx < NT:
            p.dma(hT2[tx % 2].t[:], c.hT[:, :, tx * 512:(tx + 1) * 512], writes=[hT2[tx % 2].b])
    load_h2(0)
    for tt in range(NT):
        t0 = tt * 512
        h_ = hT2[tt % 2]
        g_ = gTt[tt % 2]
        load_h2(tt + 1)
        for cc in range(NFC):
            psa, psg = c.nps(), c.nps()
            for k in range(8):
                p.mm(psa.t[:], wu.t[:, k, cc * 128:(cc + 1) * 128], h_.t[:, k, :], k == 0, k == 7, reads=[wub[k][0], h_.b], writes=[psa.b])
            for k in range(8):
                p.mm(psg.t[:], wu.t[:, k, DFF + cc * 128:DFF + (cc + 1) * 128], h_.t[:, k, :], k == 0, k == 7, reads=[wub[k][1], h_.b], writes=[psg.b])
            a_ = asb[j % 3]
            t1_, t2_, t3_, s_ = t1[j % 2], t2[j % 2], t3[j % 2], sl[j % 2]
            j += 1
            p.cp("act", a_.t[:, 2:514], psa.t[:], reads=[psa.b], writes=[a_.b])
            p.cp("pool", a_.t[:, 0:2], hal.t[:, cc, :], reads=[halb[cc]], writes=[a_.b])
            p.cp("pool", hal.t[:, cc, :], a_.t[:, 512:514], reads=[a_.b], writes=[halb[cc]])
            p.ts("pool", t1_.t[:], a_.t[:, 2:514], cw(2, cc), cbv(cc), ALU.mult, ALU.add, reads=[a_.b, vec.b], writes=[t1_.b])
            p.stt("dve", t2_.t[:], a_.t[:, 1:513], cw(1, cc), t1_.t[:], ALU.mult, ALU.add, reads=[a_.b, vec.b, t1_.b], writes=[t2_.b])
            p.stt("dve", t3_.t[:], a_.t[:, 0:512], cw(0, cc), t2_.t[:], ALU.mult, ALU.add, reads=[a_.b, vec.b, t2_.b], writes=[t3_.b])
            p.act(s_.t[:], t3_.t[:], AF.Silu, reads=[t3_.b], writes=[s_.b])
            p.tt("dve", g_.t[:, cc, :], s_.t[:], psg.t[:], ALU.mult, reads=[s_.b, psg.b], writes=[g_.b])
        p.dma(c.gT[:, :, t0:t0 + 512], g_.t[:], reads=[g_.b])
    if c.stop == "D2":
        return
    p.barrier()
    sb.reset(c.persist_mark)
    wd = sb.tile([128, NFC, D], BF16, "wd")
    stg = [sb.tile([128, D], F32, "stgD3") for _ in range(2)]
    for cc in range(NFC):
        st = stg[cc % 2]
        p.dma(st.t[:], c.w_down[li, cc * 128:(cc + 1) * 128, :], writes=[st.b])
        p.cp(alt(), wd.t[:, cc, :], st.t[:], reads=[st.b], writes=[wd.b])
    fnb = None
    if last:
        fnb = sb.tile([128, D], F32, "fnb")
        p.dma(fnb.t[:], c.fnb[:, :], writes=[fnb.b])
    g3 = [sb.tile([128, NFC, 512], BF16, "g3") for _ in range(2)]
    xt = [sb.tile([128, D], F32, "xt3") for _ in range(2)]
    xo = [sb.tile([128, D], F32, "xo3") for _ in range(2)]
    junk = sb.tile([128, D], BF16, "junk3")
    ssq = [sb.tile([128, 2], F32, "ssq3") for _ in range(2)]
    yn = [sb.tile([128, D], F32, "yn3") for _ in range(2)]
    i = 0

    def load_g3(tx):
        if tx < NT:
            p.dma(g3[tx % 2].t[:], c.gT[:, :, tx * 512:(tx + 1) * 512], writes=[g3[tx % 2].b])

    def load_x3(ix):
        if ix < 4 * NT:
            p.dma(xt[ix % 2].t[:], c.xres[ix * 128:(ix + 1) * 128, :], writes=[xt[ix % 2].b])
    load_g3(0)
    load_x3(0)
    for tt in range(NT):
        t0 = tt * 512
        g_ = g3[tt % 2]
        load_g3(tt + 1)
        for s in range(4):
            x_, xo_, ss_, yn_ = xt[i % 2], xo[i % 2], ssq[i % 2], yn[i % 2]
            i += 1
            r0 = t0 + s * 128
            load_x3(i)
            for hf in range(2):
                ps = c.nps()
                for cc in range(NFC):
                    p.mm(ps.t[:], g_.t[:, cc, s * 128:(s + 1) * 128], wd.t[:, cc, hf * 512:(hf + 1) * 512], cc == 0, cc == NFC - 1,
                         reads=[g_.b, wd.b], writes=[ps.b])
                p.tt("dve", xo_.t[:, hf * 512:(hf + 1) * 512], ps.t[:], x_.t[:, hf * 512:(hf + 1) * 512], ALU.add, reads=[ps.b, x_.b], writes=[xo_.b])
            if not last:
                p.dma(c.xres[r0:r0 + 128, :], xo_.t[:], reads=[xo_.b])
            else:
                p.act(junk.t[:], xo_.t[:], AF.Square, reads=[xo_.b], writes=[junk.b, ss_.b], accum=ss_.t[:, 0:1])
                rstd_from_ss(c, ss_.t[:, 1:2], ss_.t[:, 0:1], D, [ss_.b], [ss_.b])
                p.act(yn_.t[:], xo_.t[:], AF.Copy, reads=[xo_.b, ss_.b], writes=[yn_.b], scale=ss_.t[:, 1:2])
                p.tt("pool", yn_.t[:], yn_.t[:], fnb.t[:], ALU.mult, reads=[yn_.b, fnb.b], writes=[yn_.b])
                p.dma(c.out[r0:r0 + 128, :], yn_.t[:], reads=[yn_.b])


_NC_CACHE = {}


def kernel(x, attn_norm, w_in, mla_q_norm, mla_w_uq, mla_kv_norm, mla_w_ukv, mla_out_norm,
           gla_w_gate, gla_b_gate, gla_out_norm, ret_out_norm, w_out, ffn_norm, ffn_w_up,
           ffn_conv_w, ffn_conv_b, ffn_w_down, final_norm):
    f = lambda a: np.ascontiguousarray(np.asarray(a, dtype=np.float32))
    x = f(x)
    B, S, _ = x.shape
    T = S // 2
    nl = int(np.asarray(w_in).shape[0])
    assert B * 2 == 8
    inp = dict(attn_norm=f(attn_norm), ffn_norm=f(ffn_norm), mla_q_norm=f(mla_q_norm), mla_kv_norm=f(mla_kv_norm),
               mla_out_norm=f(mla_out_norm), gla_out_norm=f(gla_out_norm), ret_out_norm=f(ret_out_norm),
               gla_b_gate=f(gla_b_gate), ffn_conv_w=f(ffn_conv_w), ffn_conv_b=f(ffn_conv_b))
    key = (T, nl)
    if key not in _NC_CACHE:
        _NC_CACHE[key] = build(T, nl)
    nc = _NC_CACHE[key]
    cst = host_consts(T)
    vec = host_vec(inp, 0, nl)
    fnb = np.ascontiguousarray(np.broadcast_to(f(final_norm)[None, :], (128, D)))
    shared = dict(w_in=f(w_in), w_uq=f(mla_w_uq), w_ukv=f(mla_w_ukv), w_out=f(w_out), w_up=f(ffn_w_up),
                  w_down=f(ffn_w_down), w_gate=f(gla_w_gate), vec=vec, cst=cst, fnb=fnb)
    ropes = [host_rope(T, r * T) for r in range(2)]
    in_maps = []
    for core in range(8):
        b, r = core // 2, core % 2
        m = dict(shared)
        m["x"] = np.ascontiguousarray(x[b, r * T:(r + 1) * T])
        m["rope"] = ropes[r]
        m["flag"] = np.full((128, 1), float(r), np.float32)
        in_maps.append(m)
    res = run_bass_kernel_spmd(nc, in_maps, core_ids=list(range(8)))
    out = np.empty((B, S, D), np.float32)
    for core in range(8):
        b, r = core // 2, core % 2
        out[b, r * T:(r + 1) * T] = np.asarray(res.results[core]["out"])
    return out
```

```python
import numpy as np
import ml_dtypes
import concourse.bass as bass
import concourse.mybir as mybir
from concourse.bass_utils import run_bass_kernel_spmd

F32 = mybir.dt.float32
BF16 = mybir.dt.bfloat16
AF = mybir.ActivationFunctionType
ALU = mybir.AluOpType
AX = mybir.AxisListType

D = 1024
DEPTH = 4
EPS = 1e-6
DIN = 2256
DFF = 2816
NFC = DFF // 128
SCALE_MLA = 192 ** -0.5

ENGS = ("pe", "act", "dve", "pool", "sp")
KDMA = 8


class Buf:
    __slots__ = ("lw", "rd", "name")

    def __init__(self, name=""):
        self.lw = None
        self.rd = []
        self.name = name


class Op:
    __slots__ = ("eng", "fn", "deps", "kind", "signal", "tok", "idx", "dma_n")


class Prog:
    def __init__(self, nc):
        self.nc = nc
        self.ops = {e: [] for e in ENGS}
        self.all = []
        self.ndma = {e: 0 for e in ENGS}
        self.last = {e: None for e in ENGS}
        self.pending_barrier = {e: [] for e in ENGS}
        self.dmas = {e: [] for e in ENGS}
        self.lastcc = None

    def op(self, eng, fn, reads=(), writes=(), kind="c"):
        o = Op()
        o.eng, o.fn, o.kind, o.signal, o.tok = eng, fn, kind, False, None
        deps = []
        for b in reads:
            if b.lw is not None:
                deps.append(b.lw)
        for b in writes:
            if b.lw is not None:
                deps.append(b.lw)
            deps.extend(b.rd)
        deps.extend(self.pending_barrier[eng])
        self.pending_barrier[eng] = []
        dd = []
        seen = set()
        for d in deps:
            if id(d) in seen:
                continue
            seen.add(id(d))
            if d.eng == "pe" and eng == "pe" and d.kind == "c" and kind == "c":
                continue
            dd.append(d)
        o.deps = dd
        for b in writes:
            b.lw = o
            b.rd = []
        for b in reads:
            b.rd.append(o)
        if kind == "d":
            o.dma_n = self.ndma[eng]
            self.ndma[eng] += 1
            self.dmas[eng].append(o)
        if kind == "cc":
            if self.lastcc is not None and all(x is not self.lastcc for x in o.deps):
                o.deps.append(self.lastcc)
            self.lastcc = o
            self.dmas[eng].append(o)
        o.idx = len(self.ops[eng])
        self.ops[eng].append(o)
        self.all.append(o)
        self.last[eng] = o
        return o

    def barrier(self):
        lasts = [self.last[e] for e in ENGS if self.last[e] is not None]
        for e in ENGS:
            lasts.extend(self.dmas[e][-(KDMA + 2):])
        for e in ENGS:
            self.pending_barrier[e] = list(lasts)

    def dma(self, out, in_, reads=(), writes=(), eng="sp"):
        return self.op(eng, lambda e: e.dma_start(out=out, in_=in_), reads, writes, kind="d")

    def mm(self, out, lhsT, rhs, start, stop, reads=(), writes=()):
        return self.op("pe", lambda e: e.matmul(out, lhsT, rhs, start=start, stop=stop), reads, writes)

    def tr(self, out, in_, ident, reads=(), writes=()):
        return self.op("pe", lambda e: e.transpose(out, in_, ident), reads, writes)

    def act(self, out, in_, func, reads=(), writes=(), bias=None, scale=None, accum=None):
        def fn(e):
            kw = {}
            if bias is not None:
                kw["bias"] = bias
            if scale is not None:
                kw["scale"] = scale
            if accum is not None:
                kw["accum_out"] = accum
            return e.activation(out, in_, func, **kw)
        return self.op("act", fn, reads, writes)

    def tt(self, eng, out, in0, in1, op, reads=(), writes=()):
        return self.op(eng, lambda e: e.tensor_tensor(out, in0, in1, op), reads, writes)

    def ts(self, eng, out, in0, s1, s2, op0, op1=None, reads=(), writes=()):
        def fn(e):
            if op1 is None:
                return e.tensor_scalar(out, in0, s1, None, op0)
            return e.tensor_scalar(out, in0, s1, s2, op0, op1)
        return self.op(eng, fn, reads, writes)

    def stt(self, eng, out, in0, scalar, in1, op0, op1, reads=(), writes=()):
        eng = "dve"
        return self.op(eng, lambda e: e.scalar_tensor_tensor(out, in0, scalar, in1, op0, op1), reads, writes)

    def cp(self, eng, out, in_, reads=(), writes=()):
        if eng == "act":
            return self.op("act", lambda e: e.copy(out=out, in_=in_), reads, writes)
        return self.op(eng, lambda e: e.tensor_copy(out, in_), reads, writes)

    def memset(self, eng, ap, val, writes=()):
        return self.op(eng, lambda e: e.memset(ap, val), (), writes)

    def emit(self):
        nc = self.nc
        for o in self.all:
            for d in o.deps:
                d.signal = True
        import contextlib
        with contextlib.ExitStack() as es:
            csem = {e: es.enter_context(nc.semaphore("c_" + e)) for e in ("pe", "act", "dve", "pool")}
            dsem = {e: [es.enter_context(nc.semaphore("d_%s%d" % (e, i))) for i in range(KDMA)]
                    for e in ENGS if self.ndma[e] > 0}
            ccsem = es.enter_context(nc.semaphore("ccs"))
            cnt = {e: 0 for e in ENGS}
            ccn = 0
            for e in ENGS:
                for o in self.ops[e]:
                    if o.kind == "d":
                        o.signal = True
                        o.tok = (dsem[e][o.dma_n % KDMA], 16 * (o.dma_n // KDMA + 1), 16)
                    elif o.kind == "cc":
                        ccn += 1
                        o.signal = True
                        o.tok = (ccsem, ccn, 1)
                    elif o.signal:
                        cnt[e] += 1
                        o.tok = (csem[e], cnt[e], 1)
            block = es.enter_context(nc.Block())
            prog = self

            def body(ename):
                def f(eng):
                    waited = {}
                    ops = prog.ops[ename]
                    for o in ops:
                        ws = []
                        for d in o.deps:
                            ws.append((d.tok[0], d.tok[1]))
                        if o.kind == "d" and o.dma_n >= KDMA:
                            ws.append((dsem[ename][o.dma_n % KDMA], 16 * (o.dma_n // KDMA)))
                        for (s, v) in ws:
                            k = s.num
                            if waited.get(k, 0) < v:
                                eng.wait_ge(s, v)
                                waited[k] = v
                        ins = o.fn(eng)
                        if o.signal:
                            ins.then_inc(o.tok[0], o.tok[2])
                    if ename in dsem:
                        n = prog.ndma[ename]
                        for i in range(KDMA):
                            c = (n - i + KDMA - 1) // KDMA
                            if c > 0 and waited.get(dsem[ename][i].num, 0) < 16 * c:
                                eng.wait_ge(dsem[ename][i], 16 * c)
                return f

            block.tensor(body("pe"))
            block.scalar(body("act"))
            block.vector(body("dve"))
            block.gpsimd(body("pool"))
            block.sync(body("sp"))


class TL:
    __slots__ = ("t", "b")

    def __init__(self, t, name=""):
        self.t = t
        self.b = Buf(name)


class SB:
    BASE = 16512
    TOP = 229344

    def __init__(self, nc):
        self.nc = nc
        self.off = SB.BASE
        self.n = 0

    def tile(self, shape, dt, name="t"):
        per = 1
        for s in shape[1:]:
            per *= s
        nbytes = per * (2 if dt == BF16 else 4)
        nbytes = (nbytes + 31) // 32 * 32
        self.n += 1
        t = self.nc.alloc_sbuf_tensor_at("%s_%d" % (name, self.n), list(shape), dt, offset=self.off)
        self.off += nbytes
        assert self.off <= SB.TOP, ("SBUF overflow", name, self.off)
        return TL(t, name)

    def mark(self):
        return self.off

    def reset(self, m):
        self.off = m


C_ID, C_TRI, C_MASKT, C_BLK, C_B64, C_RDT, C_KDN, C_QDT, C_HM = 0, 128, 256, 384, 640, 768, 1280, 1408, 1536
C_RP = 1540


def host_consts(T):
    nch = T // 128
    ncst = C_RP + nch + 1
    c = np.zeros((128, ncst), np.float32)
    r = np.arange(128)
    c[:, C_ID:C_ID + 128] = np.eye(128, dtype=np.float32)
    c[:, C_TRI:C_TRI + 128] = (r[:, None] <= r[None, :]).astype(np.float32)
    c[:, C_MASKT:C_MASKT + 128] = np.where(r[:, None] <= r[None, :], 0.0, -30000.0)
    hh = r // 32
    for h in range(4):
        c[:, C_HM + h] = (hh == h)
    c[:, C_BLK:C_BLK + 256] = (hh[:, None] == (np.arange(256) // 64)[None, :])
    c[:, C_B64:C_B64 + 128] = ((r // 64)[:, None] == (r // 64)[None, :])
    gam = 1.0 - 2.0 ** (-5.0 - np.arange(4, dtype=np.float64))
    lg = np.log(gam)
    for h in range(4):
        dif = r[None, :] - r[:, None]
        c[:, C_RDT + h * 128:C_RDT + (h + 1) * 128] = np.where(dif >= 0, np.exp(lg[h] * np.maximum(dif, 0)), 0.0)
        c[:, C_KDN + h * 32:C_KDN + (h + 1) * 32] = np.exp(lg[h] * (127 - r))[:, None]
    c[:, C_QDT:C_QDT + 128] = np.exp(lg[hh][:, None] * (r[None, :] + 1.0))
    for n in range(nch + 1):
        c[:, C_RP + n] = np.exp(lg[hh] * 128.0 * n)
    return c


def host_rope(T, pos0):
    out = np.zeros((128, 4, T), np.float32)
    pos = (pos0 + np.arange(T)).astype(np.float32)
    inv = (10000.0 ** (-(np.arange(0, 64, 2, dtype=np.float32) / 64))).astype(np.float32)
    ang = pos[None, :] * inv[:, None]
    out[0:32, 0], out[32:64, 0] = np.cos(ang), np.cos(ang)
    out[0:32, 1], out[32:64, 1] = np.sin(ang), np.sin(ang)
    inv2 = (10000.0 ** (-(np.arange(0, 32, 2, dtype=np.float32) / 32))).astype(np.float32)
    ang2 = pos[None, :] * inv2[:, None]
    c2 = np.concatenate([np.cos(ang2), np.cos(ang2)], 0)
    s2 = np.concatenate([np.sin(ang2), np.sin(ang2)], 0)
    out[:, 2] = np.tile(c2, (4, 1))
    out[:, 3] = np.tile(s2, (4, 1))
    return out


V_AN, V_FN, V_QN, V_KVN, V_ON, V_BG, V_CW, V_CB, V_PER = 0, 8, 16, 19, 21, 29, 30, 96, 118


def host_vec(inp, nl0, nl):
    v = np.zeros((128, nl * V_PER), np.float32)

    def cols(a):
        return np.ascontiguousarray(a.reshape(-1, 128).T)
    for i in range(nl):
        l = nl0 + i
        o = i * V_PER
        v[:, o + V_AN:o + V_AN + 8] = cols(inp["attn_norm"][l])
        v[:, o + V_FN:o + V_FN + 8] = cols(inp["ffn_norm"][l])
        v[:, o + V_QN:o + V_QN + 3] = cols(inp["mla_q_norm"][l])
        v[:, o + V_KVN:o + V_KVN + 2] = cols(inp["mla_kv_norm"][l])
        v[:, o + V_ON:o + V_ON + 4] = cols(inp["mla_out_norm"][l])
        v[:, o + V_ON + 4:o + V_ON + 6] = cols(inp["gla_out_norm"][l])
        v[:, o + V_ON + 6:o + V_ON + 8] = cols(inp["ret_out_norm"][l])
        v[:, o + V_BG:o + V_BG + 1] = cols(inp["gla_b_gate"][l])
        for k in range(3):
            v[:, o + V_CW + k * NFC:o + V_CW + (k + 1) * NFC] = cols(inp["ffn_conv_w"][l, k])
        v[:, o + V_CB:o + V_CB + NFC] = cols(inp["ffn_conv_b"][l])
    return v


class Ctx:
    pass


def r3(ap, **kw):
    return ap.rearrange("p (a b) -> p a b", **kw)


def build(T, nl, final_norm=True, dbg=None, ext=(), stop_after=None, stop_layer=0):
    assert T % 512 == 0
    NT = T // 512
    NCH = T // 128
    nc = bass.Bass("TRN2", target_bir_lowering=False)
    p = Prog(nc)
    sb = SB(nc)
    c = Ctx()
    c.nc, c.p, c.sb, c.T, c.NT, c.NCH = nc, p, sb, T, NT, NCH
    c.stop = None

    def din(name, shape, dt=F32):
        return nc.dram_tensor(name, list(shape), dt, kind="ExternalInput")

    def dscr(name, shape, dt):
        if name in ext:
            return nc.dram_tensor(name, list(shape), dt, kind="ExternalOutput")
        return nc.dram_tensor(name, list(shape), dt)

    c.x = din("x", [T, D])
    c.w_in = din("w_in", [nl, D, DIN])
    c.w_uq = din("w_uq", [nl, 384, 768])
    c.w_ukv = din("w_ukv", [nl, 256, 1024])
    c.w_out = din("w_out", [nl, D, D])
    c.w_up = din("w_up", [nl, D, 2 * DFF])
    c.w_down = din("w_down", [nl, DFF, D])
    c.w_gate = din("w_gate", [nl, 16, 128])
    c.vec = din("vec", [128, nl * V_PER])
    ncst = C_RP + NCH + 1
    c.cst = din("cst", [128, ncst])
    c.rope = din("rope", [128, 4, T])
    c.flag = din("flag", [128, 1])
    c.fnb = din("fnb", [128, D])
    c.out = nc.dram_tensor("out", [T, D], F32, kind="ExternalOutput")
    c.xres = dscr("xres", [T, D], F32)
    c.dT = dscr("dT", [128, 11, T], BF16)
    c.logaT = dscr("logaT", [128, T], F32)
    c.dN = dscr("dN", [T, 512], BF16)
    c.xb1 = dscr("xb1", [128, 2 * T], BF16)
    c.xg1 = dscr("xg1", [256, 2 * T], BF16)
    c.xb2 = dscr("xb2", [128, T], BF16)
    c.xg2 = dscr("xg2", [256, T], BF16)
    c.oint = dscr("oint", [2, 128, 2, T], F32)
    c.qt = dscr("qt", [2, 128, T], BF16)
    c.sx = dscr("sx", [128, 512], F32)
    c.sgat = dscr("sgat", [256, 512], F32)
    c.yT = dscr("yT", [128, 8, T], BF16)
    c.hT = dscr("hT", [128, 8, T], BF16)
    c.halo = dscr("halo", [128, 2 * NFC], F32)
    c.hgat = dscr("hgat", [256, 2 * NFC], F32)
    c.gT = dscr("gT", [128, NFC, T], BF16)
    c.dbg = None
    if dbg is not None:
        c.dbg = nc.dram_tensor("dbg", list(dbg), F32, kind="ExternalOutput")

    c.ps = [TL(nc.alloc_psum_tensor("ps%d" % i, [128, 512], F32), "ps%d" % i) for i in range(7)]
    _sv = nc.psum_base
    c.psb = TL(nc.alloc_psum_tensor("psb", [128, 1024], BF16), "psb")
    nc.psum_base = _sv
    _ps7 = TL(nc.alloc_psum_tensor("ps7", [128, 512], F32), "ps7")
    _ps7.b = c.psb.b
    c.ps.append(_ps7)
    c.psi = 0

    def nps():
        t = c.ps[c.psi % 7]
        c.psi += 1
        return t
    c.nps = nps

    c.cstf = sb.tile([128, ncst], F32, "cstf")
    p.dma(c.cstf.t[:], c.cst[:, :], writes=[c.cstf.b])
    c.vecs = sb.tile([128, nl * V_PER], F32, "vecs")
    p.dma(c.vecs.t[:], c.vec[:, :], writes=[c.vecs.b])
    c.flg = sb.tile([128, 1], F32, "flag")
    p.dma(c.flg.t[:], c.flag[:, :], writes=[c.flg.b])
    c.cb = sb.tile([128, 1540], BF16, "cstb")
    p.cp("dve", c.cb.t[:], c.cstf.t[:, 0:1540], reads=[c.cstf.b], writes=[c.cb.b])
    c.ones = sb.tile([128, 128], BF16, "ones")
    p.memset("pool", c.ones.t[:], 1.0, writes=[c.ones.b])
    c.fones = sb.tile([128, 128], BF16, "fones")
    p.ts("dve", c.fones.t[:], c.ones.t[:], c.flg.t[:, 0:1], None, ALU.mult, reads=[c.ones.b, c.flg.b], writes=[c.fones.b])
    c.tri4 = sb.tile([128, 512], F32, "tri4")
    for h in range(4):
        p.cp("pool", c.tri4.t[:, h * 128:(h + 1) * 128], c.cstf.t[:, C_TRI:C_TRI + 128], reads=[c.cstf.b], writes=[c.tri4.b])
    c.zero = sb.tile([128, 256], F32, "zero")
    p.memset("pool", c.zero.t[:], 0.0, writes=[c.zero.b])
    zb = sb.tile([128, 512], BF16, "zb")
    p.memset("pool", zb.t[:], 0.0, writes=[zb.b])
    for a0 in range(0, T, 512):
        p.dma(c.xb2[64:128, a0:a0 + 512], zb.t[64:128, :], reads=[zb.b])
    c.persist_mark = sb.mark()

    xin = c.x
    for li in range(nl):
        c.li = li
        c.vo = li * V_PER
        if li == stop_layer:
            c.stop = stop_after
        stop_after_ = stop_after if li == stop_layer else None
        phase_A(c, xin)
        if stop_after_ == "A":
            break
        exchange(c, c.xb1, c.xg1)
        exchange(c, c.xb2, c.xg2)
        if stop_after_ == "X":
            break
        phase_B(c)
        if stop_after_ in ("B", "B1", "B2", "B2x"):
            break
        phase_C(c)
        if stop_after_ == "C":
            break
        phase_D(c, xin, last=(li == nl - 1) and final_norm)
        xin = c.xres
    if not final_norm:
        pass
    p.barrier()
    p.emit()
    return nc


def exchange(c, src, dst):
    p = c.p
    p.barrier()
    sap, dap = src.ap().opt(), dst.ap().opt()
    p.op("pool", lambda e: e.collective_compute("AllGather", ALU.bypass, replica_groups=[[0, 1], [2, 3], [4, 5], [6, 7]],
                                                ins=[sap], outs=[dap]), kind="cc")
    p.barrier()


def rstd_from_ss(c, out_ap, ss_ap, n, reads, writes):
    p = c.p
    p.act(out_ap, ss_ap, AF.Sqrt, reads=reads, writes=writes, bias=EPS, scale=1.0 / n)
    p.op("dve", lambda e: e.reciprocal(out_ap, out_ap), reads=writes, writes=writes)


class Alt:
    def __init__(self, engs):
        self.engs = engs
        self.i = 0

    def __call__(self):
        e = self.engs[self.i % len(self.engs)]
        self.i += 1
        return e


def scaled_copy(c, eng, out, in_, scal, reads, writes):
    p = c.p
    if eng == "act":
        p.act(out, in_, AF.Copy, reads=reads, writes=writes, scale=scal)
    else:
        p.ts(eng, out, in_, scal, None, ALU.mult, reads=reads, writes=writes)


WX = 2576
S32 = 32 ** -0.5


def phase_A(c, xin):
    nc, p, sb, T, NT = c.nc, c.p, c.sb, c.T, c.NT
    li, vo = c.li, c.vo
    p.barrier()
    sb.reset(c.persist_mark)
    vec = c.vecs
    gv = sb.tile([128, 4, 8], F32, "gv")
    g0 = vec.t[:, vo + V_AN:vo + V_AN + 8]
    p.cp("dve", gv.t[:, 0, :], g0, reads=[vec.b], writes=[gv.b])
    p.ts("dve", gv.t[:, 1, :], g0, -1.0, None, ALU.mult, reads=[vec.b], writes=[gv.b])
    p.ts("dve", gv.t[:, 2, :], g0, S32, None, ALU.mult, reads=[vec.b], writes=[gv.b])
    p.ts("dve", gv.t[:, 3, :], g0, -S32, None, ALU.mult, reads=[vec.b], writes=[gv.b])
    nbg = sb.tile([128, 1], F32, "nbg")
    p.ts("dve", nbg.t[:], vec.t[:, vo + V_BG:vo + V_BG + 1], -1.0, None, ALU.mult, reads=[vec.b], writes=[nbg.b])
    W = sb.tile([128, 8, WX], BF16, "winx")
    stg = [sb.tile([128, DIN], F32, "wstg") for _ in range(2)]
    alt = Alt(["act", "dve"])
    for k in range(8):
        st = stg[k % 2]
        p.dma(st.t[:], c.w_in[li, k * 128:(k + 1) * 128, :], writes=[st.b])
        g, gn, gs, gsn = (gv.t[:, j, k:k + 1] for j in range(4))

        def cv(d0, d1, s0, s1, sc):
            scaled_copy(c, alt(), W.t[:, k, d0:d1], st.t[:, s0:s1], sc, [st.b, gv.b], [W.b])

        def cvrot(d0, s0, nh, half, sp, sn):
            dv = W.t[:, k, d0:d0 + nh * 2 * half].rearrange("p (h two d) -> p h two d", h=nh, two=2)
            sv = st.t[:, s0:s0 + nh * 2 * half].rearrange("p (h two d) -> p h two d", h=nh, two=2)
            scaled_copy(c, alt(), dv[:, :, 0, :], sv[:, :, 1, :], sn, [st.b, gv.b], [W.b])
            scaled_copy(c, alt(), dv[:, :, 1, :], sv[:, :, 0, :], sp, [st.b, gv.b], [W.b])
        cv(0, 704, 0, 704, g)
        cvrot(704, 640, 1, 32, g, gn)
        cv(768, 896, 704, 832, gs)
        cv(896, 1024, 832, 960, g)
        cv(1024, 1040, 1216, 1232, g)
        cv(1040, 1296, 1232, 1488, g)
        cv(1296, 1424, 1488, 1616, g)
        cvrot(1424, 1488, 4, 16, g, gn)
        cv(1552, 1680, 1616, 1744, gs)
        cvrot(1680, 1616, 4, 16, gs, gsn)
        cv(1808, 2064, 2000, 2256, g)
        cv(2064, 2320, 960, 1216, g)
        cv(2320, 2576, 1744, 2000, g)
    wgf = sb.tile([16, 128], F32, "wgf")
    p.dma(wgf.t[:], c.w_gate[li, :, :], writes=[wgf.b])
    wg = sb.tile([16, 128], BF16, "wg")
    p.cp("dve", wg.t[:], wgf.t[:], reads=[wgf.b], writes=[wg.b])

    xt = [sb.tile([128, D], F32, "xt") for _ in range(2)]
    junk = sb.tile([128, D], BF16, "junk")
    ssq = [sb.tile([128, 2], F32, "ssq") for _ in range(2)]
    hN = [sb.tile([128, D], BF16, "hN") for _ in range(2)]
    hT = [sb.tile([128, 8, 512], BF16, "hT") for _ in range(2)]
    zf = [sb.tile([128, 3, 512], F32, "zf") for _ in range(2)]
    sq = [sb.tile([128, 3, 512], BF16, "sq") for _ in range(2)]
    rs = [sb.tile([128, 512], F32, "rs") for _ in range(2)]
    zn = [sb.tile([128, 3, 512], BF16, "zn") for _ in range(2)]
    rp = sb.tile([128, 4, 512], F32, "ropeA")
    tA = [sb.tile([128, 512], F32, "tA") for _ in range(2)]
    tB = [sb.tile([128, 512], F32, "tB") for _ in range(2)]
    ob = [sb.tile([128, 512], BF16, "obA") for _ in range(4)]
    glr = sb.tile([16, 512], BF16, "glr")
    ex = sb.tile([128, 512], F32, "exA")
    la = [sb.tile([128, 512], F32, "laA") for _ in range(2)]
    vN = [sb.tile([128, 512], BF16, "vN") for _ in range(2)]
    cnt = {"ob": 0, "sub": 0, "g": 0}
    idb = c.cb.t[:, C_ID:C_ID + 128]

    def nob():
        t = ob[cnt["ob"] % 4]
        cnt["ob"] += 1
        return t

    def proj(c0, M, hTt):
        ps = c.nps()
        for k in range(8):
            p.mm(ps.t[0:M, :], W.t[:, k, c0:c0 + M], hTt.t[:, k, :], k == 0, k == 7, reads=[W.b, hTt.b], writes=[ps.b])
        return ps

    def load_x(i):
        if i < 4 * NT:
            p.dma(xt[i % 2].t[:], xin[i * 128:(i + 1) * 128, :], writes=[xt[i % 2].b])
    load_x(0)
    for tt in range(NT):
        t0 = tt * 512
        hTt = hT[tt % 2]
        p.dma(rp.t[:], c.rope[:, :, t0:t0 + 512], writes=[rp.b])
        for s in range(4):
            i = cnt["sub"]
            cnt["sub"] += 1
            x_, ss_, hN_ = xt[i % 2], ssq[i % 2], hN[i % 2]
            load_x(i + 1)
            p.act(junk.t[:], x_.t[:], AF.Square, reads=[x_.b], writes=[junk.b, ss_.b], accum=ss_.t[:, 0:1])
            rstd_from_ss(c, ss_.t[:, 1:2], ss_.t[:, 0:1], D, [ss_.b], [ss_.b])
            p.act(hN_.t[:], x_.t[:], AF.Copy, reads=[x_.b, ss_.b], writes=[hN_.b], scale=ss_.t[:, 1:2])
            for k in range(8):
                p.tr(c.psb.t[:, k * 128:(k + 1) * 128], hN_.t[:, k * 128:(k + 1) * 128], idb, reads=[hN_.b, c.cb.b], writes=[c.psb.b])
            p.cp("dve", hTt.t[:, :, s * 128:(s + 1) * 128], r3(c.psb.t[:], a=8), reads=[c.psb.b], writes=[hTt.b])

        for (c0, nchk, n, which) in ((0, 3, 384, "cq"), (384, 2, 256, "ckv")):
            gi = cnt["g"]
            cnt["g"] += 1
            zf_, sq_, rs_, zn_ = zf[gi % 2], sq[gi % 2], rs[gi % 2], zn[gi % 2]
            for j in range(nchk):
                ps = proj(c0 + j * 128, 128, hTt)
                p.cp("act", zf_.t[:, j, :], ps.t[:], reads=[ps.b], writes=[zf_.b])
                p.tt("pool", sq_.t[:, j, :], zf_.t[:, j, :], zf_.t[:, j, :], ALU.mult, reads=[zf_.b], writes=[sq_.b])
            pss = c.nps()
            for j in range(nchk):
                p.mm(pss.t[:], c.ones.t[:], sq_.t[:, j, :], j == 0, j == nchk - 1, reads=[c.ones.b, sq_.b], writes=[pss.b])
            rstd_from_ss(c, rs_.t[:], pss.t[:], n, [pss.b], [rs_.b])
            for j in range(nchk):
                p.tt("dve", zn_.t[:, j, :], zf_.t[:, j, :], rs_.t[:], ALU.mult, reads=[zf_.b, rs_.b], writes=[zn_.b])
            if which == "cq":
                p.dma(c.dT[:, 0:3, t0:t0 + 512], zn_.t[:, 0:3, :], reads=[zn_.b])
            else:
                p.dma(r3(c.xb1[:, :], a=2)[:, :, t0:t0 + 512], zn_.t[:, 0:2, :], reads=[zn_.b])

        def roped(c_raw, c_rot, M, ci, si, dst_ap):
            pr_, pt_ = proj(c_raw, M, hTt), proj(c_rot, M, hTt)
            a_, b_ = tA[cnt["g"] % 2], tB[cnt["g"] % 2]
            cnt["g"] += 1
            p.tt("dve", a_.t[0:M, :], pr_.t[0:M, :], rp.t[0:M, ci, :], ALU.mult, reads=[pr_.b, rp.b], writes=[a_.b])
            p.tt("dve", b_.t[0:M, :], pt_.t[0:M, :], rp.t[0:M, si, :], ALU.mult, reads=[pt_.b, rp.b], writes=[b_.b])
            o_ = nob()
            p.tt("pool", o_.t[0:M, :], a_.t[0:M, :], b_.t[0:M, :], ALU.add, reads=[a_.b, b_.b], writes=[o_.b])
            p.dma(dst_ap, o_.t[0:M, :], reads=[o_.b])

        roped(640, 704, 64, 0, 1, c.xb2[0:64, t0:t0 + 512])
        roped(1296, 1424, 128, 2, 3, c.dT[:, 5, t0:t0 + 512])
        roped(1552, 1680, 128, 2, 3, c.dT[:, 6, t0:t0 + 512])

        for (c0, slot) in ((768, 3), (896, 4)):
            ps = proj(c0, 128, hTt)
            o_ = nob()
            p.cp("act", o_.t[:], ps.t[:], reads=[ps.b], writes=[o_.b])
            p.dma(c.dT[:, slot, t0:t0 + 512], o_.t[:], reads=[o_.b])
        for (c0, slot) in ((1040, 7), (1168, 8), (1808, 9), (1936, 10)):
            ps = proj(c0, 128, hTt)
            o_ = nob()
            p.act(o_.t[:], ps.t[:], AF.Silu, reads=[ps.b], writes=[o_.b])
            p.dma(c.dT[:, slot, t0:t0 + 512], o_.t[:], reads=[o_.b])
        ps = proj(1024, 16, hTt)
        p.cp("act", glr.t[:], ps.t[0:16, :], reads=[ps.b], writes=[glr.b])
        ps2 = c.nps()
        p.mm(ps2.t[:], wg.t[:], glr.t[:], True, True, reads=[wg.b, glr.b], writes=[ps2.b])
        p.act(ex.t[:], ps2.t[:], AF.Exp, reads=[ps2.b, nbg.b], writes=[ex.b], bias=nbg.t[:, 0:1], scale=-1.0)
        la_ = la[tt % 2]
        p.act(la_.t[:], ex.t[:], AF.Ln, reads=[ex.b], writes=[la_.b], bias=1.0)
        p.ts("pool", la_.t[:], la_.t[:], -1.0 / 16.0, None, ALU.mult, reads=[la_.b], writes=[la_.b])
        p.dma(c.logaT[:, t0:t0 + 512], la_.t[:], reads=[la_.b])
        for s in range(4):
            ps = c.nps()
            for k in range(8):
                p.mm(ps.t[:], hTt.t[:, k, s * 128:(s + 1) * 128], W.t[:, k, 2064:2576], k == 0, k == 7, reads=[W.b, hTt.b], writes=[ps.b])
            v_ = vN[s % 2]
            p.cp("act" if s % 2 else "dve", v_.t[:], ps.t[:], reads=[ps.b], writes=[v_.b])
            p.dma(c.dN[t0 + s * 128:t0 + (s + 1) * 128, :], v_.t[:], reads=[v_.b])


def phase_B(c):
    nc, p, sb, T, NT, NCH = c.nc, c.p, c.sb, c.T, c.NT, c.NCH
    p.barrier()
    sb.reset(c.persist_mark)
    cf, cbf = c.cstf, c.cb
    idb = cbf.t[:, C_ID:C_ID + 128]
    idf = cf.t[:, C_ID:C_ID + 128]
    trif = cf.t[:, C_TRI:C_TRI + 128]
    blk = cf.t[:, C_BLK:C_BLK + 256]
    U = [sb.tile([128, NCH, 256], F32, "Uall%d" % m) for m in range(2)]
    Ub = [[Buf() for _ in range(NCH)] for m in range(2)]
    dall = sb.tile([128, NCH], F32, "dall")
    Pp = sb.tile([128, NCH], F32, "Pp")
    vpad = [sb.tile([128, 4, 128], BF16, "vpad%d" % m) for m in range(2)]
    for m in range(2):
        p.memset("pool", vpad[m].t[:], 0.0, writes=[vpad[m].b])
    inT = [sb.tile([128, 4, 128], BF16, "inT") for _ in range(2)]
    la = [sb.tile([128, 128], F32, "laB") for _ in range(2)]
    vN = [sb.tile([128, 512], BF16, "vNB") for _ in range(2)]
    laN = sb.tile([128, 128], F32, "laN")
    eneg = sb.tile([128, 128], F32, "eneg")
    epos = sb.tile([128, 128], F32, "epos")
    kt = sb.tile([128, 128], BF16, "kt")
    qtl = [sb.tile([128, 128], BF16, "qtl") for _ in range(2)]
    qtm = sb.tile([128, 4, 128], BF16, "qtm")
    ktN = sb.tile([128, 128], BF16, "ktN")
    Am = [sb.tile([128, 512], BF16, "Am%d" % m) for m in range(2)]
    oev = [sb.tile([128, 2, 128], F32, "oev") for _ in range(4)]
    rqm = sb.tile([128, 4, 128], BF16, "rqm")
    qdec = [sb.tile([128, 128], BF16, "qdec") for _ in range(2)]
    kdN = sb.tile([128, 128], BF16, "kdN")
    stmp = sb.tile([128, 256], F32, "stmp")
    mB1 = sb.mark()

    def load_B(n):
        if n < NCH:
            tk = n * 128
            p.dma(inT[n % 2].t[:], c.dT[:, 3:7, tk:tk + 128], writes=[inT[n % 2].b])
            p.dma(la[n % 2].t[:], c.logaT[:, tk:tk + 128], writes=[la[n % 2].b])
            p.dma(vN[n % 2].t[:], c.dN[tk:tk + 128, :], writes=[vN[n % 2].b])
    load_B(0)
    for n in range(NCH):
        tok = n * 128
        inT_, la_, vN_ = inT[n % 2], la[n % 2], vN[n % 2]
        load_B(n + 1)
        psl = c.nps()
        p.tr(psl.t[:, 0:128], la_.t[:], idf, reads=[la_.b, cf.b], writes=[psl.b])
        p.cp("act", laN.t[:], psl.t[:, 0:128], reads=[psl.b], writes=[laN.b])
        psB = c.nps()
        p.mm(psB.t[:, 0:128], laN.t[:], trif, True, True, reads=[laN.b, cf.b], writes=[psB.b])
        p.act(eneg.t[:], psB.t[:, 0:128], AF.Exp, reads=[psB.b], writes=[eneg.b], scale=-1.0)
        p.act(epos.t[:], psB.t[:, 0:128], AF.Exp, reads=[psB.b], writes=[epos.b])
        p.cp("dve", dall.t[:, n:n + 1], epos.t[:, 127:128], reads=[epos.b], writes=[dall.b])
        p.tt("dve", kt.t[:], inT_.t[:, 1, :], eneg.t[:], ALU.mult, reads=[inT_.b, eneg.b], writes=[kt.b])
        q_ = qtl[n % 2]
        p.tt("pool", q_.t[:], inT_.t[:, 0, :], epos.t[:], ALU.mult, reads=[inT_.b, epos.b], writes=[q_.b])
        p.dma(c.qt[0, :, tok:tok + 128], q_.t[:], reads=[q_.b])
        for h in range(4):
            p.stt("dve" if h % 2 == 0 else "pool", qtm.t[:, h, :], inT_.t[:, 0, :], cf.t[:, C_HM + h:C_HM + h + 1], epos.t[:],
                  ALU.mult, ALU.mult, reads=[inT_.b, epos.b, cf.b], writes=[qtm.b])
        p.tr(c.psb.t[:, 0:128], kt.t[:], idb, reads=[kt.b, cbf.b], writes=[c.psb.b])
        p.cp("act", ktN.t[:], c.psb.t[:, 0:128], reads=[c.psb.b], writes=[ktN.b])
        psU = c.nps()
        p.mm(psU.t[:, 0:256], ktN.t[:], vN_.t[:, 0:256], True, True, reads=[ktN.b, vN_.b], writes=[psU.b])
        p.stt("dve", U[0].t[:, n, :], psU.t[:, 0:256], dall.t[:, n:n + 1], blk, ALU.mult, ALU.mult,
              reads=[psU.b, dall.b, cf.b], writes=[Ub[0][n]])
        psA = c.nps()
        for h in range(4):
            p.mm(psA.t[:, h * 128:(h + 1) * 128], kt.t[:], qtm.t[:, h, :], True, True, reads=[kt.b, qtm.b], writes=[psA.b])
        p.tt("dve", Am[0].t[:], psA.t[:], c.tri4.t[:], ALU.mult, reads=[psA.b, c.tri4.b], writes=[Am[0].b])
        for h in range(4):
            p.ts("pool" if h % 2 == 0 else "dve", rqm.t[:, h, :], inT_.t[:, 2, :], cf.t[:, C_HM + h:C_HM + h + 1], None, ALU.mult,
                 reads=[inT_.b, cf.b], writes=[rqm.b])
        qd_ = qdec[n % 2]
        p.tt("pool", qd_.t[:], inT_.t[:, 2, :], cf.t[:, C_QDT:C_QDT + 128], ALU.mult, reads=[inT_.b, cf.b], writes=[qd_.b])
        p.dma(c.qt[1, :, tok:tok + 128], qd_.t[:], reads=[qd_.b])
        p.tr(c.psb.t[:, 128:256], inT_.t[:, 3, :], idb, reads=[inT_.b, cbf.b], writes=[c.psb.b])
        p.tt("dve", kdN.t[:], c.psb.t[:, 128:256], cf.t[:, C_KDN:C_KDN + 128], ALU.mult, reads=[c.psb.b, cf.b], writes=[kdN.b])
        psU2 = c.nps()
        p.mm(psU2.t[:, 0:256], kdN.t[:], vN_.t[:, 256:512], True, True, reads=[kdN.b, vN_.b], writes=[psU2.b])
        p.tt("dve", U[1].t[:, n, :], psU2.t[:, 0:256], blk, ALU.mult, reads=[psU2.b, cf.b], writes=[Ub[1][n]])
        psA2 = c.nps()
        for h in range(4):
            p.mm(psA2.t[:, h * 128:(h + 1) * 128], inT_.t[:, 3, :], rqm.t[:, h, :], True, True, reads=[inT_.b, rqm.b], writes=[psA2.b])
        p.tt("dve", Am[1].t[:], psA2.t[:], cf.t[:, C_RDT:C_RDT + 512], ALU.mult, reads=[psA2.b, cf.b], writes=[Am[1].b])
        for m in range(2):
            vp = vpad[m]
            src = vN_.t[:, m * 256:(m + 1) * 256].rearrange("p (a two d) -> p a two d", a=2, two=2)
            dst = vp.t[:].rearrange("p (a two) d -> p a two d", two=2)
            p.cp("pool", dst[:, :, 0, 0:64], src[:, :, 0, :], reads=[vN_.b], writes=[vp.b])
            p.cp("pool", dst[:, :, 1, 64:128], src[:, :, 1, :], reads=[vN_.b], writes=[vp.b])
            psO = c.nps()
            for pr in range(2):
                for hh in range(2):
                    h = 2 * pr + hh
                    p.mm(psO.t[:, pr * 128:(pr + 1) * 128], vp.t[:, h, :], Am[m].t[:, h * 128:(h + 1) * 128], hh == 0, hh == 1,
                         reads=[vp.b, Am[m].b], writes=[psO.b])
            o_ = oev[(2 * n + m) % 4]
            p.cp("act", o_.t[:], r3(psO.t[:, 0:256], a=2), reads=[psO.b], writes=[o_.b])
            p.dma(c.oint[m, :, :, tok:tok + 128], o_.t[:], reads=[o_.b])

    if c.stop == "B1":
        return
    p.memset("dve", Pp.t[:, 0:1], 1.0, writes=[Pp.b])
    for n in range(1, NCH):
        p.stt("dve", U[0].t[:, n, :], U[0].t[:, n - 1, :], dall.t[:, n:n + 1], U[0].t[:, n, :], ALU.mult, ALU.add,
              reads=[Ub[0][n - 1], dall.b], writes=[Ub[0][n]])
        p.stt("dve", U[1].t[:, n, :], U[1].t[:, n - 1, :], cf.t[:, C_RP + 1:C_RP + 2], U[1].t[:, n, :], ALU.mult, ALU.add,
              reads=[Ub[1][n - 1], cf.b], writes=[Ub[1][n]])
        p.tt("dve", Pp.t[:, n:n + 1], Pp.t[:, n - 1:n], dall.t[:, n - 1:n], ALU.mult, reads=[dall.b], writes=[Pp.b])
    p.dma(c.sx[:, 0:256], U[0].t[:, NCH - 1, :], reads=[Ub[0][NCH - 1]])
    p.dma(c.sx[:, 256:512], U[1].t[:, NCH - 1, :], reads=[Ub[1][NCH - 1]])
    if c.stop == "B2":
        return
    exchange(c, c.sx, c.sgat)
    if c.stop == "B2x":
        return
    sb.reset(mB1)
    sin = sb.tile([128, 512], F32, "sin")
    p.dma(sin.t[:], c.sgat[0:128, :], writes=[sin.b])
    sinf = sb.tile([128, 512], F32, "sinf")
    p.ts("dve", sinf.t[:], sin.t[:], c.flg.t[:, 0:1], None, ALU.mult, reads=[sin.b, c.flg.b], writes=[sinf.b])
    qT = [sb.tile([128, 512], BF16, "qTB") for _ in range(2)]
    oi = [sb.tile([128, 2, 512], F32, "oiB") for _ in range(2)]
    gt = [sb.tile([128, 2, 512], BF16, "gtB") for _ in range(2)]
    Sp = [sb.tile([128, 256], BF16, "Sp") for _ in range(4)]
    of = [sb.tile([128, 512], F32, "ofB") for _ in range(2)]
    sqo = [sb.tile([128, 512], BF16, "sqoB") for _ in range(2)]
    rs = [sb.tile([128, 512], F32, "rsB") for _ in range(2)]
    of2 = [sb.tile([128, 512], F32, "of2B") for _ in range(2)]
    yo = [sb.tile([128, 512], BF16, "yoB") for _ in range(2)]
    k = 0
    kk = 0
    def load_B3(kx):
        if kx < 2 * NT:
            tt_, m_ = kx // 2, kx % 2
            a0 = tt_ * 512
            p.dma(qT[kx % 2].t[:], c.qt[m_, :, a0:a0 + 512], writes=[qT[kx % 2].b])
            p.dma(oi[kx % 2].t[:], c.oint[m_, :, :, a0:a0 + 512], writes=[oi[kx % 2].b])
            p.dma(gt[kx % 2].t[:], c.dT[:, 7 + 2 * m_:9 + 2 * m_, a0:a0 + 512], writes=[gt[kx % 2].b])
    load_B3(0)
    for tt in range(NT):
        t0 = tt * 512
        for m in range(2):
            qT_, oi_, gt_ = qT[k % 2], oi[k % 2], gt[k % 2]
            k += 1
            load_B3(k)
            psI = [c.nps(), c.nps()]
            for cch in range(4):
                n = 4 * tt + cch
                Sp_ = Sp[(4 * k + cch) % 4]
                if m == 0:
                    psc, rd = Pp.t[:, n:n + 1], [Pp.b]
                else:
                    psc, rd = cf.t[:, C_RP + n:C_RP + n + 1], [cf.b]
                if n > 0:
                    prev, rd2 = U[m].t[:, n - 1, :], [Ub[m][n - 1]]
                else:
                    prev, rd2 = c.zero.t[:], [c.zero.b]
                p.stt("dve" if cch % 2 == 0 else "pool", Sp_.t[:], sinf.t[:, m * 256:(m + 1) * 256], psc, prev, ALU.mult, ALU.add,
                      reads=[sinf.b] + rd + rd2, writes=[Sp_.b])
                for pr in range(2):
                    p.mm(psI[pr].t[:, cch * 128:(cch + 1) * 128], Sp_.t[:, pr * 128:(pr + 1) * 128], qT_.t[:, cch * 128:(cch + 1) * 128],
                         True, True, reads=[Sp_.b, qT_.b], writes=[psI[pr].b])
            for pr in range(2):
                of_, sq_, rs_, of2_, yo_ = of[kk % 2], sqo[kk % 2], rs[kk % 2], of2[kk % 2], yo[kk % 2]
                kk += 1
                p.tt("dve", of_.t[:], psI[pr].t[:], oi_.t[:, pr, :], ALU.add, reads=[psI[pr].b, oi_.b], writes=[of_.b])
                p.act(sq_.t[:], of_.t[:], AF.Square, reads=[of_.b], writes=[sq_.b])
                pss = c.nps()
                p.mm(pss.t[:], cbf.t[:, C_B64:C_B64 + 128], sq_.t[:], True, True, reads=[cbf.b, sq_.b], writes=[pss.b])
                rstd_from_ss(c, rs_.t[:], pss.t[:], 64, [pss.b], [rs_.b])
                p.tt("dve", of2_.t[:], of_.t[:], rs_.t[:], ALU.mult, reads=[of_.b, rs_.b], writes=[of2_.b])
                p.tt("pool", yo_.t[:], of2_.t[:], gt_.t[:, pr, :], ALU.mult, reads=[of2_.b, gt_.b], writes=[yo_.b])
                p.dma(c.yT[:, 4 + 2 * m + pr, t0:t0 + 512], yo_.t[:], reads=[yo_.b])


def phase_C(c):
    nc, p, sb, T, NT, NCH = c.nc, c.p, c.sb, c.T, c.NT, c.NCH
    li, vo = c.li, c.vo
    p.barrier()
    sb.reset(c.persist_mark)
    vec, cf, cbf = c.vecs, c.cstf, c.cb
    idb = cbf.t[:, C_ID:C_ID + 128]
    maskb = cbf.t[:, C_MASKT:C_MASKT + 128]
    NKT = 2 * T // 512
    NKB = 2 * T // 128
    gq = sb.tile([128, 2, 3], F32, "gqC")
    p.ts("dve", gq.t[:, 0, :], vec.t[:, vo + V_QN:vo + V_QN + 3], SCALE_MLA, None, ALU.mult, reads=[vec.b], writes=[gq.b])
    p.ts("dve", gq.t[:, 1, :], vec.t[:, vo + V_QN:vo + V_QN + 3], -SCALE_MLA, None, ALU.mult, reads=[vec.b], writes=[gq.b])
    wq = sb.tile([128, 3, 1024], BF16, "wq")
    wkv = sb.tile([128, 2, 1024], BF16, "wkv")
    stg = [sb.tile([128, 1024], F32, "stgC") for _ in range(2)]
    alt = Alt(["act", "dve"])
    for k in range(3):
        st = stg[k % 2]
        p.dma(st.t[:, 0:768], c.w_uq[li, k * 128:(k + 1) * 128, :], writes=[st.b])
        scaled_copy(c, alt(), wq.t[:, k, 0:768], st.t[:, 0:768], gq.t[:, 0, k:k + 1], [st.b, gq.b], [wq.b])
        for h in range(4):
            s0 = h * 192 + 128
            scaled_copy(c, alt(), wq.t[:, k, 768 + h * 64:768 + h * 64 + 32], st.t[:, s0 + 32:s0 + 64], gq.t[:, 1, k:k + 1], [st.b, gq.b], [wq.b])
            scaled_copy(c, alt(), wq.t[:, k, 768 + h * 64 + 32:768 + h * 64 + 64], st.t[:, s0:s0 + 32], gq.t[:, 0, k:k + 1], [st.b, gq.b], [wq.b])
    for k in range(2):
        st = stg[(k + 1) % 2]
        p.dma(st.t[:], c.w_ukv[li, k * 128:(k + 1) * 128, :], writes=[st.b])
        scaled_copy(c, alt(), wkv.t[:, k, :], st.t[:], vec.t[:, vo + V_KVN + k:vo + V_KVN + k + 1], [st.b, vec.b], [wkv.b])
    cq = sb.tile([128, 3, T], BF16, "cqC")
    ckv = sb.tile([128, 2, 2 * T], BF16, "ckvC")
    KrT = sb.tile([64, 2 * T], BF16, "KrT")
    PC = 1024 if T % 1024 == 0 else 512
    for a0 in range(0, T, PC):
        p.dma(cq.t[:, :, a0:a0 + PC], c.dT[:, 0:3, a0:a0 + PC], writes=[cq.b])
        p.dma(ckv.t[:, :, a0:a0 + PC], r3(c.xg1[0:128, :], a=2)[:, :, a0:a0 + PC], writes=[ckv.b])
        p.dma(ckv.t[:, :, T + a0:T + a0 + PC], r3(c.xb1[:, :], a=2)[:, :, a0:a0 + PC], writes=[ckv.b])
        p.dma(KrT.t[:, a0:a0 + PC], c.xg2[0:64, a0:a0 + PC], writes=[KrT.b])
        p.dma(KrT.t[:, T + a0:T + a0 + PC], c.xb2[0:64, a0:a0 + PC], writes=[KrT.b])
    KnT = sb.tile([128, 2 * T], BF16, "KnT")
    V = sb.tile([128, NKB, 128], BF16, "V")
    QnT = sb.tile([128, T], BF16, "QnT")
    QrT = sb.tile([64, T], BF16, "QrT")
    sqa = [sb.tile([128, 512], BF16, "sqa") for _ in range(2)]
    rpt = [sb.tile([64, 2, 512], F32, "rptC") for _ in range(2)]
    ta = [sb.tile([64, 512], F32, "taC") for _ in range(2)]
    tb = [sb.tile([64, 512], F32, "tbC") for _ in range(2)]
    mx = sb.tile([128, 4, max(NKT, NT)], F32, "mxC")
    red = sb.tile([128, 8], F32, "redC")
    bias = sb.tile([128, NT], F32, "biasC")
    PT = [sb.tile([128, 512], BF16, "PT") for _ in range(6)]
    rden = [sb.tile([128, 512], F32, "rden") for _ in range(2)]
    uu = [sb.tile([128, 512], F32, "uu") for _ in range(2)]
    squ = [sb.tile([128, 512], BF16, "squ") for _ in range(2)]
    rsu = [sb.tile([128, 512], F32, "rsu") for _ in range(2)]
    yo = [sb.tile([128, 512], BF16, "yoC") for _ in range(2)]
    dacc = [[sb.tile([128, 512], F32, "dacc%d" % j) for j in range(2)] for _ in range(2)]
    onesf = sb.tile([128, 128], F32, "onesf")
    p.memset("pool", onesf.t[:], 1.0, writes=[onesf.b])
    biasp = sb.tile([128, NT], F32, "biasP")
    fm1 = sb.tile([128, 1], F32, "fm1")
    p.ts("dve", fm1.t[:], c.flg.t[:, 0:1], -1.0, 30000.0, ALU.add, ALU.mult, reads=[c.flg.b], writes=[fm1.b])
    k_ = {"sq": 0, "pt": 0, "s": 0, "e": 0}

    def nsq():
        t = sqa[k_["sq"] % 2]
        k_["sq"] += 1
        return t

    def rmax(dst_ap, ps, M, rd, wr):
        p.op("dve", lambda e: e.tensor_reduce(dst_ap, ps.t[0:M, :] if M < 128 else ps.t[:], AX.X, ALU.max), reads=rd, writes=wr)

    for kt in range(NKT):
        s_ = nsq()
        p.tt("pool", s_.t[0:64, :], KrT.t[:, kt * 512:(kt + 1) * 512], KrT.t[:, kt * 512:(kt + 1) * 512], ALU.mult, reads=[KrT.b], writes=[s_.b])
        ps = c.nps()
        p.mm(ps.t[:], c.ones.t[0:64, :], s_.t[0:64, :], True, True, reads=[c.ones.b, s_.b], writes=[ps.b])
        rmax(mx.t[:, 1, kt:kt + 1], ps, 128, [ps.b], [mx.b])
    p.op("dve", lambda e: e.tensor_reduce(red.t[:, 1:2], mx.t[:, 1, 0:NKT], AX.X, ALU.max), reads=[mx.b], writes=[red.b])

    for h in range(4):
        for kt in range(NKT):
            ps = c.nps()
            for k in range(2):
                p.mm(ps.t[:], wkv.t[:, k, h * 256:h * 256 + 128], ckv.t[:, k, kt * 512:(kt + 1) * 512], k == 0, k == 1,
                     reads=[wkv.b, ckv.b], writes=[ps.b])
            p.cp("act", KnT.t[:, kt * 512:(kt + 1) * 512], ps.t[:], reads=[ps.b], writes=[KnT.b])
            s_ = nsq()
            p.tt("pool", s_.t[:], KnT.t[:, kt * 512:(kt + 1) * 512], KnT.t[:, kt * 512:(kt + 1) * 512], ALU.mult, reads=[KnT.b], writes=[s_.b])
            ps2 = c.nps()
            p.mm(ps2.t[:], c.ones.t[:], s_.t[:], True, True, reads=[c.ones.b, s_.b], writes=[ps2.b])
            rmax(mx.t[:, 0, kt:kt + 1], ps2, 128, [ps2.b], [mx.b])
        p.op("dve", lambda e: e.tensor_reduce(red.t[:, 0:1], mx.t[:, 0, 0:NKT], AX.X, ALU.max), reads=[mx.b], writes=[red.b])
        p.tt("dve", red.t[:, 2:3], red.t[:, 0:1], red.t[:, 1:2], ALU.add, reads=[red.b], writes=[red.b])
        for g in range(NKB // 4):
            ps = c.nps()
            for j in range(4):
                kb = 4 * g + j
                for k in range(2):
                    p.mm(ps.t[:, j * 128:(j + 1) * 128], ckv.t[:, k, kb * 128:(kb + 1) * 128], wkv.t[:, k, h * 256 + 128:(h + 1) * 256],
                         k == 0, k == 1, reads=[wkv.b, ckv.b], writes=[ps.b])
            dst = V.t[:, 4 * g:4 * g + 4, :]
            p.cp("act" if g % 2 else "dve", dst, r3(ps.t[:], a=4), reads=[ps.b], writes=[V.b])
        for tt in range(NT):
            t0 = tt * 512
            rp_ = rpt[tt % 2]
            p.dma(rp_.t[:], c.rope[0:64, 0:2, t0:t0 + 512], writes=[rp_.b])
            ps = c.nps()
            for k in range(3):
                p.mm(ps.t[:], wq.t[:, k, h * 192:h * 192 + 128], cq.t[:, k, t0:t0 + 512], k == 0, k == 2, reads=[wq.b, cq.b], writes=[ps.b])
            p.cp("act", QnT.t[:, t0:t0 + 512], ps.t[:], reads=[ps.b], writes=[QnT.b])
            psr, pst = c.nps(), c.nps()
            for k in range(3):
                p.mm(psr.t[0:64, :], wq.t[:, k, h * 192 + 128:h * 192 + 192], cq.t[:, k, t0:t0 + 512], k == 0, k == 2, reads=[wq.b, cq.b], writes=[psr.b])
            for k in range(3):
                p.mm(pst.t[0:64, :], wq.t[:, k, 768 + h * 64:768 + h * 64 + 64], cq.t[:, k, t0:t0 + 512], k == 0, k == 2, reads=[wq.b, cq.b], writes=[pst.b])
            a_, b_ = ta[tt % 2], tb[tt % 2]
            p.tt("dve", a_.t[:], psr.t[0:64, :], rp_.t[:, 0, :], ALU.mult, reads=[psr.b, rp_.b], writes=[a_.b])
            p.tt("dve", b_.t[:], pst.t[0:64, :], rp_.t[:, 1, :], ALU.mult, reads=[pst.b, rp_.b], writes=[b_.b])
            p.tt("pool", QrT.t[:, t0:t0 + 512], a_.t[:], b_.t[:], ALU.add, reads=[a_.b, b_.b], writes=[QrT.b])
            s_ = nsq()
            p.tt("pool", s_.t[:], QnT.t[:, t0:t0 + 512], QnT.t[:, t0:t0 + 512], ALU.mult, reads=[QnT.b], writes=[s_.b])
            ps2 = c.nps()
            p.mm(ps2.t[:], c.ones.t[:], s_.t[:], True, True, reads=[c.ones.b, s_.b], writes=[ps2.b])
            rmax(mx.t[:, 2, tt:tt + 1], ps2, 128, [ps2.b], [mx.b])
            s2_ = nsq()
            p.tt("pool", s2_.t[0:64, :], QrT.t[:, t0:t0 + 512], QrT.t[:, t0:t0 + 512], ALU.mult, reads=[QrT.b], writes=[s2_.b])
            ps3 = c.nps()
            p.mm(ps3.t[:], c.ones.t[0:64, :], s2_.t[0:64, :], True, True, reads=[c.ones.b, s2_.b], writes=[ps3.b])
            rmax(mx.t[:, 3, tt:tt + 1], ps3, 128, [ps3.b], [mx.b])
        p.tt("dve", bias.t[:, 0:NT], mx.t[:, 2, 0:NT], mx.t[:, 3, 0:NT], ALU.add, reads=[mx.b], writes=[bias.b])
        p.ts("dve", bias.t[:, 0:NT], bias.t[:, 0:NT], red.t[:, 2:3], None, ALU.mult, reads=[bias.b, red.b], writes=[bias.b])
        p.act(bias.t[:, 0:NT], bias.t[:, 0:NT], AF.Ln, reads=[bias.b], writes=[bias.b])
        p.act(bias.t[:, 0:NT], bias.t[:, 0:NT], AF.Exp, reads=[bias.b], writes=[bias.b], scale=0.5)
        p.ts("dve", bias.t[:, 0:NT], bias.t[:, 0:NT], -1.0, None, ALU.mult, reads=[bias.b], writes=[bias.b])
        p.ts("dve", biasp.t[:, 0:NT], bias.t[:, 0:NT], fm1.t[:, 0:1], None, ALU.add, reads=[bias.b, fm1.b], writes=[biasp.b])

        if h == 0:
            pend = {"f": None}
        for qt in range(NT):
            q0 = qt * 512
            accO, accD = c.ps[k_["e"] % 2], c.ps[2 + k_["e"] % 2]
            blocks = [(kb, -1) for kb in range(NCH)] + [(NCH + kb, (kb - 4 * qt) if kb >= 4 * qt else -1) for kb in range(4 * qt + 4)]
            nb = len(blocks)
            stiles = [None] * nb

            G = 2
            groups = [list(range(a, min(a + G, nb))) for a in range(0, nb, G)]

            def emit_Sg(g):
                idxs = groups[g]
                banks = []
                for j_, i in enumerate(idxs):
                    S = c.ps[4 + 2 * (g % 2) + j_]
                    stiles[i] = S
                    banks.append(S.b)
                first = True
                for i in idxs:
                    kb, dj = blocks[i]
                    S = stiles[i]
                    lo = 0 if dj < 0 else 128 * dj
                    p.mm(S.t[:, lo:512], KnT.t[:, kb * 128:(kb + 1) * 128], QnT.t[:, q0 + lo:q0 + 512], True, False,
                         reads=[KnT.b, QnT.b], writes=(banks if first else [S.b]))
                    first = False
                for i in idxs:
                    kb, dj = blocks[i]
                    S = stiles[i]
                    lo = 0 if dj < 0 else 128 * dj
                    p.mm(S.t[:, lo:512], KrT.t[:, kb * 128:(kb + 1) * 128], QrT.t[:, q0 + lo:q0 + 512], False, dj < 0,
                         reads=[KrT.b, QrT.b], writes=[S.b])
                for i in idxs:
                    kb, dj = blocks[i]
                    S = stiles[i]
                    lo = 0 if dj < 0 else 128 * dj
                    if dj >= 0:
                        p.mm(S.t[:, lo:lo + 128], idb, maskb, False, True, reads=[cbf.b], writes=[S.b])

            def emit_PVg(g):
                idxs = groups[g]
                Ps = []
                for i in idxs:
                    kb, dj = blocks[i]
                    S = stiles[i]
                    lo = 0 if dj < 0 else 128 * dj
                    P_ = PT[k_["pt"] % 6]
                    k_["pt"] += 1
                    Ps.append(P_)
                    bt = biasp if kb < NCH else bias
                    p.act(P_.t[:, lo:512], S.t[:, lo:512], AF.Exp, reads=[S.b, bt.b], writes=[P_.b], bias=bt.t[:, qt:qt + 1], scale=1.0)
                first = True
                for j_, (i, P_) in enumerate(zip(idxs, Ps)):
                    kb, dj = blocks[i]
                    lo = 0 if dj < 0 else 128 * dj
                    rd = [V.b] + ([x.b for x in Ps] if first else [P_.b])
                    first = False
                    p.mm(accO.t[:, lo:512], V.t[:, kb, :], P_.t[:, lo:512], i == 0, i == nb - 1, reads=rd, writes=[accO.b])
                    eng = "dve" if j_ == 0 else "pool"
                    da = dacc[k_["e"] % 2][j_]
                    if g == 0:
                        p.cp(eng, da.t[:], P_.t[:], reads=[P_.b], writes=[da.b])
                    else:
                        p.tt(eng, da.t[:, lo:512], da.t[:, lo:512], P_.t[:, lo:512], ALU.add, reads=[P_.b], writes=[da.b])

            emit_Sg(0)
            for g in range(len(groups)):
                if g + 1 < len(groups):
                    emit_Sg(g + 1)
                emit_PVg(g)
                if g == 3 and pend["f"] is not None:
                    pend["f"]()
                    pend["f"] = None
            e = k_["e"]
            k_["e"] += 1
            rd_, u_, sq_, rs_, yo_ = rden[e % 2], uu[e % 2], squ[e % 2], rsu[e % 2], yo[e % 2]
            d0, d1 = dacc[e % 2]
            p.tt("pool", d1.t[:], d1.t[:], d0.t[:], ALU.add, reads=[d0.b], writes=[d1.b])
            p.mm(accD.t[:], onesf.t[:], d1.t[:], True, True, reads=[onesf.b, d1.b], writes=[accD.b])
            p.op("dve", lambda e_, rd_=rd_, accD=accD: e_.reciprocal(rd_.t[:], accD.t[:]), reads=[accD.b], writes=[rd_.b])
            p.tt("dve", u_.t[:], accO.t[:], rd_.t[:], ALU.mult, reads=[accO.b, rd_.b], writes=[u_.b])
            p.tt("pool", sq_.t[:], u_.t[:], u_.t[:], ALU.mult, reads=[u_.b], writes=[sq_.b])

            def part2(u_=u_, sq_=sq_, rs_=rs_, yo_=yo_, h=h, q0=q0):
                pss = c.ps[6]
                p.mm(pss.t[:], c.ones.t[:], sq_.t[:], True, True, reads=[c.ones.b, sq_.b], writes=[pss.b])
                p.act(rs_.t[:], pss.t[:], AF.Ln, reads=[pss.b], writes=[rs_.b], bias=EPS, scale=1.0 / 128)
                p.act(rs_.t[:], rs_.t[:], AF.Exp, reads=[rs_.b], writes=[rs_.b], scale=-0.5)
                p.tt("pool", yo_.t[:], u_.t[:], rs_.t[:], ALU.mult, reads=[u_.b, rs_.b], writes=[yo_.b])
                p.dma(c.yT[:, h, q0:q0 + 512], yo_.t[:], reads=[yo_.b])
            pend["f"] = part2
    if pend["f"] is not None:
        pend["f"]()
        pend["f"] = None


def phase_D(c, xin, last):
    nc, p, sb, T, NT, NCH = c.nc, c.p, c.sb, c.T, c.NT, c.NCH
    li, vo = c.li, c.vo
    p.barrier()
    sb.reset(c.persist_mark)
    vec, cf, cbf = c.vecs, c.cstf, c.cb
    idb = cbf.t[:, C_ID:C_ID + 128]
    wo = sb.tile([128, 8, D], BF16, "wo")
    wu = sb.tile([128, 8, 2 * DFF], BF16, "wu")
    mW = sb.mark()
    stg = [sb.tile([128, DFF], F32, "stgD") for _ in range(3)]
    alt = Alt(["act", "dve"])
    for k in range(8):
        st = stg[k % 3]
        p.dma(st.t[:, 0:D], c.w_out[li, k * 128:(k + 1) * 128, :], writes=[st.b])
        scaled_copy(c, alt(), wo.t[:, k, :], st.t[:, 0:D], vec.t[:, vo + V_ON + k:vo + V_ON + k + 1], [st.b, vec.b], [wo.b])
    pieces = [(k, hf) for hf in range(2) for k in range(8)]
    wub = [[Buf() for _ in range(2)] for _ in range(8)]

    def piece_dma(j):
        if j < len(pieces):
            k, hf = pieces[j]
            st = stg[j % 3]
            p.dma(st.t[:], c.w_up[li, k * 128:(k + 1) * 128, hf * DFF:(hf + 1) * DFF], writes=[st.b])

    def piece_cvt(j):
        if j < len(pieces):
            k, hf = pieces[j]
            st = stg[j % 3]
            scaled_copy(c, alt(), wu.t[:, k, hf * DFF:(hf + 1) * DFF], st.t[:], vec.t[:, vo + V_FN + k:vo + V_FN + k + 1], [st.b, vec.b], [wub[k][hf]])
    piece_dma(0)
    piece_dma(1)
    pj = {"j": 0}

    def piece_step():
        j = pj["j"]
        if j < len(pieces):
            piece_dma(j + 2)
            piece_cvt(j)
            pj["j"] += 1
    wu_a = [wub[k][0] for k in range(8)]
    wu_all = [wub[k][hf] for k in range(8) for hf in range(2)]
    yt = [sb.tile([128, 8, 512], BF16, "ytD") for _ in range(2)]
    xt = [sb.tile([128, D], F32, "xtD") for _ in range(2)]
    xm = [sb.tile([128, D], F32, "xmD") for _ in range(2)]
    junk = sb.tile([128, D], BF16, "junkD")
    ssq = [sb.tile([128, 2], F32, "ssqD") for _ in range(2)]
    hN = [sb.tile([128, D], BF16, "hND") for _ in range(2)]
    hTt = [sb.tile([128, 8, 512], BF16, "hTD") for _ in range(2)]
    i = 0

    def load_x1(ix):
        if ix < 4 * NT:
            p.dma(xt[ix % 2].t[:], xin[ix * 128:(ix + 1) * 128, :], writes=[xt[ix % 2].b])

    def load_y1(tx):
        if tx < NT:
            p.dma(yt[tx % 2].t[:], c.yT[:, :, tx * 512:(tx + 1) * 512], writes=[yt[tx % 2].b])
    load_y1(0)
    load_x1(0)
    for tt in range(NT):
        t0 = tt * 512
        yt_, hT_ = yt[tt % 2], hTt[tt % 2]
        load_y1(tt + 1)
        for s in range(4):
            x_, xm_, ss_, hN_ = xt[i % 2], xm[i % 2], ssq[i % 2], hN[i % 2]
            i += 1
            r0 = t0 + s * 128
            load_x1(i)
            for hf in range(2):
                ps = c.nps()
                for k in range(8):
                    p.mm(ps.t[:], yt_.t[:, k, s * 128:(s + 1) * 128], wo.t[:, k, hf * 512:(hf + 1) * 512], k == 0, k == 7,
                         reads=[yt_.b, wo.b], writes=[ps.b])
                p.tt("dve", xm_.t[:, hf * 512:(hf + 1) * 512], ps.t[:], x_.t[:, hf * 512:(hf + 1) * 512], ALU.add, reads=[ps.b, x_.b], writes=[xm_.b])
            p.dma(c.xres[r0:r0 + 128, :], xm_.t[:], reads=[xm_.b])
            p.act(junk.t[:], xm_.t[:], AF.Square, reads=[xm_.b], writes=[junk.b, ss_.b], accum=ss_.t[:, 0:1])
            rstd_from_ss(c, ss_.t[:, 1:2], ss_.t[:, 0:1], D, [ss_.b], [ss_.b])
            p.act(hN_.t[:], xm_.t[:], AF.Copy, reads=[xm_.b, ss_.b], writes=[hN_.b], scale=ss_.t[:, 1:2])
            for k in range(8):
                p.tr(c.psb.t[:, k * 128:(k + 1) * 128], hN_.t[:, k * 128:(k + 1) * 128], idb, reads=[hN_.b, cbf.b], writes=[c.psb.b])
            p.cp("pool" if False else "dve", hT_.t[:, :, s * 128:(s + 1) * 128], r3(c.psb.t[:], a=8), reads=[c.psb.b], writes=[hT_.b])
            piece_step()
        p.dma(c.hT[:, :, t0:t0 + 512], hT_.t[:], reads=[hT_.b])
    while pj["j"] < len(pieces):
        piece_step()
    if c.stop == "D1":
        return
    hl = hTt[(NT - 1) % 2]
    ps = c.nps()
    for cc in range(NFC):
        for k in range(8):
            p.mm(ps.t[:, 2 * cc:2 * cc + 2], wu.t[:, k, cc * 128:(cc + 1) * 128], hl.t[:, k, 510:512], k == 0, k == 7,
                 reads=[wub[k][0], hl.b], writes=[ps.b])
    hout = sb.tile([128, 2 * NFC], F32, "hout")
    p.cp("act", hout.t[:], ps.t[:, 0:2 * NFC], reads=[ps.b], writes=[hout.b])
    p.dma(c.halo[:, :], hout.t[:], reads=[hout.b])
    exchange(c, c.halo, c.hgat)
    if c.stop == "Dh":
        return
    sb.reset(mW)
    hin = sb.tile([128, 2 * NFC], F32, "hin")
    p.dma(hin.t[:], c.hgat[0:128, :], writes=[hin.b])
    hal = sb.tile([128, NFC, 2], F32, "hal")
    halb = [Buf() for _ in range(NFC)]
    p.ts("dve", hal.t[:], r3(hin.t[:], a=NFC), c.flg.t[:, 0:1], None, ALU.mult, reads=[hin.b, c.flg.b], writes=halb)
    hT2 = [sb.tile([128, 8, 512], BF16, "hT2") for _ in range(2)]
    asb = [sb.tile([128, 514], F32, "asb") for _ in range(3)]
    t1 = [sb.tile([128, 512], F32, "t1D") for _ in range(2)]
    t2 = [sb.tile([128, 512], F32, "t2D") for _ in range(2)]
    t3 = [sb.tile([128, 512], F32, "t3D") for _ in range(2)]
    sl = [sb.tile([128, 512], F32, "slD") for _ in range(2)]
    gTt = [sb.tile([128, NFC, 512], BF16, "gTt") for _ in range(2)]
    cw = lambda k_, cc: vec.t[:, vo + V_CW + k_ * NFC + cc:vo + V_CW + k_ * NFC + cc + 1]
    cbv = lambda cc: vec.t[:, vo + V_CB + cc:vo + V_CB + cc + 1]
    j = 0

    def load_h2(tx):
        if tx < NT:
            p.dma(hT2[tx % 2].t[:], c.hT[:, :, tx * 512:(tx + 1) * 512], writes=[hT2[tx % 2].b])
    load_h2(0)
    for tt in range(NT):
        t0 = tt * 512
        h_ = hT2[tt % 2]
        g_ = gTt[tt % 2]
        load_h2(tt + 1)
        for cc in range(NFC):
            psa, psg = c.nps(), c.nps()
            for k in range(8):
                p.mm(psa.t[:], wu.t[:, k, cc * 128:(cc + 1) * 128], h_.t[:, k, :], k == 0, k == 7, reads=[wub[k][0], h_.b], writes=[psa.b])
            for k in range(8):
                p.mm(psg.t[:], wu.t[:, k, DFF + cc * 128:DFF + (cc + 1) * 128], h_.t[:, k, :], k == 0, k == 7, reads=[wub[k][1], h_.b], writes=[psg.b])
            a_ = asb[j % 3]
            t1_, t2_, t3_, s_ = t1[j % 2], t2[j % 2], t3[j % 2], sl[j % 2]
            j += 1
            p.cp("act", a_.t[:, 2:514], psa.t[:], reads=[psa.b], writes=[a_.b])
            p.cp("pool", a_.t[:, 0:2], hal.t[:, cc, :], reads=[halb[cc]], writes=[a_.b])
            p.cp("pool", hal.t[:, cc, :], a_.t[:, 512:514], reads=[a_.b], writes=[halb[cc]])
            p.ts("pool", t1_.t[:], a_.t[:, 2:514], cw(2, cc), cbv(cc), ALU.mult, ALU.add, reads=[a_.b, vec.b], writes=[t1_.b])
            p.stt("dve", t2_.t[:], a_.t[:, 1:513], cw(1, cc), t1_.t[:], ALU.mult, ALU.add, reads=[a_.b, vec.b, t1_.b], writes=[t2_.b])
            p.stt("dve", t3_.t[:], a_.t[:, 0:512], cw(0, cc), t2_.t[:], ALU.mult, ALU.add, reads=[a_.b, vec.b, t2_.b], writes=[t3_.b])
            p.act(s_.t[:], t3_.t[:], AF.Silu, reads=[t3_.b], writes=[s_.b])
            p.tt("dve", g_.t[:, cc, :], s_.t[:], psg.t[:], ALU.mult, reads=[s_.b, psg.b], writes=[g_.b])
        p.dma(c.gT[:, :, t0:t0 + 512], g_.t[:], reads=[g_.b])
    if c.stop == "D2":
        return
    p.barrier()
    sb.reset(c.persist_mark)
    wd = sb.tile([128, NFC, D], BF16, "wd")
    stg = [sb.tile([128, D], F32, "stgD3") for _ in range(2)]
    for cc in range(NFC):
        st = stg[cc % 2]
        p.dma(st.t[:], c.w_down[li, cc * 128:(cc + 1) * 128, :], writes=[st.b])
        p.cp(alt(), wd.t[:, cc, :], st.t[:], reads=[st.b], writes=[wd.b])
    fnb = None
    if last:
        fnb = sb.tile([128, D], F32, "fnb")
        p.dma(fnb.t[:], c.fnb[:, :], writes=[fnb.b])
    g3 = [sb.tile([128, NFC, 512], BF16, "g3") for _ in range(2)]
    xt = [sb.tile([128, D], F32, "xt3") for _ in range(2)]
    xo = [sb.tile([128, D], F32, "xo3") for _ in range(2)]
    junk = sb.tile([128, D], BF16, "junk3")
    ssq = [sb.tile([128, 2], F32, "ssq3") for _ in range(2)]
    yn = [sb.tile([128, D], F32, "yn3") for _ in range(2)]
    i = 0

    def load_g3(tx):
        if tx < NT:
            p.dma(g3[tx % 2].t[:], c.gT[:, :, tx * 512:(tx + 1) * 512], writes=[g3[tx % 2].b])

    def load_x3(ix):
        if ix < 4 * NT:
            p.dma(xt[ix % 2].t[:], c.xres[ix * 128:(ix + 1) * 128, :], writes=[xt[ix % 2].b])
    load_g3(0)
    load_x3(0)
    for tt in range(NT):
        t0 = tt * 512
        g_ = g3[tt % 2]
        load_g3(tt + 1)
        for s in range(4):
            x_, xo_, ss_, yn_ = xt[i % 2], xo[i % 2], ssq[i % 2], yn[i % 2]
            i += 1
            r0 = t0 + s * 128
            load_x3(i)
            for hf in range(2):
                ps = c.nps()
                for cc in range(NFC):
                    p.mm(ps.t[:], g_.t[:, cc, s * 128:(s + 1) * 128], wd.t[:, cc, hf * 512:(hf + 1) * 512], cc == 0, cc == NFC - 1,
                         reads=[g_.b, wd.b], writes=[ps.b])
                p.tt("dve", xo_.t[:, hf * 512:(hf + 1) * 512], ps.t[:], x_.t[:, hf * 512:(hf + 1) * 512], ALU.add, reads=[ps.b, x_.b], writes=[xo_.b])
            if not last:
                p.dma(c.xres[r0:r0 + 128, :], xo_.t[:], reads=[xo_.b])
            else:
                p.act(junk.t[:], xo_.t[:], AF.Square, reads=[xo_.b], writes=[junk.b, ss_.b], accum=ss_.t[:, 0:1])
                rstd_from_ss(c, ss_.t[:, 1:2], ss_.t[:, 0:1], D, [ss_.b], [ss_.b])
                p.act(yn_.t[:], xo_.t[:], AF.Copy, reads=[xo_.b, ss_.b], writes=[yn_.b], scale=ss_.t[:, 1:2])
                p.tt("pool", yn_.t[:], yn_.t[:], fnb.t[:], ALU.mult, reads=[yn_.b, fnb.b], writes=[yn_.b])
                p.dma(c.out[r0:r0 + 128, :], yn_.t[:], reads=[yn_.b])


_NC_CACHE = {}


def kernel(x, attn_norm, w_in, mla_q_norm, mla_w_uq, mla_kv_norm, mla_w_ukv, mla_out_norm,
           gla_w_gate, gla_b_gate, gla_out_norm, ret_out_norm, w_out, ffn_norm, ffn_w_up,
           ffn_conv_w, ffn_conv_b, ffn_w_down, final_norm):
    f = lambda a: np.ascontiguousarray(np.asarray(a, dtype=np.float32))
    x = f(x)
    B, S, _ = x.shape
    T = S // 2
    nl = int(np.asarray(w_in).shape[0])
    assert B * 2 == 8
    inp = dict(attn_norm=f(attn_norm), ffn_norm=f(ffn_norm), mla_q_norm=f(mla_q_norm), mla_kv_norm=f(mla_kv_norm),
               mla_out_norm=f(mla_out_norm), gla_out_norm=f(gla_out_norm), ret_out_norm=f(ret_out_norm),
               gla_b_gate=f(gla_b_gate), ffn_conv_w=f(ffn_conv_w), ffn_conv_b=f(ffn_conv_b))
    key = (T, nl)
    if key not in _NC_CACHE:
        _NC_CACHE[key] = build(T, nl)
    nc = _NC_CACHE[key]
    cst = host_consts(T)
    vec = host_vec(inp, 0, nl)
    fnb = np.ascontiguousarray(np.broadcast_to(f(final_norm)[None, :], (128, D)))
    shared = dict(w_in=f(w_in), w_uq=f(mla_w_uq), w_ukv=f(mla_w_ukv), w_out=f(w_out), w_up=f(ffn_w_up),
                  w_down=f(ffn_w_down), w_gate=f(gla_w_gate), vec=vec, cst=cst, fnb=fnb)
    ropes = [host_rope(T, r * T) for r in range(2)]
    in_maps = []
    for core in range(8):
        b, r = core // 2, core % 2
        m = dict(shared)
        m["x"] = np.ascontiguousarray(x[b, r * T:(r + 1) * T])
        m["rope"] = ropes[r]
        m["flag"] = np.full((128, 1), float(r), np.float32)
        in_maps.append(m)
    res = run_bass_kernel_spmd(nc, in_maps, core_ids=list(range(8)))
    out = np.empty((B, S, D), np.float32)
    for core in range(8):
        b, r = core // 2, core % 2
        out[b, r * T:(r + 1) * T] = np.asarray(res.results[core]["out"])
    return out
```
